# Optimizing a Trainium2 kernel written in Bass

```python
import jax, jax.numpy as jnp
from jax import lax
import numpy as np

D_MODEL = 1024
BATCH = 8
SEQ = 4096
DEPTH = 1

GMLP_WIDTH = D_MODEL
GMLP_GROUPS = 4
GMLP_CHUNK = 128
N_HEADS = 8
HEAD_DIM = D_MODEL // N_HEADS
ATTN_WIDTH = N_HEADS * HEAD_DIM
MOBA_BLOCK = 256
MOBA_TOPK = 3
MOBA_QCHUNK = 16
N_EXPERTS = 32
TOP_K = 4
D_EXPERT = D_MODEL
SWIGLU_LIMIT = 7.0
SWIGLU_ALPHA = 1.702
MOE_BLOCK = 256
LN_EPS = 1e-5
DEEPNORM_ALPHA = (2 * DEPTH) ** 0.25
DEEPNORM_BETA = (8 * DEPTH) ** -0.25
IN_SPLITS = (GMLP_WIDTH, GMLP_WIDTH, ATTN_WIDTH, ATTN_WIDTH, ATTN_WIDTH, D_MODEL, D_MODEL)
IN_COLS = sum(IN_SPLITS)

kernel_name = "hybrid_gmlp_moba_moe_deepnorm"


def layer_norm(x, g, b):
    xf = x.astype(jnp.float32)
    mu = jnp.mean(xf, axis=-1, keepdims=True)
    var = jnp.mean(jnp.square(xf - mu), axis=-1, keepdims=True)
    y = (xf - mu) * lax.rsqrt(var + LN_EPS)
    return (y * g.astype(jnp.float32) + b.astype(jnp.float32)).astype(x.dtype)


def chunked_spatial_gating(u, v, w_s, b_s, ln_g, ln_b):
    B, S, _ = u.shape
    C = GMLP_CHUNK
    nc = S // C
    gd = GMLP_WIDTH // GMLP_GROUPS
    v = layer_norm(v, ln_g, ln_b).reshape(B, nc, C, GMLP_GROUPS, gd)
    causal = jnp.tril(jnp.ones((C, C), dtype=bool))
    w = jnp.where(causal[None], w_s, jnp.zeros_like(w_s))
    vs = jnp.einsum('gts,bcsgd->bctgd', w, v) + b_s.T[None, None, :, :, None]
    return u * vs.reshape(B, S, GMLP_WIDTH)


def moba_attention(q, k, v):
    B, H, S, dh = q.shape
    L = MOBA_BLOCK
    nb = -(-S // L)
    Sp = nb * L
    padw = ((0, 0), (0, 0), (0, Sp - S), (0, 0))
    q = jnp.pad(q, padw)
    k = jnp.pad(k, padw)
    v = jnp.pad(v, padw)
    kb = k.reshape(B, H, nb, L, dh)
    vb = v.reshape(B, H, nb, L, dh)
    k_mean = jnp.mean(kb.astype(jnp.float32), axis=3)
    gate = jnp.einsum('bhsd,bhnd->bhsn', q.astype(jnp.float32), k_mean)
    q_blk = jnp.arange(Sp) // L
    past = jnp.arange(nb)[None, :] < q_blk[:, None]
    gate = jnp.where(past, gate, -jnp.inf)
    topk = min(MOBA_TOPK, nb)
    _, sel = lax.top_k(gate, topk)
    sel_ok = sel < q_blk[:, None]
    scale = dh ** -0.5
    QC = MOBA_QCHUNK
    bi = jnp.arange(B)[:, None, None, None]
    hi = jnp.arange(H)[None, :, None, None]

    def query_chunk(c):
        s0 = c * QC
        qc = lax.dynamic_slice_in_dim(q, s0, QC, axis=2)
        idx = lax.dynamic_slice_in_dim(sel, s0, QC, axis=2)
        ok = lax.dynamic_slice_in_dim(sel_ok, s0, QC, axis=2)
        k_sel = kb[bi, hi, idx]
        v_sel = vb[bi, hi, idx]
        own = s0 // L
        k_own = lax.dynamic_index_in_dim(kb, own, axis=2, keepdims=False)
        v_own = lax.dynamic_index_in_dim(vb, own, axis=2, keepdims=False)
        s_sel = jnp.einsum('bhqd,bhqnld->bhqnl', qc, k_sel).astype(jnp.float32) * scale
        s_sel = jnp.where(ok[..., None], s_sel, -jnp.inf)
        s_own = jnp.einsum('bhqd,bhld->bhql', qc, k_own).astype(jnp.float32) * scale
        causal = (own * L + jnp.arange(L))[None, :] <= (s0 + jnp.arange(QC))[:, None]
        s_own = jnp.where(causal, s_own, -jnp.inf)
        s_all = jnp.concatenate([s_sel.reshape(B, H, QC, topk * L), s_own], axis=-1)
        p = jax.nn.softmax(s_all, axis=-1).astype(v.dtype)
        p_sel = p[..., :topk * L].reshape(B, H, QC, topk, L)
        p_own = p[..., topk * L:]
        return (jnp.einsum('bhqnl,bhqnld->bhqd', p_sel, v_sel)
                + jnp.einsum('bhql,bhld->bhqd', p_own, v_own))

    outs = lax.map(query_chunk, jnp.arange(Sp // QC))
    out = outs.transpose(1, 2, 0, 3, 4).reshape(B, H, Sp, dh)
    return out[:, :, :S]


def moe_ffn(h, w_router, b_router, w_up, b_up, w_down, b_down):
    B, S, D = h.shape
    x = h.reshape(-1, D)
    T = x.shape[0]
    F = D_EXPERT
    M = MOE_BLOCK
    logits = (x @ w_router).astype(jnp.float32) + b_router.astype(jnp.float32)
    top_val, top_idx = lax.top_k(logits, TOP_K)
    gate = jax.nn.softmax(top_val, axis=-1)
    A = T * TOP_K
    R = (-(-A // M)) * M + N_EXPERTS * M
    flat_e = top_idx.reshape(-1)
    flat_tok = jnp.arange(A, dtype=jnp.int32) // TOP_K
    flat_w = gate.reshape(-1)
    order = jnp.argsort(flat_e)
    e_sorted = flat_e[order]
    counts = jnp.bincount(flat_e, length=N_EXPERTS)
    padded = (counts + M - 1) // M * M
    start = jnp.cumsum(counts) - counts
    pend = jnp.cumsum(padded)
    pstart = pend - padded
    dest = pstart[e_sorted] + jnp.arange(A) - start[e_sorted]
    row_tok = jnp.zeros((R,), jnp.int32).at[dest].set(flat_tok[order])
    row_w = jnp.zeros((R,), jnp.float32).at[dest].set(flat_w[order])
    n_blocks = R // M
    block_e = jnp.minimum(jnp.searchsorted(pend, jnp.arange(n_blocks) * M, side='right'),
                          N_EXPERTS - 1)

    def expert_block(args):
        e, tok = args
        xe = x[tok]
        gu = xe @ w_up[e] + b_up[e]
        g = jnp.minimum(gu[:, :F], SWIGLU_LIMIT)
        u = jnp.clip(gu[:, F:], -SWIGLU_LIMIT, SWIGLU_LIMIT)
        act = (u + 1.0) * (g * jax.nn.sigmoid(SWIGLU_ALPHA * g))
        return act @ w_down[e] + b_down[e]

    y = lax.map(expert_block, (block_e, row_tok.reshape(n_blocks, M)))
    y = y.reshape(R, D) * row_w[:, None].astype(y.dtype)
    out = jnp.zeros_like(x).at[row_tok].add(y)
    return out.reshape(B, S, D)


def setup_inputs(seed: int = 0) -> dict:
    key = jax.random.key(seed)
    ks = jax.random.split(key, 24)
    f32 = jnp.float32

    def nrm(k, shape, scale):
        return jax.random.normal(k, shape, f32) * scale

    D, L, E, F = D_MODEL, DEPTH, N_EXPERTS, D_EXPERT
    C, G = GMLP_CHUNK, GMLP_GROUPS
    return {
        "x": nrm(ks[0], (BATCH, SEQ, D), 1.0),
        "ln_in_g": 1.0 + nrm(ks[1], (D,), 0.02),
        "ln_in_b": nrm(ks[2], (D,), 0.02),
        "w_in": nrm(ks[3], (L, D, IN_COLS), D ** -0.5),
        "gmlp_ln_g": 1.0 + nrm(ks[4], (L, GMLP_WIDTH), 0.02),
        "gmlp_ln_b": nrm(ks[5], (L, GMLP_WIDTH), 0.02),
        "w_spatial": nrm(ks[6], (L, G, C, C), C ** -0.5),
        "b_spatial": 1.0 + nrm(ks[7], (L, G, C), 0.02),
        "w_branch_a": nrm(ks[8], (L, GMLP_WIDTH, D), GMLP_WIDTH ** -0.5),
        "w_branch_b": nrm(ks[9], (L, ATTN_WIDTH, D), ATTN_WIDTH ** -0.5),
        "w_out": nrm(ks[10], (L, D, D), DEEPNORM_BETA * D ** -0.5),
        "ln_mix_g": 1.0 + nrm(ks[11], (L, D), 0.02),
        "ln_mix_b": nrm(ks[12], (L, D), 0.02),
        "w_router": nrm(ks[13], (L, D, E), D ** -0.5),
        "b_router": nrm(ks[14], (L, E), 0.01),
        "w_up": nrm(ks[15], (L, E, D, 2 * F), D ** -0.5),
        "b_up": nrm(ks[16], (L, E, 2 * F), 0.01),
        "w_down": nrm(ks[17], (L, E, F, D), DEEPNORM_BETA * F ** -0.5),
        "b_down": nrm(ks[18], (L, E, D), 0.01),
        "ln_ffn_g": 1.0 + nrm(ks[19], (L, D), 0.02),
        "ln_ffn_b": nrm(ks[20], (L, D), 0.02),
    }


def reference(x, ln_in_g, ln_in_b, w_in, gmlp_ln_g, gmlp_ln_b, w_spatial, b_spatial,
              w_branch_a, w_branch_b, w_out, ln_mix_g, ln_mix_b, w_router, b_router,
              w_up, b_up, w_down, b_down, ln_ffn_g, ln_ffn_b):
    B, S, D = x.shape
    offsets = list(np.cumsum(IN_SPLITS)[:-1])
    h = layer_norm(x, ln_in_g, ln_in_b)
    for l in range(DEPTH):
        p = h @ w_in[l]
        u_a, v_a, q, k, v, g_a, g_b = jnp.split(p, offsets, axis=-1)
        y_a = chunked_spatial_gating(jax.nn.gelu(u_a, approximate=False),
                                     jax.nn.gelu(v_a, approximate=False),
                                     w_spatial[l], b_spatial[l], gmlp_ln_g[l], gmlp_ln_b[l])
        def heads(t):
            return t.reshape(B, S, N_HEADS, HEAD_DIM).transpose(0, 2, 1, 3)
        y_b = moba_attention(heads(q), heads(k), heads(v))
        y_b = y_b.transpose(0, 2, 1, 3).reshape(B, S, ATTN_WIDTH)
        merged = (jax.nn.sigmoid(g_a) * (y_a @ w_branch_a[l])
                  + jax.nn.sigmoid(g_b) * (y_b @ w_branch_b[l]))
        h = layer_norm(DEEPNORM_ALPHA * h + merged @ w_out[l], ln_mix_g[l], ln_mix_b[l])
        f = moe_ffn(h, w_router[l], b_router[l], w_up[l], b_up[l], w_down[l], b_down[l])
        h = layer_norm(DEEPNORM_ALPHA * h + f, ln_ffn_g[l], ln_ffn_b[l])
    return h
```

```python
import os
import bisect
import numpy as np
from contextlib import ExitStack
import concourse.bass as bass
import concourse.mybir as mybir
from concourse.bass_utils import run_bass_kernel_spmd

F32 = mybir.dt.float32
BF16 = mybir.dt.bfloat16
I32 = mybir.dt.int32
AF = mybir.ActivationFunctionType
ALU = mybir.AluOpType
AX = mybir.AxisListType

S_TOK = 4096
D = 1024
NT = 32
NE = 32
CAP = 768
NSL = CAP // 128
ALPHA = float(2 ** 0.25)
EPS = 1e-5
DEBUG = bool(int(os.environ.get("MK_DEBUG", "0")))
STOP_AFTER = int(os.environ.get("MK_STOP", "99"))


class Buf:
    __slots__ = ("w", "r")

    def __init__(self):
        self.w = None
        self.r = []


class Eng:
    def __init__(self, nc, eng, name):
        self.eng = eng
        self.name = name
        self.sem = nc.alloc_semaphore(name=name)
        self.count = 0
        self.seen = {}


class Sync:
    def __init__(self, nc, waitsets=None):
        self.nc = nc
        self.waitsets = waitsets
        self.waited = {}
        self.pe = Eng(nc, nc.tensor, "s_pe")
        self.act = Eng(nc, nc.scalar, "s_act")
        self.dve = Eng(nc, nc.vector, "s_dve")
        self.pool = Eng(nc, nc.gpsimd, "s_pool")
        self.sp = Eng(nc, nc.sync, "s_sp")
        self.engs = [self.pe, self.act, self.dve, self.pool, self.sp]
        self.dsems = {}
        self._keep = []
        self.wsets = {k: set(v) for k, v in (waitsets or {}).items()}

    def dsem(self, key):
        k = id(key)
        if k not in self.dsems:
            self.dsems[k] = Eng(self.nc, None, "d_%d" % len(self.dsems))
            self._keep.append(key)
        return self.dsems[k]

    def _wait(self, E, reads, writes):
        deps = {}

        def add(tok):
            if tok is None:
                return
            s, v = tok
            if deps.get(s, 0) < v:
                deps[s] = v

        for t in reads:
            add(t.w)
        for t in writes:
            add(t.w)
            for r in t.r:
                add(r)
        for s, v in deps.items():
            if E.seen.get(s, 0) < v:
                E.eng.wait_ge(s.sem, self._val(s, v))
                E.seen[s] = v

    def _val(self, s, v):
        if s.eng is None:
            return v
        if self.waitsets is None:
            self.waited.setdefault(s.name, set()).add(v)
            return v
        return bisect.bisect_right(self.waitsets[s.name], v)

    def _signals(self, E):
        return self.waitsets is None or E.count in self.wsets.get(E.name, ())

    def _done(self, tok, reads, writes):
        for t in reads:
            t.r.append(tok)
            if len(t.r) > 64:
                best = {}
                for s, v in t.r:
                    if best.get(s, 0) < v:
                        best[s] = v
                t.r = list(best.items())
        for t in writes:
            t.w = tok
            t.r = []

    def op(self, E, fn, reads=(), writes=()):
        self._wait(E, reads, writes)
        inst = fn(E.eng)
        E.count += 1
        if self._signals(E):
            inst.then_inc(E.sem, 1)
        self._done((E, E.count), reads, writes)

    def group(self, E, fns, reads=(), writes=()):
        self._wait(E, reads, writes)
        inst = None
        for fn in fns:
            inst = fn(E.eng)
        E.count += 1
        if self._signals(E):
            inst.then_inc(E.sem, 1)
        self._done((E, E.count), reads, writes)

    def dma(self, Q, dname, fn, reads=(), writes=()):
        key = writes[0] if len(writes) else reads[0]
        Dm = self.dsem(key)
        self._wait(Q, reads, writes)
        inst = fn(Q.eng)
        Dm.count += 16
        inst.then_inc(Dm.sem, 16)
        self._done((Dm, Dm.count), reads, writes)

    def barrier(self):
        allq = self.engs + list(self.dsems.values())
        for E in self.engs:
            for X in allq:
                if X is E or X.count == 0:
                    continue
                if E.seen.get(X, 0) < X.count:
                    E.eng.wait_ge(X.sem, self._val(X, X.count))
                    E.seen[X] = X.count


class Ring:
    def __init__(self, alloc, name, shape, dt, n):
        self.items = [(alloc("%s_%d" % (name, i), shape, dt), Buf()) for i in range(n)]
        self.i = 0

    def next(self):
        it = self.items[self.i % len(self.items)]
        self.i += 1
        return it


def build_program():
    _, waited = _build(None)
    nc, _ = _build({k: sorted(v) for k, v in waited.items()})
    return nc


def _build(waitsets):
    nc = bass.Bass("TRN2", target_bir_lowering=False)

    def din(name, shape, dt=F32):
        return nc.dram_tensor(name, list(shape), dt, kind="ExternalInput").ap()

    x_d = din("x", [S_TOK, D])
    ln_in_g = din("ln_in_g", [D])
    ln_in_b = din("ln_in_b", [D])
    w_in = din("w_in", [D, 7 * D])
    gmlp_g = din("gmlp_ln_g", [D])
    gmlp_b = din("gmlp_ln_b", [D])
    w_sp = din("w_spatial", [4, 128, 128])
    b_sp = din("b_spatial", [512])
    w_ba = din("w_branch_a", [D, D])
    w_bb = din("w_branch_b", [D, D])
    w_o = din("w_out", [D, D])
    ln_mix_g = din("ln_mix_g", [D])
    ln_mix_b = din("ln_mix_b", [D])
    w_rt = din("w_router", [D, NE])
    b_rt = din("b_router", [NE])
    w_up = din("w_up", [NE, D, 2 * D])
    b_up = din("b_up", [NE, 2 * D])
    w_dn = din("w_down", [NE, D, D])
    b_dn = din("b_down", [NE, D])
    ln_ffn_g = din("ln_ffn_g", [D])
    ln_ffn_b = din("ln_ffn_b", [D])
    out_d = nc.dram_tensor("out", [S_TOK, D], F32, kind="ExternalOutput").ap()

    dbgkind = "ExternalOutput" if DEBUG else "Internal"
    YBd = nc.dram_tensor("ybd", [128, 8, S_TOK], BF16, kind=dbgkind).ap()
    MBd = nc.dram_tensor("mbd", [128, 8, S_TOK], BF16, kind="Internal").ap()
    MGd = nc.dram_tensor("mgd", [128, 8, S_TOK], BF16, kind=dbgkind).ap()
    H2d = nc.dram_tensor("h2d", [S_TOK, D], F32, kind=dbgkind).ap()
    Xg = nc.dram_tensor("xg", [NE * CAP, D], BF16, kind="Internal").ap()
    Hd = nc.dram_tensor("hd", [S_TOK, D], F32, kind="Internal").ap()
    HTd = nc.dram_tensor("htd", [128, 8, S_TOK], BF16, kind="Internal").ap()
    Yg = nc.dram_tensor("yg", [NE * CAP, D], F32, kind="Internal").ap()
    if DEBUG:
        IDXd = nc.dram_tensor("idxd", [128, NT * 4], I32, kind="ExternalOutput").ap()
        GATd = nc.dram_tensor("gatd", [128, NT * 4], F32, kind="ExternalOutput").ap()

    win_v = w_in.rearrange("(k p) n -> p k n", p=128)

    with ExitStack() as es:
        def sbg(name, shape, dt):
            return es.enter_context(nc.sbuf_tensor(name, shape, dt))

        def psg(name, shape, dt):
            return es.enter_context(nc.psum_tensor(name, shape, dt))

        S = Sync(nc, waitsets)
        PE, ACT, DVE, POOL, SP = S.pe, S.act, S.dve, S.pool, S.sp

        psT = Ring(psg, "psT", [128, 8, 128], BF16, 2)
        psA = Ring(psg, "psA", [128, 512], F32, 4)
        psO_t = psg("psO", [128, 512], F32)
        psO_b = Buf()
        psR_t = psg("psR", [128, 512], F32)
        psR_b = Buf()

        onesf = sbg("onesf", [128, 128], F32)
        identf = sbg("identf", [128, 128], F32)
        ident = sbg("ident", [128, 128], BF16)
        onesb = sbg("onesb", [128, 128], BF16)
        tri = sbg("tri", [128, 128], BF16)
        ustr = sbg("ustr", [128, 128], BF16)
        idx_all = sbg("idx_all", [128, NT, 4], I32)
        gate_all = sbg("gate_all", [128, NT, 4], F32)
        b_const = Buf()
        b_idx = Buf()
        b_gate = Buf()
        S.op(POOL, lambda e: e.memset(onesf[:], 1.0), writes=[b_const])
        S.op(POOL, lambda e: e.memset(onesb[:], 1.0), writes=[b_const])
        S.op(POOL, lambda e: e.affine_select(out=identf[:], in_=onesf[:], pattern=[[-1, 128]], compare_op=ALU.is_equal,
                                             fill=0.0, base=0, channel_multiplier=1), reads=[b_const], writes=[b_const])
        S.op(POOL, lambda e: e.affine_select(out=ident[:], in_=onesf[:], pattern=[[-1, 128]], compare_op=ALU.is_equal,
                                             fill=0.0, base=0, channel_multiplier=1), reads=[b_const], writes=[b_const])
        S.op(POOL, lambda e: e.affine_select(out=tri[:], in_=onesf[:], pattern=[[1, 128]], compare_op=ALU.is_ge,
                                             fill=0.0, base=0, channel_multiplier=-1), reads=[b_const], writes=[b_const])
        S.op(POOL, lambda e: e.affine_select(out=ustr[:], in_=onesf[:], pattern=[[1, 128]], compare_op=ALU.is_ge,
                                             fill=0.0, base=-1, channel_multiplier=-1), reads=[b_const], writes=[b_const])

        def bcast_load(dst, src1d, buf):
            S.dma(SP, "const", lambda e: e.dma_start(out=dst, in_=src1d.partition_broadcast(128)), writes=[buf])

        def ln_stats_a(sc, src, b_src):
            st, b_st = sc["st"].next()
            mv, b_mv = sc["mv"].next()
            rs, b_rs = sc["rs"].next()
            for i in range(2):
                S.op(DVE, lambda e, i=i: e.bn_stats(out=st[:, i, :], in_=src[:, i * 512:(i + 1) * 512]),
                     reads=[b_src], writes=[b_st])
            S.op(DVE, lambda e: e.bn_aggr(out=mv[:], in_=st[:].rearrange("p a b -> p (a b)")), reads=[b_st], writes=[b_mv])
            S.op(DVE, lambda e: e.tensor_scalar(out=rs[:], in0=mv[:, 1:2], scalar1=EPS, scalar2=None, op0=ALU.add),
                 reads=[b_mv], writes=[b_rs])
            return (mv, b_mv, rs, b_rs)

        def ln_stats_b(stats):
            mv, b_mv, rs, b_rs = stats
            S.op(ACT, lambda e: e.activation(out=rs[:], in_=rs[:], func=AF.Sqrt), reads=[b_rs], writes=[b_rs])

        def ln_stats_c(stats):
            mv, b_mv, rs, b_rs = stats
            S.op(DVE, lambda e: e.reciprocal(out=rs[:], in_=rs[:]), reads=[b_rs], writes=[b_rs])

        def ln_stats(sc, src, b_src):
            stats = ln_stats_a(sc, src, b_src)
            ln_stats_b(stats)
            ln_stats_c(stats)
            return stats

        def ln_apply(sc, src, b_src, stats, g_t, b_t, b_par, dst, b_dst):
            mv, b_mv, rs, b_rs = stats
            tmp, b_tmp = sc["tmp"].next()
            S.op(DVE, lambda e: e.scalar_tensor_tensor(out=tmp[:], in0=src, scalar=mv[:, 0:1], in1=g_t[:],
                                                       op0=ALU.subtract, op1=ALU.mult),
                 reads=[b_src, b_mv, b_par], writes=[b_tmp])
            S.op(DVE, lambda e: e.scalar_tensor_tensor(out=dst, in0=tmp[:], scalar=rs[:, 0:1], in1=b_t[:],
                                                       op0=ALU.mult, op1=ALU.add),
                 reads=[b_tmp, b_rs, b_par], writes=[b_dst])

        def ln_apply_split(sc, src, b_src, stats, g_t, b_t, b_par, dst, b_dst):
            mv, b_mv, rs, b_rs = stats
            tmp, b_tmp = sc["tmp"].next()
            S.op(DVE, lambda e: e.scalar_tensor_tensor(out=tmp[:], in0=src, scalar=mv[:, 0:1], in1=g_t[:],
                                                       op0=ALU.subtract, op1=ALU.mult),
                 reads=[b_src, b_mv, b_par], writes=[b_tmp])
            S.op(ACT, lambda e: e.activation(out=tmp[:], in_=tmp[:], func=AF.Identity, scale=rs[:, 0:1]),
                 reads=[b_tmp, b_rs], writes=[b_tmp])
            S.op(POOL, lambda e: e.tensor_tensor(out=dst, in0=tmp[:], in1=b_t[:], op=ALU.add),
                 reads=[b_tmp, b_par], writes=[b_dst])

        def layer_norm(sc, src, b_src, g_t, b_t, b_par, dst, b_dst):
            stats = ln_stats(sc, src, b_src)
            ln_apply(sc, src, b_src, stats, g_t, b_t, b_par, dst, b_dst)

        def ln_scratch(alloc, pfx, n=2):
            return {"st": Ring(alloc, pfx + "st", [128, 2, 6], F32, n), "mv": Ring(alloc, pfx + "mv", [128, 2], F32, n),
                    "rs": Ring(alloc, pfx + "rs", [128, 1], F32, n), "tmp": Ring(alloc, pfx + "tmp", [128, D], F32, 2)}

        def transpose_bf(src_t, b_src, dst_ap, b_dst, evac):
            pt, b_pt = psT.next()
            S.group(PE, [(lambda e, k=k: e.transpose(out=pt[:, k, :], in_=src_t[:, k * 128:(k + 1) * 128], identity=ident[:]))
                         for k in range(8)], reads=[b_src, b_const], writes=[b_pt])
            if evac is ACT:
                S.op(ACT, lambda e: e.copy(out=dst_ap, in_=pt[:]), reads=[b_pt], writes=[b_dst])
            else:
                S.op(DVE, lambda e: e.tensor_copy(out=dst_ap, in_=pt[:]), reads=[b_pt], writes=[b_dst])

        def mm8(ps_ap, lhs_fn, rhs_fn):
            return [(lambda e, k=k: e.matmul(ps_ap, lhsT=lhs_fn(k), rhs=rhs_fn(k), start=(k == 0), stop=(k == 7)))
                    for k in range(8)]

        def load_w_cast(dst, src, buf, dname="w"):
            S.dma(POOL, dname, lambda e: e.dma_start(out=dst, in_=src), writes=[buf])

        with ExitStack() as sa:
            def sba(name, shape, dt):
                return sa.enter_context(nc.sbuf_tensor(name, shape, dt))

            hT = sba("hT", [128, 8, S_TOK], BF16)
            hT_b = [Buf() for _ in range(NT)]

            with ExitStack() as p1:
                def sb1(name, shape, dt):
                    return p1.enter_context(nc.sbuf_tensor(name, shape, dt))

                g_in = sb1("g_in", [128, D], F32)
                bb_in = sb1("bb_in", [128, D], F32)
                b_par1 = Buf()
                bcast_load(g_in[:], ln_in_g, b_par1)
                bcast_load(bb_in[:], ln_in_b, b_par1)
                xr = Ring(sb1, "x1", [128, D], F32, 2)
                hbr = Ring(sb1, "hb1", [128, D], BF16, 2)
                hfr = Ring(sb1, "hf1", [128, D], F32, 3)
                sc1 = ln_scratch(sb1, "l1", 3)
                for t in range(NT):
                    xt, b_xt = xr.next()
                    S.dma(SP, "x", lambda e: e.dma_start(out=xt[:], in_=x_d[t * 128:(t + 1) * 128, :]), writes=[b_xt])
                    hf, b_hf = hfr.next()
                    layer_norm(sc1, xt[:], b_xt, g_in, bb_in, b_par1, hf[:], b_hf)
                    S.dma(POOL, "st", lambda e: e.dma_start(out=Hd[t * 128:(t + 1) * 128, :], in_=hf[:]), reads=[b_hf])
                    hb, b_hb = hbr.next()
                    S.op(ACT, lambda e: e.copy(out=hb[:], in_=hf[:]), reads=[b_hf], writes=[b_hb])
                    transpose_bf(hb, b_hb, hT[:, :, t * 128:(t + 1) * 128], hT_b[t], DVE if t % 2 else ACT)
                S.barrier()

            if STOP_AFTER >= 2:
              with ExitStack() as p2:
                def sb2(name, shape, dt):
                    return p2.enter_context(nc.sbuf_tensor(name, shape, dt))

                S.dma(SP, "st", lambda e: e.dma_start(out=HTd[:, :, :], in_=hT[:]), reads=hT_b)
                Esel = sb2("Esel", [128, 32, 128], BF16)
                PB = sb2("PB", [128, 32, 32], F32)
                C1 = sb2("C1", [128, 32, 32], F32)
                C2 = sb2("C2", [128, 32, 32], F32)
                zt = sb2("zt", [128, 32, 32], F32)
                b_c2 = Buf()
                S.op(POOL, lambda e: e.memset(Esel[:], 1.0), writes=[b_c2])
                S.op(POOL, lambda e: e.affine_select(out=Esel[:], in_=Esel[:], pattern=[[-1, 32], [0, 128]],
                                                     compare_op=ALU.is_equal, fill=0.0, base=0, channel_multiplier=1),
                     reads=[b_c2], writes=[b_c2])
                S.op(POOL, lambda e: e.memset(zt[:], 0.0), writes=[b_c2])
                blkpat = [[1, 16], [0, 2], [-1, 16], [0, 2]]
                S.op(POOL, lambda e: e.affine_select(out=PB[:], in_=zt[:], pattern=blkpat, compare_op=ALU.is_ge,
                                                     fill=-1e30, base=-1, channel_multiplier=0), reads=[b_c2], writes=[b_c2])
                S.op(POOL, lambda e: e.affine_select(out=C2[:], in_=zt[:], pattern=blkpat, compare_op=ALU.is_equal,
                                                     fill=-30000.0, base=0, channel_multiplier=0), reads=[b_c2], writes=[b_c2])
                S.op(POOL, lambda e: e.affine_select(out=C2[:], in_=C2[:], pattern=[[1, 32], [-1, 32]], compare_op=ALU.is_ge,
                                                     fill=-30000.0, base=0, channel_multiplier=0), reads=[b_c2], writes=[b_c2])
                S.op(POOL, lambda e: e.memset(zt[:], 30000.0), reads=[b_c2], writes=[b_c2])
                S.op(POOL, lambda e: e.affine_select(out=C1[:], in_=zt[:], pattern=blkpat, compare_op=ALU.is_ge,
                                                     fill=0.0, base=-1, channel_multiplier=0), reads=[b_c2], writes=[b_c2])

                wqkv_r = Ring(sb2, "wqkv", [128, 3, 8, 128], BF16, 3)
                NB2 = 2
                qT_l = [sb2("qT%d" % i, [128, S_TOK], BF16) for i in range(NB2)]
                kT_l = [sb2("kT%d" % i, [128, S_TOK], BF16) for i in range(NB2)]
                Vt_l = [sb2("Vt%d" % i, [128, NT, 128], BF16) for i in range(NB2)]
                bT_l = [sb2("biasT%d" % i, [128, S_TOK], BF16) for i in range(NB2)]
                qT_bl = [[Buf() for _ in range(8)] for _ in range(NB2)]
                kT_bl = [[Buf() for _ in range(8)] for _ in range(NB2)]
                V_bl = [[Buf() for _ in range(8)] for _ in range(NB2)]
                bias_bl = [[Buf() for _ in range(8)] for _ in range(NB2)]
                for i in range(NB2):
                    S.op(POOL, lambda e, i=i: e.memset(bT_l[i][:], 0.0), writes=bias_bl[i])
                kmf = sb2("kmf", [128, 16], F32)
                km2 = sb2("km2", [128, 32], BF16)
                b_km = Buf()
                gm = sb2("gm", [128, 32, 32], F32)
                b_gm = Buf()
                m8 = sb2("m8", [128, 32, 8], F32)
                b_m8 = Buf()
                pT_r = Ring(sb2, "pT", [128, 512], BF16, 4)
                rinv_r = Ring(sb2, "rinv", [128, 512], F32, 2)
                osb_r = Ring(sb2, "osb", [128, 512], F32, 2)
                ybt_r = Ring(sb2, "ybt", [128, 512], BF16, 2)
                qscale = float(128 ** -0.5)

                wq_of = {}

                def load_w(hd):
                    wq, b_wq = wqkv_r.next()
                    for i in range(3):
                        c0 = 2048 + i * 1024 + hd * 128
                        load_w_cast(wq[:, i, :, :], win_v[:, :, c0:c0 + 128], b_wq, "w")
                    wq_of[hd] = (wq, b_wq)

                def prep_head(hd):
                    sl = hd % NB2
                    qT, kT, Vt, biasT = qT_l[sl], kT_l[sl], Vt_l[sl], bT_l[sl]
                    qT_b, kT_b, V_b, bias_b = qT_bl[sl], kT_bl[sl], V_bl[sl], bias_bl[sl]
                    wq, b_wq = wq_of[hd]
                    for gi in range(8):
                        ps, b_ps = psA.next()
                        S.group(PE, mm8(ps[:], lambda k: wq[:, 0, k, :], lambda k: hT[:, k, gi * 512:(gi + 1) * 512]),
                                reads=[b_wq] + hT_b[4 * gi:4 * gi + 4], writes=[b_ps])
                        S.op(ACT, lambda e: e.activation(out=qT[:, gi * 512:(gi + 1) * 512], in_=ps[:], func=AF.Copy,
                                                         scale=qscale), reads=[b_ps], writes=[qT_b[gi]])
                        ps, b_ps = psA.next()
                        S.group(PE, mm8(ps[:], lambda k: wq[:, 1, k, :], lambda k: hT[:, k, gi * 512:(gi + 1) * 512]),
                                reads=[b_wq] + hT_b[4 * gi:4 * gi + 4], writes=[b_ps])
                        S.op(DVE, lambda e: e.tensor_copy(out=kT[:, gi * 512:(gi + 1) * 512], in_=ps[:]),
                             reads=[b_ps], writes=[kT_b[gi]])
                    for g4 in range(8):
                        ps, b_ps = psA.next()
                        fns = []
                        for i in range(4):
                            t = 4 * g4 + i
                            fns += mm8(ps[:, i * 128:(i + 1) * 128], lambda k, t=t: hT[:, k, t * 128:(t + 1) * 128],
                                       lambda k: wq[:, 2, k, :])
                        S.group(PE, fns, reads=[b_wq] + hT_b[4 * g4:4 * g4 + 4], writes=[b_ps])
                        S.op(ACT, lambda e: e.copy(out=Vt[:, 4 * g4:4 * g4 + 4, :],
                                                   in_=ps[:].rearrange("p (a b) -> p a b", a=4)),
                             reads=[b_ps], writes=[V_b[g4]])
                    S.op(DVE, lambda e: e.tensor_reduce(out=kmf[:], in_=kT[:].rearrange("p (n l) -> p n l", l=256),
                                                        axis=AX.X, op=ALU.add), reads=kT_b, writes=[b_km])
                    S.op(DVE, lambda e: e.tensor_scalar(out=km2[:].rearrange("p (n two) -> p n two", two=2),
                                                        in0=kmf[:].unsqueeze(2).to_broadcast([128, 16, 2]),
                                                        scalar1=1.0 / 256.0, scalar2=None, op0=ALU.mult),
                         reads=[b_km], writes=[b_km])
                    for half in range(2):
                        ps, b_ps = psA.next()
                        fns = []
                        for i in range(16):
                            t = half * 16 + i
                            fns.append(lambda e, t=t, i=i: e.matmul(ps[:, i * 32:(i + 1) * 32], lhsT=qT[:, t * 128:(t + 1) * 128],
                                                                   rhs=km2[:], start=True, stop=True))
                        S.group(PE, fns, reads=[b_km] + qT_b[4 * half:4 * half + 4], writes=[b_ps])
                        S.op(DVE, lambda e: e.tensor_tensor(out=gm[:, half * 16:(half + 1) * 16, :].rearrange("p a b -> p (a b)"),
                                                            in0=ps[:],
                                                            in1=PB[:, half * 16:(half + 1) * 16, :].rearrange("p a b -> p (a b)"),
                                                            op=ALU.add), reads=[b_ps, b_c2], writes=[b_gm])
                    for t in range(NT):
                        S.op(DVE, lambda e, t=t: e.max(out=m8[:, t, :], in_=gm[:, t, :]), reads=[b_gm], writes=[b_m8])
                    S.op(DVE, lambda e: e.tensor_tensor(out=gm[:], in0=gm[:], in1=m8[:, :, 5:6].to_broadcast([128, 32, 32]),
                                                        op=ALU.is_ge), reads=[b_gm, b_m8], writes=[b_gm])
                    S.op(DVE, lambda e: e.tensor_tensor(out=gm[:], in0=gm[:], in1=C1[:], op=ALU.mult),
                         reads=[b_gm, b_c2], writes=[b_gm])
                    S.op(DVE, lambda e: e.tensor_tensor(out=gm[:], in0=gm[:], in1=C2[:], op=ALU.add),
                         reads=[b_gm, b_c2], writes=[b_gm])
                def prep_b(hd):
                    sl = hd % NB2
                    biasT, bias_b = bT_l[sl], bias_bl[sl]
                    for c in range(8):
                        ps, b_ps = psA.next()
                        S.group(PE, [(lambda e, i=i: e.transpose(out=ps[0:32, i * 128:(i + 1) * 128], in_=gm[:, 4 * c + i, :],
                                                                 identity=identf[:])) for i in range(4)],
                                reads=[b_gm, b_const], writes=[b_ps])
                        S.op(ACT, lambda e: e.copy(out=biasT[0:32, c * 512:(c + 1) * 512], in_=ps[0:32, :]),
                             reads=[b_ps], writes=[bias_b[c]])

                def main_head(hd, mid_fn=None):
                    sl = hd % NB2
                    qT, kT, Vt, biasT = qT_l[sl], kT_l[sl], Vt_l[sl], bT_l[sl]
                    qT_b, kT_b, V_b, bias_b = qT_bl[sl], kT_bl[sl], V_bl[sl], bias_bl[sl]
                    its = [(c, j) for c in range(8) for j in range(4 * c + 4)]

                    def stage_a(c, j):
                        ps, b_ps = psA.next()
                        S.group(PE, [
                            lambda e: e.matmul(ps[:], lhsT=kT[:, j * 128:(j + 1) * 128], rhs=qT[:, c * 512:(c + 1) * 512],
                                               start=True, stop=False),
                            lambda e: e.matmul(ps[:], lhsT=Esel[:, j, :], rhs=biasT[:, c * 512:(c + 1) * 512],
                                               start=False, stop=True)],
                            reads=[kT_b[j // 4], qT_b[c], bias_b[c], b_c2], writes=[b_ps])
                        pT, b_pT = pT_r.next()
                        S.op(ACT, lambda e: e.activation(out=pT[:], in_=ps[:], func=AF.Exp), reads=[b_ps], writes=[b_pT])
                        if j >= 4 * c:
                            col = (j - 4 * c) * 128
                            S.op(POOL, lambda e: e.tensor_tensor(out=pT[:, col:col + 128], in0=pT[:, col:col + 128],
                                                                 in1=tri[:], op=ALU.mult),
                                 reads=[b_pT, b_const], writes=[b_pT])
                        return pT, b_pT

                    def stage_b(c, j, pT, b_pT):
                        nj = 4 * c + 4
                        S.group(PE, [
                            lambda e: e.matmul(psO_t[:], lhsT=Vt[:, j, :], rhs=pT[:], start=(j == 0), stop=(j == nj - 1)),
                            lambda e: e.matmul(psR_t[:], lhsT=onesb[:], rhs=pT[:], start=(j == 0), stop=(j == nj - 1))],
                            reads=[V_b[j // 4], b_pT, b_const], writes=[psO_b, psR_b])
                        if j == nj - 1:
                            rinv, b_rinv = rinv_r.next()
                            osb, b_osb = osb_r.next()
                            S.op(ACT, lambda e: e.copy(out=rinv[:], in_=psR_t[:]), reads=[psR_b], writes=[b_rinv])
                            S.op(ACT, lambda e: e.copy(out=osb[:], in_=psO_t[:]), reads=[psO_b], writes=[b_osb])
                            S.op(DVE, lambda e: e.reciprocal(out=rinv[:], in_=rinv[:]), reads=[b_rinv], writes=[b_rinv])
                            ybt, b_ybt = ybt_r.next()
                            S.op(DVE, lambda e: e.tensor_tensor(out=ybt[:], in0=osb[:], in1=rinv[:], op=ALU.mult),
                                 reads=[b_osb, b_rinv], writes=[b_ybt])
                            S.dma(SP, "st", lambda e: e.dma_start(out=YBd[:, hd, c * 512:(c + 1) * 512], in_=ybt[:]),
                                  reads=[b_ybt])

                    SK = 2
                    pend = []
                    for i in range(len(its) + SK):
                        if i == 48 and mid_fn is not None:
                            mid_fn()
                        if i < len(its):
                            pend.append(stage_a(*its[i]))
                        if i >= SK:
                            stage_b(*its[i - SK], *pend[i - SK])

                load_w(0)
                load_w(1)
                prep_head(0)
                prep_b(0)
                for hd in range(8):
                    if hd + 1 < 8:
                        prep_head(hd + 1)
                    if hd + 2 < 8:
                        load_w(hd + 2)
                    main_head(hd, (lambda h=hd + 1: prep_b(h)) if hd + 1 < 8 else None)
                S.barrier()

            if STOP_AFTER >= 3:
              with ExitStack() as p3:
                def sb3(name, shape, dt):
                    return p3.enter_context(nc.sbuf_tensor(name, shape, dt))

                wb = sb3("wb", [128, 8, D], BF16)
                wgb = sb3("wgb", [128, 8, D], BF16)
                b_w3 = Buf()
                load_w_cast(wb[:], w_bb.rearrange("(k p) n -> p k n", p=128), b_w3)
                load_w_cast(wgb[:], win_v[:, :, 6144:7168], b_w3)
                ybg_r = Ring(sb3, "ybg", [128, 8, 512], BF16, 2)
                sg_r = Ring(sb3, "sg3", [128, 512], F32, 2)
                mbt_r = Ring(sb3, "mbt", [128, 8, 512], BF16, 2)
                for c in range(8):
                    ybg, b_ybg = ybg_r.next()
                    S.dma(POOL, "ld3", lambda e: e.dma_start(out=ybg[:], in_=YBd[:, :, c * 512:(c + 1) * 512]), writes=[b_ybg])
                    mbt, b_mbt = mbt_r.next()
                    for dc in range(8):
                        ps1, b_ps1 = psA.next()
                        S.group(PE, mm8(ps1[:], lambda k: wb[:, k, dc * 128:(dc + 1) * 128], lambda k: ybg[:, k, :]),
                                reads=[b_w3, b_ybg], writes=[b_ps1])
                        ps2, b_ps2 = psA.next()
                        S.group(PE, mm8(ps2[:], lambda k: wgb[:, k, dc * 128:(dc + 1) * 128],
                                        lambda k: hT[:, k, c * 512:(c + 1) * 512]),
                                reads=[b_w3] + hT_b[4 * c:4 * c + 4], writes=[b_ps2])
                        sg, b_sg = sg_r.next()
                        S.op(ACT, lambda e: e.activation(out=sg[:], in_=ps2[:], func=AF.Sigmoid), reads=[b_ps2], writes=[b_sg])
                        S.op(DVE, lambda e: e.tensor_tensor(out=mbt[:, dc, :], in0=ps1[:], in1=sg[:], op=ALU.mult),
                             reads=[b_ps1, b_sg], writes=[b_mbt])
                    S.dma(SP, "st", lambda e: e.dma_start(out=MBd[:, :, c * 512:(c + 1) * 512], in_=mbt[:]), reads=[b_mbt])
                S.barrier()

        if STOP_AFTER >= 4:
          with ExitStack() as p4:
            def sb4(name, shape, dt):
                return p4.enter_context(nc.sbuf_tensor(name, shape, dt))

            wu = sb4("wu", [128, 8, D], BF16)
            wv = sb4("wv", [128, 8, D], BF16)
            wa = sb4("wa", [128, 8, D], BF16)
            wga = sb4("wga", [128, 8, D], BF16)
            b_w4 = Buf()
            load_w_cast(wu[:], win_v[:, :, 0:1024], b_w4)
            load_w_cast(wv[:], win_v[:, :, 1024:2048], b_w4)
            load_w_cast(wa[:], w_ba.rearrange("(k p) n -> p k n", p=128), b_w4)
            load_w_cast(wga[:], win_v[:, :, 5120:6144], b_w4)
            g_in = sb4("g_in4", [128, D], F32)
            bb_in = sb4("bb_in4", [128, D], F32)
            g_gm = sb4("g_gm", [128, D], F32)
            bb_gm = sb4("bb_gm", [128, D], F32)
            bsb = sb4("bsb", [128, 4, 128], F32)
            b_par4 = Buf()
            bcast_load(g_in[:], ln_in_g, b_par4)
            bcast_load(bb_in[:], ln_in_b, b_par4)
            bcast_load(g_gm[:], gmlp_g, b_par4)
            bcast_load(bb_gm[:], gmlp_b, b_par4)
            bcast_load(bsb[:].rearrange("p g t -> p (g t)"), b_sp, b_par4)
            wsn = sb4("wsn", [128, 4, 128], F32)
            wsT = sb4("wsT", [128, 4, 128], BF16)
            b_ws = Buf()
            S.dma(SP, "const", lambda e: e.dma_start(out=wsn[:], in_=w_sp.rearrange("g t s -> t g s")), writes=[b_ws])
            ps, b_ps = psA.next()
            S.group(PE, [(lambda e, g=g: e.transpose(out=ps[:, g * 128:(g + 1) * 128], in_=wsn[:, g, :], identity=identf[:]))
                         for g in range(4)], reads=[b_ws, b_const], writes=[b_ps])
            S.op(DVE, lambda e: e.tensor_tensor(out=wsT[:], in0=ps[:].rearrange("p (g t) -> p g t", g=4),
                                                in1=tri[:].unsqueeze(1).to_broadcast([128, 4, 128]), op=ALU.mult),
                 reads=[b_ps, b_const], writes=[b_ws])

            xr = Ring(sb4, "x4", [128, D], F32, 2)
            hbr = Ring(sb4, "hb4", [128, D], BF16, 2)
            sc4 = ln_scratch(sb4, "l4")
            hTg_r = Ring(sb4, "hTg", [128, 8, 512], BF16, 2)
            uT_r = Ring(sb4, "uT", [128, 8, 512], BF16, 1)
            gv_r = Ring(sb4, "gv", [128, D], F32, 2)
            vn_r = Ring(sb4, "vn", [128, 4, D], BF16, 2)
            yat_r = Ring(sb4, "yat", [128, 8, 512], BF16, 1)
            mbg_r = Ring(sb4, "mbg", [128, 8, 512], BF16, 2)
            mgt_r = Ring(sb4, "mgt", [128, 8, 512], BF16, 2)
            sg_r = Ring(sb4, "sg4", [128, 512], F32, 2)
            t4_r = Ring(sb4, "t4", [128, 512], F32, 2)
            def prep_group(c, hTg, b_hTg):
                S.dma(POOL, "ld4h", lambda e: e.dma_start(out=hTg[:], in_=HTd[:, :, c * 512:(c + 1) * 512]), writes=[b_hTg])

            def v_tile(c, i, hTg, b_hTg, vn, b_vn):
                gv, b_gv = gv_r.next()
                for half in range(2):
                    ps, b_ps = psA.next()
                    S.group(PE, mm8(ps[:], lambda k: hTg[:, k, i * 128:(i + 1) * 128],
                                    lambda k: wv[:, k, half * 512:(half + 1) * 512]),
                            reads=[b_w4, b_hTg], writes=[b_ps])
                    S.op(ACT, lambda e: e.activation(out=gv[:, half * 512:(half + 1) * 512], in_=ps[:], func=AF.Gelu),
                         reads=[b_ps], writes=[b_gv])
                layer_norm(sc4, gv[:], b_gv, g_gm, bb_gm, b_par4, vn[:, i, :], b_vn)

            def u_stage(c, hTg, b_hTg):
                uT, b_uT = uT_r.next()
                for dc in range(8):
                    ps, b_ps = psA.next()
                    S.group(PE, mm8(ps[:], lambda k: wu[:, k, dc * 128:(dc + 1) * 128], lambda k: hTg[:, k, :]),
                            reads=[b_w4, b_hTg], writes=[b_ps])
                    S.op(ACT, lambda e: e.activation(out=uT[:, dc, :], in_=ps[:], func=AF.Gelu), reads=[b_ps], writes=[b_uT])
                return uT, b_uT

            def vs_stage(c, vn, b_vn, uT, b_uT):
                yat, b_yat = yat_r.next()
                for dc in range(8):
                    g = dc // 2
                    ps, b_ps = psA.next()
                    S.group(PE, [(lambda e, i=i: e.matmul(ps[:, i * 128:(i + 1) * 128], lhsT=vn[:, i, dc * 128:(dc + 1) * 128],
                                                          rhs=wsT[:, g, :], start=True, stop=True)) for i in range(4)],
                            reads=[b_vn, b_ws], writes=[b_ps])
                    t4, b_t4 = t4_r.next()
                    S.op(DVE, lambda e: e.tensor_tensor(out=t4[:].rearrange("p (a b) -> p a b", a=4),
                                                        in0=ps[:].rearrange("p (a b) -> p a b", a=4),
                                                        in1=bsb[:, g:g + 1, :].to_broadcast([128, 4, 128]), op=ALU.add),
                         reads=[b_ps, b_par4], writes=[b_t4])
                    S.op(POOL, lambda e: e.tensor_tensor(out=yat[:, dc, :], in0=t4[:], in1=uT[:, dc, :], op=ALU.mult),
                         reads=[b_t4, b_uT], writes=[b_yat])
                return yat, b_yat

            def zg_stage(c, dcs, yat, b_yat, hTg, b_hTg, mbg, b_mbg, mgt, b_mgt):
                for dc in dcs:
                    ps1, b_ps1 = psA.next()
                    S.group(PE, mm8(ps1[:], lambda k: wa[:, k, dc * 128:(dc + 1) * 128], lambda k: yat[:, k, :]),
                            reads=[b_w4, b_yat], writes=[b_ps1])
                    ps2, b_ps2 = psA.next()
                    S.group(PE, mm8(ps2[:], lambda k: wga[:, k, dc * 128:(dc + 1) * 128], lambda k: hTg[:, k, :]),
                            reads=[b_w4, b_hTg], writes=[b_ps2])
                    sg, b_sg = sg_r.next()
                    S.op(ACT, lambda e: e.activation(out=sg[:], in_=ps2[:], func=AF.Sigmoid), reads=[b_ps2], writes=[b_sg])
                    t4, b_t4 = t4_r.next()
                    S.op(DVE, lambda e: e.tensor_tensor(out=t4[:], in0=ps1[:], in1=sg[:], op=ALU.mult),
                         reads=[b_ps1, b_sg], writes=[b_t4])
                    S.op(POOL, lambda e: e.tensor_tensor(out=mgt[:, dc, :], in0=t4[:], in1=mbg[:, dc, :], op=ALU.add),
                         reads=[b_t4, b_mbg], writes=[b_mgt])

            cur = hTg_r.next()
            prep_group(0, *cur)
            vcur = vn_r.next()
            for i in range(4):
                v_tile(0, i, *cur, *vcur)
            for c in range(8):
                hTg, b_hTg = cur
                vn, b_vn = vcur
                mbg, b_mbg = mbg_r.next()
                S.dma(SP, "ld4", lambda e: e.dma_start(out=mbg[:], in_=MBd[:, :, c * 512:(c + 1) * 512]), writes=[b_mbg])
                nxt = hTg_r.next() if c + 1 < 8 else None
                vnxt = vn_r.next() if c + 1 < 8 else None
                if nxt is not None:
                    prep_group(c + 1, *nxt)
                uT, b_uT = u_stage(c, hTg, b_hTg)
                yat, b_yat = vs_stage(c, vn, b_vn, uT, b_uT)
                mgt, b_mgt = mgt_r.next()
                for i in range(4):
                    zg_stage(c, range(2 * i, 2 * i + 2), yat, b_yat, hTg, b_hTg, mbg, b_mbg, mgt, b_mgt)
                    if nxt is not None:
                        v_tile(c + 1, i, *nxt, *vnxt)
                S.dma(SP, "st", lambda e: e.dma_start(out=MGd[:, :, c * 512:(c + 1) * 512], in_=mgt[:]), reads=[b_mgt])
                cur, vcur = nxt, vnxt
            S.barrier()

        if STOP_AFTER >= 5:
          with ExitStack() as p5:
            def sb5(name, shape, dt):
                return p5.enter_context(nc.sbuf_tensor(name, shape, dt))

            wo = sb5("wo", [128, 8, D], BF16)
            b_w5 = Buf()
            load_w_cast(wo[:], w_o.rearrange("(k p) n -> p k n", p=128), b_w5)
            wr = sb5("wr", [128, 8, NE], F32)
            S.dma(SP, "const", lambda e: e.dma_start(out=wr[:], in_=w_rt.rearrange("(k p) n -> p k n", p=128)), writes=[b_w5])
            g_in = sb5("g_in5", [128, D], F32)
            bb_in = sb5("bb_in5", [128, D], F32)
            g_mx = sb5("g_mx", [128, D], F32)
            bb_mx = sb5("bb_mx", [128, D], F32)
            brt = sb5("brt", [128, NE], F32)
            b_par5 = Buf()
            bcast_load(g_in[:], ln_in_g, b_par5)
            bcast_load(bb_in[:], ln_in_b, b_par5)
            bcast_load(g_mx[:], ln_mix_g, b_par5)
            bcast_load(bb_mx[:], ln_mix_b, b_par5)
            bcast_load(brt[:], b_rt, b_par5)
            ebase = sb5("ebase", [128, NE], F32)
            ebi = sb5("ebi", [128, NE], I32)
            S.op(POOL, lambda e: e.iota(ebi[:], pattern=[[CAP, NE]], base=0, channel_multiplier=0), writes=[b_par5])
            S.op(DVE, lambda e: e.tensor_copy(out=ebase[:], in_=ebi[:]), reads=[b_par5], writes=[b_par5])
            carry = sb5("carry", [128, NE], F32)
            b_carry = Buf()
            S.op(DVE, lambda e: e.memset(carry[:], 0.0), writes=[b_carry])

            B5 = 2
            D5 = 3
            hres_r = Ring(sb5, "hres", [128, D], F32, 4)
            sc5 = ln_scratch(sb5, "l5", 8)
            mgl_r = Ring(sb5, "mgl", [128, 8, 512], BF16, 2)
            t2_r = Ring(sb5, "t2", [128, D], F32, 6)
            h2_r = Ring(sb5, "h2", [128, D], F32, 4)
            h2b_r = Ring(sb5, "h2b", [128, D], BF16, 8)
            h2T_r = Ring(sb5, "h2T", [128, 8, 128], F32, 3)
            sm = {n: Ring(sb5, "sm_" + n, shp, dt, 10) for n, shp, dt in [
                ("lg", [128, NE], F32), ("m8", [128, 8], F32), ("nm", [128, 1], F32), ("ew", [128, 4], F32),
                ("ss", [128, 1], F32), ("selb", [128, NE], BF16), ("pos", [128, NE], F32), ("oh", [128, NE], F32),
                ("junk", [128, NE], F32), ("idxf", [128, 4], F32), ("pp", [128, 2 * NE], F32)]}
            mgl_cur = [None, None]

            def s0(cx):
                t = cx["t"]
                c, i = divmod(t, 4)
                if i == 0:
                    mgl, b_mgl = mgl_r.next()
                    S.dma(SP, "ld5", lambda e: e.dma_start(out=mgl[:], in_=MGd[:, :, c * 512:(c + 1) * 512]), writes=[b_mgl])
                    mgl_cur[0], mgl_cur[1] = mgl, b_mgl
                cx["mgl"], cx["b_mgl"] = mgl_cur[0], mgl_cur[1]
                hres, b_hres = hres_r.next()
                S.dma(SP, "x", lambda e: e.dma_start(out=hres[:], in_=Hd[t * 128:(t + 1) * 128, :]), writes=[b_hres])
                cx["hres"], cx["b_hres"] = hres, b_hres

            def s3a(cx):
                i = cx["t"] % 4
                mgl, b_mgl = cx["mgl"], cx["b_mgl"]
                cx["ps3"] = []
                for half in range(2):
                    ps, b_ps = psA.next()
                    S.group(PE, mm8(ps[:], lambda k: mgl[:, k, i * 128:(i + 1) * 128],
                                    lambda k: wo[:, k, half * 512:(half + 1) * 512]), reads=[b_w5, b_mgl], writes=[b_ps])
                    cx["ps3"].append((ps, b_ps))

            def s3b(cx):
                hres, b_hres = cx["hres"], cx["b_hres"]
                t2, b_t2 = t2_r.next()
                for half in range(2):
                    ps, b_ps = cx["ps3"][half]
                    S.op(DVE, lambda e: e.scalar_tensor_tensor(out=t2[:, half * 512:(half + 1) * 512],
                                                               in0=hres[:, half * 512:(half + 1) * 512], scalar=ALPHA,
                                                               in1=ps[:], op0=ALU.mult, op1=ALU.add),
                         reads=[b_hres, b_ps], writes=[b_t2])
                cx["t2"], cx["b_t2"] = t2, b_t2
                cx["st2"] = ln_stats_a(sc5, t2[:], b_t2)

            def s4b(cx):
                ln_stats_b(cx["st2"])

            def s5(cx):
                t = cx["t"]
                ln_stats_c(cx["st2"])
                h2, b_h2 = h2_r.next()
                ln_apply(sc5, cx["t2"][:], cx["b_t2"], cx["st2"], g_mx, bb_mx, b_par5, h2[:], b_h2)
                cx["h2"], cx["b_h2"] = h2, b_h2

            def s5b(cx):
                t = cx["t"]
                h2, b_h2 = cx["h2"], cx["b_h2"]
                S.dma(POOL, "st", lambda e: e.dma_start(out=H2d[t * 128:(t + 1) * 128, :], in_=h2[:]), reads=[b_h2])
                h2b, b_h2b = h2b_r.next()
                S.op(ACT, lambda e: e.copy(out=h2b[:], in_=h2[:]), reads=[b_h2], writes=[b_h2b])
                cx["h2b"], cx["b_h2b"] = h2b, b_h2b
                h2T, b_h2T = h2T_r.next()
                for hf in range(2):
                    ps, b_ps = psA.next()
                    S.group(PE, [(lambda e, k=k: e.transpose(out=ps[:, k * 128:(k + 1) * 128],
                                                             in_=h2[:, (4 * hf + k) * 128:(4 * hf + k + 1) * 128],
                                                             identity=identf[:])) for k in range(4)],
                            reads=[b_h2, b_const], writes=[b_ps])
                    S.op(ACT, lambda e: e.copy(out=h2T[:, 4 * hf:4 * hf + 4, :], in_=ps[:].rearrange("p (a b) -> p a b", a=4)),
                         reads=[b_ps], writes=[b_h2T])
                cx["h2T"], cx["b_h2T"] = h2T, b_h2T

            def s6b(cx):
                h2T, b_h2T = cx["h2T"], cx["b_h2T"]
                r8 = cx["t"] % 8
                ps, b_ps = psR_t[:, r8 * NE:(r8 + 1) * NE], psR_reg[r8]
                S.group(PE, mm8(ps, lambda k: h2T[:, k, :], lambda k: wr[:, k, :]), reads=[b_h2T, b_w5], writes=[b_ps])
                cx["psr"] = (ps, b_ps)

            def s6c(cx):
                ps, b_ps = cx["psr"]
                lg, b_lg = sm["lg"].next()
                S.op(DVE, lambda e: e.tensor_tensor(out=lg[:], in0=ps, in1=brt[:], op=ALU.add),
                     reads=[b_ps, b_par5], writes=[b_lg])
                m8t, b_m8t = sm["m8"].next()
                S.op(DVE, lambda e: e.max(out=m8t[:], in_=lg[:]), reads=[b_lg], writes=[b_m8t])
                nm, b_nm = sm["nm"].next()
                S.op(DVE, lambda e: e.tensor_scalar(out=nm[:], in0=m8t[:, 0:1], scalar1=-1.0, scalar2=None, op0=ALU.mult),
                     reads=[b_m8t], writes=[b_nm])
                selb, b_selb = sm["selb"].next()
                S.op(DVE, lambda e: e.tensor_scalar(out=selb[:], in0=lg[:], scalar1=m8t[:, 3:4], scalar2=None, op0=ALU.is_ge),
                     reads=[b_lg, b_m8t], writes=[b_selb])
                cx.update(lg=lg, b_lg=b_lg, m8t=m8t, b_m8t=b_m8t, nm=nm, b_nm=b_nm, selb=selb, b_selb=b_selb)

            def s7a(cx):
                m8t, b_m8t, nm, b_nm, selb, b_selb = cx["m8t"], cx["b_m8t"], cx["nm"], cx["b_nm"], cx["selb"], cx["b_selb"]
                ew, b_ew = sm["ew"].next()
                ss, b_ss = sm["ss"].next()
                S.op(ACT, lambda e: e.activation(out=ew[:], in_=m8t[:, 0:4], func=AF.Exp, bias=nm[:, 0:1], scale=1.0,
                                                 accum_out=ss[:]), reads=[b_m8t, b_nm], writes=[b_ew, b_ss])
                r8 = cx["t"] % 8
                ps, b_ps = psO_t[:, r8 * 2 * NE:(r8 + 1) * 2 * NE], psO_reg[r8]
                S.group(PE, [lambda e: e.matmul(ps[:, 0:NE], lhsT=ustr[:], rhs=selb[:], start=True, stop=True),
                             lambda e: e.matmul(ps[:, NE:2 * NE], lhsT=onesb[:], rhs=selb[:], start=True, stop=True)],
                        reads=[b_selb, b_const], writes=[b_ps])
                cx.update(ew=ew, b_ew=b_ew, ss=ss, b_ss=b_ss, psp=(ps, b_ps))

            def s7b(cx):
                t = cx["t"]
                ew, b_ew, ss, b_ss = cx["ew"], cx["b_ew"], cx["ss"], cx["b_ss"]
                ps, b_ps = cx["psp"]
                pp, b_pp = sm["pp"].next()
                S.op(DVE, lambda e: e.tensor_copy(out=pp[:], in_=ps), reads=[b_ps], writes=[b_pp])
                S.op(DVE, lambda e: e.reciprocal(out=ss[:], in_=ss[:]), reads=[b_ss], writes=[b_ss])
                S.op(DVE, lambda e: e.tensor_scalar(out=gate_all[:, t, :], in0=ew[:], scalar1=ss[:, 0:1], scalar2=None,
                                                    op0=ALU.mult), reads=[b_ew, b_ss], writes=[b_gate])
                cx.update(pp=pp, b_pp=b_pp)

            def s8(cx):
                t = cx["t"]
                lg, b_lg, m8t, b_m8t, pp, b_pp = cx["lg"], cx["b_lg"], cx["m8t"], cx["b_m8t"], cx["pp"], cx["b_pp"]
                h2b, b_h2b = cx["h2b"], cx["b_h2b"]
                pos, b_pos = sm["pos"].next()
                S.op(DVE, lambda e: e.tensor_tensor(out=pos[:], in0=pp[:, 0:NE], in1=carry[:], op=ALU.add),
                     reads=[b_pp, b_carry], writes=[b_pos])
                S.op(DVE, lambda e: e.tensor_tensor(out=carry[:], in0=pp[:, NE:2 * NE], in1=carry[:], op=ALU.add),
                     reads=[b_pp, b_carry], writes=[b_carry])
                S.op(DVE, lambda e: e.tensor_tensor(out=pos[:], in0=pos[:], in1=ebase[:], op=ALU.add),
                     reads=[b_pos, b_par5], writes=[b_pos])
                idxf, b_idxf = sm["idxf"].next()
                for k in range(4):
                    oh, b_oh = sm["oh"].next()
                    S.op(DVE, lambda e, k=k: e.tensor_scalar(out=oh[:], in0=lg[:], scalar1=m8t[:, k:k + 1], scalar2=None,
                                                             op0=ALU.is_equal), reads=[b_lg, b_m8t], writes=[b_oh])
                    junk, b_junk = sm["junk"].next()
                    S.op(DVE, lambda e, k=k: e.scalar_tensor_tensor(out=junk[:], in0=oh[:], scalar=1.0, in1=pos[:],
                                                                    op0=ALU.mult, op1=ALU.mult, accum_out=idxf[:, k:k + 1]),
                         reads=[b_oh, b_pos], writes=[b_junk, b_idxf])
                S.op(DVE, lambda e: e.tensor_copy(out=idx_all[:, t, :], in_=idxf[:]), reads=[b_idxf], writes=[b_idx])
                for k in range(4 if not os.environ.get("MK_NOSC") else 0):
                    S.dma(POOL, "sc", lambda e, k=k: e.indirect_dma_start(
                        out=Xg[:, :], out_offset=bass.IndirectOffsetOnAxis(ap=idx_all[:, t, k:k + 1], axis=0),
                        in_=h2b[:], in_offset=None), reads=[b_h2b, b_idx])

            _rb, _ob = Buf(), Buf()
            psR_reg = [_rb] * 8
            psO_reg = [_ob] * 8
            assert 2 * B5 <= len(psA.items)
            stages5 = [(s0,), (s3a, s3b), (s4b,), (s5,), (s5b,), (s6b,), (s6c,), (s7a,), (s7b,), (s8,)]
            nb5 = NT // B5
            batches5 = [[{"t": t} for t in range(b * B5, (b + 1) * B5)] for b in range(nb5)]
            for step in range(len(stages5) + D5 * (nb5 - 1)):
                for b in range(nb5):
                    k = step - D5 * b
                    if 0 <= k < len(stages5):
                        for fn in stages5[k]:
                            for cx in batches5[b]:
                                fn(cx)
            if DEBUG:
                S.dma(SP, "st", lambda e: e.dma_start(out=IDXd[:, :], in_=idx_all[:].rearrange("p a b -> p (a b)")), reads=[b_idx])
                S.dma(SP, "st", lambda e: e.dma_start(out=GATd[:, :], in_=gate_all[:].rearrange("p a b -> p (a b)")), reads=[b_gate])
            S.barrier()

        if STOP_AFTER >= 6:
          with ExitStack() as p6:
            def sb6(name, shape, dt):
                return p6.enter_context(nc.sbuf_tensor(name, shape, dt))

            bun = sb6("bun", [NE, 2 * D], F32)
            bu_all = sb6("bu_all", [128, 16, NE], F32)
            b_bu = Buf()
            S.dma(SP, "const", lambda e: e.dma_start(out=bun[:], in_=b_up[:, :]), writes=[b_bu])
            for q4 in range(4):
                ps, b_ps = psA.next()
                S.group(PE, [(lambda e, i=i: e.transpose(out=ps[:, i * NE:(i + 1) * NE],
                                                         in_=bun[:, (4 * q4 + i) * 128:(4 * q4 + i + 1) * 128],
                                                         identity=identf[0:NE, 0:NE])) for i in range(4)],
                        reads=[b_bu, b_const], writes=[b_ps])
                S.op(DVE, lambda e: e.tensor_copy(out=bu_all[:, 4 * q4:4 * q4 + 4, :],
                                                  in_=ps[:, 0:4 * NE].rearrange("p (a b) -> p a b", a=4)),
                     reads=[b_ps], writes=[b_bu])
            wu_r = Ring(sb6, "wup", [128, 8, 2 * D], BF16, 2)
            wd_r = Ring(sb6, "wdn", [128, 8, D], BF16, 2)
            bd_r = Ring(sb6, "bdn", [128, D], F32, 2)
            xs_r = Ring(sb6, "xs", [128, D], BF16, 6)
            XT_r = Ring(sb6, "XT", [128, 8, CAP], BF16, 2)
            aT_r = Ring(sb6, "aT", [128, 8, CAP], BF16, 1)
            HW = CAP // 2
            gb_r = Ring(sb6, "gb", [128, HW], F32, 2)
            sg_r = Ring(sb6, "sg6", [128, HW], F32, 2)
            ub_r = Ring(sb6, "ub", [128, HW], F32, 2)
            ys_r = Ring(sb6, "ys", [128, D], F32, 2)

            def load_expert(e_):
                wu_t, b_wu = wu_r.next()
                wd_t, b_wd = wd_r.next()
                bd_t, b_bd = bd_r.next()
                load_w_cast(wu_t[:], w_up[e_].rearrange("(k p) n -> p k n", p=128), b_wu, "we")
                load_w_cast(wd_t[:], w_dn[e_].rearrange("(k p) n -> p k n", p=128), b_wd, "we")
                S.dma(SP, "bd", lambda e: e.dma_start(out=bd_t[:], in_=b_dn[e_].partition_broadcast(128)), writes=[b_bd])
                return (wu_t, b_wu, wd_t, b_wd, bd_t, b_bd)

            def load_x_dma(e_):
                tiles = []
                for s_ in range(NSL):
                    xs, b_xs = xs_r.next()
                    r0 = e_ * CAP + s_ * 128
                    S.dma(SP, "xs", lambda e: e.dma_start(out=xs[:], in_=Xg[r0:r0 + 128, :]), writes=[b_xs])
                    tiles.append((xs, b_xs))
                return tiles

            def load_x_tile(tiles, s_, XT, b_XT):
                xs, b_xs = tiles[s_]
                transpose_bf(xs, b_xs, XT[:, :, s_ * 128:(s_ + 1) * 128], b_XT, DVE if s_ % 2 else ACT)

            def load_x(e_):
                XT, b_XT = XT_r.next()
                tiles = load_x_dma(e_)
                for s_ in range(NSL):
                    load_x_tile(tiles, s_, XT, b_XT)
                return XT, b_XT

            def up_proj(ex, wts, XT, b_XT):
                wu_t, b_wu = wts[0], wts[1]
                aT, b_aT = aT_r.next()
                for cc in range(8):
                    for hf in range(2):
                        sl = slice(hf * HW, (hf + 1) * HW)
                        psg_, b_psg = psA.next()
                        S.group(PE, mm8(psg_[:, 0:HW], lambda k: wu_t[:, k, cc * 128:(cc + 1) * 128], lambda k: XT[:, k, sl]),
                                reads=[b_wu, b_XT], writes=[b_psg])
                        psu_, b_psu = psA.next()
                        S.group(PE, mm8(psu_[:, 0:HW], lambda k: wu_t[:, k, D + cc * 128:D + (cc + 1) * 128],
                                        lambda k: XT[:, k, sl]), reads=[b_wu, b_XT], writes=[b_psu])
                        gb, b_gb = gb_r.next()
                        S.op(ACT, lambda e: e.activation(out=gb[:], in_=psg_[:, 0:HW], func=AF.Identity,
                                                         bias=bu_all[:, cc, ex:ex + 1], scale=1.0),
                             reads=[b_psg, b_bu], writes=[b_gb])
                        ub, b_ub = ub_r.next()
                        S.op(ACT, lambda e: e.activation(out=ub[:], in_=psu_[:, 0:HW], func=AF.Identity,
                                                         bias=bu_all[:, 8 + cc, ex:ex + 1], scale=1.0),
                             reads=[b_psu, b_bu], writes=[b_ub])
                        S.op(DVE, lambda e: e.tensor_scalar(out=gb[:], in0=gb[:], scalar1=7.0, scalar2=None, op0=ALU.min),
                             reads=[b_gb], writes=[b_gb])
                        sg, b_sg = sg_r.next()
                        S.op(ACT, lambda e: e.activation(out=sg[:], in_=gb[:], func=AF.Sigmoid, scale=1.702),
                             reads=[b_gb], writes=[b_sg])
                        S.op(POOL, lambda e: e.tensor_tensor(out=sg[:], in0=gb[:], in1=sg[:], op=ALU.mult),
                             reads=[b_gb, b_sg], writes=[b_sg])
                        S.op(DVE, lambda e: e.tensor_scalar(out=ub[:], in0=ub[:], scalar1=7.0, scalar2=-7.0, op0=ALU.min,
                                                            op1=ALU.max), reads=[b_ub], writes=[b_ub])
                        S.op(DVE, lambda e: e.scalar_tensor_tensor(out=aT[:, cc, sl], in0=ub[:], scalar=1.0, in1=sg[:],
                                                                   op0=ALU.add, op1=ALU.mult),
                             reads=[b_ub, b_sg], writes=[b_aT])
                return aT, b_aT

            def down_proj(ex, wts, aT, b_aT, xnext=None):
                wd_t, b_wd, bd_t, b_bd = wts[2], wts[3], wts[4], wts[5]
                for s_ in range(NSL):
                    if xnext is not None:
                        load_x_tile(xnext[0], s_, xnext[1], xnext[2])
                    ys, b_ys = ys_r.next()
                    for hf in range(2):
                        ps, b_ps = psA.next()
                        S.group(PE, mm8(ps[:], lambda k: aT[:, k, s_ * 128:(s_ + 1) * 128],
                                        lambda k: wd_t[:, k, hf * 512:(hf + 1) * 512]), reads=[b_aT, b_wd], writes=[b_ps])
                        S.op(DVE, lambda e: e.tensor_tensor(out=ys[:, hf * 512:(hf + 1) * 512], in0=ps[:],
                                                            in1=bd_t[:, hf * 512:(hf + 1) * 512], op=ALU.add),
                             reads=[b_ps, b_bd], writes=[b_ys])
                    r0 = ex * CAP + s_ * 128
                    S.dma(SP, "st", lambda e: e.dma_start(out=Yg[r0:r0 + 128, :], in_=ys[:]), reads=[b_ys])

            wts = load_expert(0)
            XTc = load_x(0)
            for ex in range(NE):
                wts_n = load_expert(ex + 1) if ex + 1 < NE else None
                aTc = up_proj(ex, wts, *XTc)
                XTn, xnext = None, None
                if ex + 1 < NE:
                    XTn = XT_r.next()
                    xnext = (load_x_dma(ex + 1), XTn[0], XTn[1])
                down_proj(ex, wts, *aTc, xnext=xnext)
                wts, XTc = wts_n, XTn
            S.barrier()

        if STOP_AFTER >= 7:
          with ExitStack() as p7:
            def sb7(name, shape, dt):
                return p7.enter_context(nc.sbuf_tensor(name, shape, dt))

            g_ff = sb7("g_ff", [128, D], F32)
            bb_ff = sb7("bb_ff", [128, D], F32)
            b_par7 = Buf()
            bcast_load(g_ff[:], ln_ffn_g, b_par7)
            bcast_load(bb_ff[:], ln_ffn_b, b_par7)
            B7 = 4
            yk_r = Ring(sb7, "yk", [128, 4, D], F32, B7)
            h2l_r = Ring(sb7, "h2l", [128, D], F32, B7)
            acc_r = Ring(sb7, "acc", [128, D], F32, B7)
            o_r = Ring(sb7, "o7", [128, D], F32, B7)
            sc7 = ln_scratch(sb7, "l7", B7 + 1)

            def c0(cx):
                t = cx["t"]
                yk, b_yk = yk_r.next()
                for k in range(4):
                    S.dma(POOL, "ga", lambda e, k=k: e.indirect_dma_start(
                        out=yk[:, k, :], out_offset=None, in_=Yg[:, :],
                        in_offset=bass.IndirectOffsetOnAxis(ap=idx_all[:, t, k:k + 1], axis=0)), reads=[b_idx], writes=[b_yk])
                h2l, b_h2l = h2l_r.next()
                S.dma(POOL, "ld7", lambda e: e.dma_start(out=h2l[:], in_=H2d[t * 128:(t + 1) * 128, :]), writes=[b_h2l])
                cx.update(yk=yk, b_yk=b_yk, h2l=h2l, b_h2l=b_h2l)

            def c1(cx):
                t = cx["t"]
                yk, b_yk, h2l, b_h2l = cx["yk"], cx["b_yk"], cx["h2l"], cx["b_h2l"]
                acc, b_acc = acc_r.next()
                S.op(ACT, lambda e: e.mul(out=acc[:], in_=h2l[:], mul=ALPHA), reads=[b_h2l], writes=[b_acc])
                for k in range(4 if not os.environ.get('MK_NOSTT') else 1):
                    S.op(DVE, lambda e, k=k: e.scalar_tensor_tensor(out=acc[:], in0=yk[:, k, :], scalar=gate_all[:, t, k:k + 1],
                                                                    in1=acc[:], op0=ALU.mult, op1=ALU.add),
                         reads=[b_yk, b_gate, b_acc], writes=[b_acc])
                cx.update(acc=acc, b_acc=b_acc)

            def c2(cx):
                cx["st"] = ln_stats(sc7, cx["acc"][:], cx["b_acc"])

            def c3(cx):
                t = cx["t"]
                ot, b_ot = o_r.next()
                ln_apply(sc7, cx["acc"][:], cx["b_acc"], cx["st"], g_ff, bb_ff, b_par7, ot[:], b_ot)
                S.dma(SP, "out", lambda e: e.dma_start(out=out_d[t * 128:(t + 1) * 128, :], in_=ot[:]), reads=[b_ot])

            for b0 in range(0, NT, B7):
                cxs = [{"t": t} for t in range(b0, b0 + B7)]
                for st_fn in [c0, c1, c2, c3]:
                    for cx in cxs:
                        st_fn(cx)
            S.barrier()

        S.barrier()
        waited = S.waited
    return nc, waited


_INPUT_ORDER = ["x", "ln_in_g", "ln_in_b", "w_in", "gmlp_ln_g", "gmlp_ln_b", "w_spatial", "b_spatial", "w_branch_a",
                "w_branch_b", "w_out", "ln_mix_g", "ln_mix_b", "w_router", "b_router", "w_up", "b_up", "w_down",
                "b_down", "ln_ffn_g", "ln_ffn_b"]


def _prep_inputs(inputs):
    a = {k: np.ascontiguousarray(np.asarray(v), dtype=np.float32) for k, v in inputs.items()}
    shared = {
        "ln_in_g": a["ln_in_g"].reshape(D), "ln_in_b": a["ln_in_b"].reshape(D),
        "w_in": a["w_in"].reshape(D, 7 * D),
        "gmlp_ln_g": a["gmlp_ln_g"].reshape(D), "gmlp_ln_b": a["gmlp_ln_b"].reshape(D),
        "w_spatial": a["w_spatial"].reshape(4, 128, 128), "b_spatial": a["b_spatial"].reshape(512),
        "w_branch_a": a["w_branch_a"].reshape(D, D), "w_branch_b": a["w_branch_b"].reshape(D, D),
        "w_out": a["w_out"].reshape(D, D),
        "ln_mix_g": a["ln_mix_g"].reshape(D), "ln_mix_b": a["ln_mix_b"].reshape(D),
        "w_router": a["w_router"].reshape(D, NE), "b_router": a["b_router"].reshape(NE),
        "w_up": a["w_up"].reshape(NE, D, 2 * D), "b_up": a["b_up"].reshape(NE, 2 * D),
        "w_down": a["w_down"].reshape(NE, D, D), "b_down": a["b_down"].reshape(NE, D),
        "ln_ffn_g": a["ln_ffn_g"].reshape(D), "ln_ffn_b": a["ln_ffn_b"].reshape(D),
    }
    return a["x"], shared


def kernel(**inputs):
    x, shared = _prep_inputs(inputs)
    n = x.shape[0]
    nc = build_program()
    in_maps = []
    for b in range(n):
        m = dict(shared)
        m["x"] = np.ascontiguousarray(x[b])
        in_maps.append(m)
    res = run_bass_kernel_spmd(nc, in_maps, core_ids=list(range(n)))
    out = np.stack([np.asarray(r["out"]) for r in res.results], axis=0).astype(np.float32)
    return out
```

```python
import os
import bisect
import numpy as np
from contextlib import ExitStack
import concourse.bass as bass
import concourse.mybir as mybir
from concourse.bass_utils import run_bass_kernel_spmd

F32 = mybir.dt.float32
BF16 = mybir.dt.bfloat16
I32 = mybir.dt.int32
AF = mybir.ActivationFunctionType
ALU = mybir.AluOpType
AX = mybir.AxisListType

S_TOK = 4096
D = 1024
NT = 32
NE = 32
CAP = 768
NSL = CAP // 128
ALPHA = float(2 ** 0.25)
EPS = 1e-5
DEBUG = bool(int(os.environ.get("MK_DEBUG", "0")))
STOP_AFTER = int(os.environ.get("MK_STOP", "99"))


class Buf:
    __slots__ = ("w", "r")

    def __init__(self):
        self.w = None
        self.r = []


class Eng:
    def __init__(self, nc, eng, name):
        self.eng = eng
        self.name = name
        self.sem = nc.alloc_semaphore(name=name)
        self.count = 0
        self.seen = {}


class Sync:
    def __init__(self, nc, waitsets=None):
        self.nc = nc
        self.waitsets = waitsets
        self.waited = {}
        self.pe = Eng(nc, nc.tensor, "s_pe")
        self.act = Eng(nc, nc.scalar, "s_act")
        self.dve = Eng(nc, nc.vector, "s_dve")
        self.pool = Eng(nc, nc.gpsimd, "s_pool")
        self.sp = Eng(nc, nc.sync, "s_sp")
        self.engs = [self.pe, self.act, self.dve, self.pool, self.sp]
        self.dsems = {}
        self._keep = []
        self.wsets = {k: set(v) for k, v in (waitsets or {}).items()}

    def dsem(self, key):
        k = id(key)
        if k not in self.dsems:
            self.dsems[k] = Eng(self.nc, None, "d_%d" % len(self.dsems))
            self._keep.append(key)
        return self.dsems[k]

    def _wait(self, E, reads, writes):
        deps = {}

        def add(tok):
            if tok is None:
                return
            s, v = tok
            if deps.get(s, 0) < v:
                deps[s] = v

        for t in reads:
            add(t.w)
        for t in writes:
            add(t.w)
            for r in t.r:
                add(r)
        for s, v in deps.items():
            if E.seen.get(s, 0) < v:
                E.eng.wait_ge(s.sem, self._val(s, v))
                E.seen[s] = v

    def _val(self, s, v):
        if s.eng is None:
            return v
        if self.waitsets is None:
            self.waited.setdefault(s.name, set()).add(v)
            return v
        return bisect.bisect_right(self.waitsets[s.name], v)

    def _signals(self, E):
        return self.waitsets is None or E.count in self.wsets.get(E.name, ())

    def _done(self, tok, reads, writes):
        for t in reads:
            t.r.append(tok)
            if len(t.r) > 64:
                best = {}
                for s, v in t.r:
                    if best.get(s, 0) < v:
                        best[s] = v
                t.r = list(best.items())
        for t in writes:
            t.w = tok
            t.r = []

    def op(self, E, fn, reads=(), writes=()):
        self._wait(E, reads, writes)
        inst = fn(E.eng)
        E.count += 1
        if self._signals(E):
            inst.then_inc(E.sem, 1)
        self._done((E, E.count), reads, writes)

    def group(self, E, fns, reads=(), writes=()):
        self._wait(E, reads, writes)
        inst = None
        for fn in fns:
            inst = fn(E.eng)
        E.count += 1
        if self._signals(E):
            inst.then_inc(E.sem, 1)
        self._done((E, E.count), reads, writes)

    def dma(self, Q, dname, fn, reads=(), writes=()):
        key = writes[0] if len(writes) else reads[0]
        Dm = self.dsem(key)
        self._wait(Q, reads, writes)
        inst = fn(Q.eng)
        Dm.count += 16
        inst.then_inc(Dm.sem, 16)
        self._done((Dm, Dm.count), reads, writes)

    def barrier(self):
        allq = self.engs + list(self.dsems.values())
        for E in self.engs:
            for X in allq:
                if X is E or X.count == 0:
                    continue
                if E.seen.get(X, 0) < X.count:
                    E.eng.wait_ge(X.sem, self._val(X, X.count))
                    E.seen[X] = X.count


class Ring:
    def __init__(self, alloc, name, shape, dt, n):
        self.items = [(alloc("%s_%d" % (name, i), shape, dt), Buf()) for i in range(n)]
        self.i = 0

    def next(self):
        it = self.items[self.i % len(self.items)]
        self.i += 1
        return it


def build_program():
    _, waited = _build(None)
    nc, _ = _build({k: sorted(v) for k, v in waited.items()})
    return nc


def _build(waitsets):
    nc = bass.Bass("TRN2", target_bir_lowering=False)

    def din(name, shape, dt=F32):
        return nc.dram_tensor(name, list(shape), dt, kind="ExternalInput").ap()

    x_d = din("x", [S_TOK, D])
    ln_in_g = din("ln_in_g", [D])
    ln_in_b = din("ln_in_b", [D])
    w_in = din("w_in", [D, 7 * D])
    gmlp_g = din("gmlp_ln_g", [D])
    gmlp_b = din("gmlp_ln_b", [D])
    w_sp = din("w_spatial", [4, 128, 128])
    b_sp = din("b_spatial", [512])
    w_ba = din("w_branch_a", [D, D])
    w_bb = din("w_branch_b", [D, D])
    w_o = din("w_out", [D, D])
    ln_mix_g = din("ln_mix_g", [D])
    ln_mix_b = din("ln_mix_b", [D])
    w_rt = din("w_router", [D, NE])
    b_rt = din("b_router", [NE])
    w_up = din("w_up", [NE, D, 2 * D])
    b_up = din("b_up", [NE, 2 * D])
    w_dn = din("w_down", [NE, D, D])
    b_dn = din("b_down", [NE, D])
    ln_ffn_g = din("ln_ffn_g", [D])
    ln_ffn_b = din("ln_ffn_b", [D])
    out_d = nc.dram_tensor("out", [S_TOK, D], F32, kind="ExternalOutput").ap()

    dbgkind = "ExternalOutput" if DEBUG else "Internal"
    YBd = nc.dram_tensor("ybd", [128, 8, S_TOK], BF16, kind=dbgkind).ap()
    MBd = nc.dram_tensor("mbd", [128, 8, S_TOK], BF16, kind="Internal").ap()
    MGd = nc.dram_tensor("mgd", [128, 8, S_TOK], BF16, kind=dbgkind).ap()
    H2d = nc.dram_tensor("h2d", [S_TOK, D], F32, kind=dbgkind).ap()
    Xg = nc.dram_tensor("xg", [NE * CAP, D], BF16, kind="Internal").ap()
    Hd = nc.dram_tensor("hd", [S_TOK, D], F32, kind="Internal").ap()
    HTd = nc.dram_tensor("htd", [128, 8, S_TOK], BF16, kind="Internal").ap()
    Yg = nc.dram_tensor("yg", [NE * CAP, D], F32, kind="Internal").ap()
    if DEBUG:
        IDXd = nc.dram_tensor("idxd", [128, NT * 4], I32, kind="ExternalOutput").ap()
        GATd = nc.dram_tensor("gatd", [128, NT * 4], F32, kind="ExternalOutput").ap()

    win_v = w_in.rearrange("(k p) n -> p k n", p=128)

    with ExitStack() as es:
        def sbg(name, shape, dt):
            return es.enter_context(nc.sbuf_tensor(name, shape, dt))

        def psg(name, shape, dt):
            return es.enter_context(nc.psum_tensor(name, shape, dt))

        S = Sync(nc, waitsets)
        PE, ACT, DVE, POOL, SP = S.pe, S.act, S.dve, S.pool, S.sp

        psT = Ring(psg, "psT", [128, 8, 128], BF16, 2)
        psA = Ring(psg, "psA", [128, 512], F32, 4)
        psO_t = psg("psO", [128, 512], F32)
        psO_b = Buf()
        psR_t = psg("psR", [128, 512], F32)
        psR_b = Buf()

        onesf = sbg("onesf", [128, 128], F32)
        identf = sbg("identf", [128, 128], F32)
        ident = sbg("ident", [128, 128], BF16)
        onesb = sbg("onesb", [128, 128], BF16)
        tri = sbg("tri", [128, 128], BF16)
        ustr = sbg("ustr", [128, 128], BF16)
        idx_all = sbg("idx_all", [128, NT, 4], I32)
        gate_all = sbg("gate_all", [128, NT, 4], F32)
        b_const = Buf()
        b_idx = Buf()
        b_gate = Buf()
        S.op(POOL, lambda e: e.memset(onesf[:], 1.0), writes=[b_const])
        S.op(POOL, lambda e: e.memset(onesb[:], 1.0), writes=[b_const])
        S.op(POOL, lambda e: e.affine_select(out=identf[:], in_=onesf[:], pattern=[[-1, 128]], compare_op=ALU.is_equal,
                                             fill=0.0, base=0, channel_multiplier=1), reads=[b_const], writes=[b_const])
        S.op(POOL, lambda e: e.affine_select(out=ident[:], in_=onesf[:], pattern=[[-1, 128]], compare_op=ALU.is_equal,
                                             fill=0.0, base=0, channel_multiplier=1), reads=[b_const], writes=[b_const])
        S.op(POOL, lambda e: e.affine_select(out=tri[:], in_=onesf[:], pattern=[[1, 128]], compare_op=ALU.is_ge,
                                             fill=0.0, base=0, channel_multiplier=-1), reads=[b_const], writes=[b_const])
        S.op(POOL, lambda e: e.affine_select(out=ustr[:], in_=onesf[:], pattern=[[1, 128]], compare_op=ALU.is_ge,
                                             fill=0.0, base=-1, channel_multiplier=-1), reads=[b_const], writes=[b_const])

        def bcast_load(dst, src1d, buf):
            S.dma(SP, "const", lambda e: e.dma_start(out=dst, in_=src1d.partition_broadcast(128)), writes=[buf])

        def ln_stats_a(sc, src, b_src):
            st, b_st = sc["st"].next()
            mv, b_mv = sc["mv"].next()
            rs, b_rs = sc["rs"].next()
            for i in range(2):
                S.op(DVE, lambda e, i=i: e.bn_stats(out=st[:, i, :], in_=src[:, i * 512:(i + 1) * 512]),
                     reads=[b_src], writes=[b_st])
            S.op(DVE, lambda e: e.bn_aggr(out=mv[:], in_=st[:].rearrange("p a b -> p (a b)")), reads=[b_st], writes=[b_mv])
            S.op(DVE, lambda e: e.tensor_scalar(out=rs[:], in0=mv[:, 1:2], scalar1=EPS, scalar2=None, op0=ALU.add),
                 reads=[b_mv], writes=[b_rs])
            return (mv, b_mv, rs, b_rs)

        def ln_stats_b(stats):
            mv, b_mv, rs, b_rs = stats
            S.op(ACT, lambda e: e.activation(out=rs[:], in_=rs[:], func=AF.Sqrt), reads=[b_rs], writes=[b_rs])

        def ln_stats_c(stats):
            mv, b_mv, rs, b_rs = stats
            S.op(DVE, lambda e: e.reciprocal(out=rs[:], in_=rs[:]), reads=[b_rs], writes=[b_rs])

        def ln_stats(sc, src, b_src):
            stats = ln_stats_a(sc, src, b_src)
            ln_stats_b(stats)
            ln_stats_c(stats)
            return stats

        def ln_apply(sc, src, b_src, stats, g_t, b_t, b_par, dst, b_dst):
            mv, b_mv, rs, b_rs = stats
            tmp, b_tmp = sc["tmp"].next()
            S.op(DVE, lambda e: e.scalar_tensor_tensor(out=tmp[:], in0=src, scalar=mv[:, 0:1], in1=g_t[:],
                                                       op0=ALU.subtract, op1=ALU.mult),
                 reads=[b_src, b_mv, b_par], writes=[b_tmp])
            S.op(DVE, lambda e: e.scalar_tensor_tensor(out=dst, in0=tmp[:], scalar=rs[:, 0:1], in1=b_t[:],
                                                       op0=ALU.mult, op1=ALU.add),
                 reads=[b_tmp, b_rs, b_par], writes=[b_dst])

        def ln_apply_split(sc, src, b_src, stats, g_t, b_t, b_par, dst, b_dst):
            mv, b_mv, rs, b_rs = stats
            tmp, b_tmp = sc["tmp"].next()
            S.op(DVE, lambda e: e.scalar_tensor_tensor(out=tmp[:], in0=src, scalar=mv[:, 0:1], in1=g_t[:],
                                                       op0=ALU.subtract, op1=ALU.mult),
                 reads=[b_src, b_mv, b_par], writes=[b_tmp])
            S.op(ACT, lambda e: e.activation(out=tmp[:], in_=tmp[:], func=AF.Identity, scale=rs[:, 0:1]),
                 reads=[b_tmp, b_rs], writes=[b_tmp])
            S.op(POOL, lambda e: e.tensor_tensor(out=dst, in0=tmp[:], in1=b_t[:], op=ALU.add),
                 reads=[b_tmp, b_par], writes=[b_dst])

        def layer_norm(sc, src, b_src, g_t, b_t, b_par, dst, b_dst):
            stats = ln_stats(sc, src, b_src)
            ln_apply(sc, src, b_src, stats, g_t, b_t, b_par, dst, b_dst)

        def ln_scratch(alloc, pfx, n=2):
            return {"st": Ring(alloc, pfx + "st", [128, 2, 6], F32, n), "mv": Ring(alloc, pfx + "mv", [128, 2], F32, n),
                    "rs": Ring(alloc, pfx + "rs", [128, 1], F32, n), "tmp": Ring(alloc, pfx + "tmp", [128, D], F32, 2)}

        def transpose_bf(src_t, b_src, dst_ap, b_dst, evac):
            pt, b_pt = psT.next()
            S.group(PE, [(lambda e, k=k: e.transpose(out=pt[:, k, :], in_=src_t[:, k * 128:(k + 1) * 128], identity=ident[:]))
                         for k in range(8)], reads=[b_src, b_const], writes=[b_pt])
            if evac is ACT:
                S.op(ACT, lambda e: e.copy(out=dst_ap, in_=pt[:]), reads=[b_pt], writes=[b_dst])
            else:
                S.op(DVE, lambda e: e.tensor_copy(out=dst_ap, in_=pt[:]), reads=[b_pt], writes=[b_dst])

        def mm8(ps_ap, lhs_fn, rhs_fn):
            return [(lambda e, k=k: e.matmul(ps_ap, lhsT=lhs_fn(k), rhs=rhs_fn(k), start=(k == 0), stop=(k == 7)))
                    for k in range(8)]

        def load_w_cast(dst, src, buf, dname="w"):
            S.dma(POOL, dname, lambda e: e.dma_start(out=dst, in_=src), writes=[buf])

        with ExitStack() as sa:
            def sba(name, shape, dt):
                return sa.enter_context(nc.sbuf_tensor(name, shape, dt))

            hT = sba("hT", [128, 8, S_TOK], BF16)
            hT_b = [Buf() for _ in range(NT)]

            with ExitStack() as p1:
                def sb1(name, shape, dt):
                    return p1.enter_context(nc.sbuf_tensor(name, shape, dt))

                g_in = sb1("g_in", [128, D], F32)
                bb_in = sb1("bb_in", [128, D], F32)
                b_par1 = Buf()
                bcast_load(g_in[:], ln_in_g, b_par1)
                bcast_load(bb_in[:], ln_in_b, b_par1)
                xr = Ring(sb1, "x1", [128, D], F32, 2)
                hbr = Ring(sb1, "hb1", [128, D], BF16, 2)
                hfr = Ring(sb1, "hf1", [128, D], F32, 3)
                sc1 = ln_scratch(sb1, "l1", 3)
                for t in range(NT):
                    xt, b_xt = xr.next()
                    S.dma(SP, "x", lambda e: e.dma_start(out=xt[:], in_=x_d[t * 128:(t + 1) * 128, :]), writes=[b_xt])
                    hf, b_hf = hfr.next()
                    layer_norm(sc1, xt[:], b_xt, g_in, bb_in, b_par1, hf[:], b_hf)
                    S.dma(POOL, "st", lambda e: e.dma_start(out=Hd[t * 128:(t + 1) * 128, :], in_=hf[:]), reads=[b_hf])
                    hb, b_hb = hbr.next()
                    S.op(ACT, lambda e: e.copy(out=hb[:], in_=hf[:]), reads=[b_hf], writes=[b_hb])
                    transpose_bf(hb, b_hb, hT[:, :, t * 128:(t + 1) * 128], hT_b[t], DVE if t % 2 else ACT)
                S.barrier()

            if STOP_AFTER >= 2:
              with ExitStack() as p2:
                def sb2(name, shape, dt):
                    return p2.enter_context(nc.sbuf_tensor(name, shape, dt))

                S.dma(SP, "st", lambda e: e.dma_start(out=HTd[:, :, :], in_=hT[:]), reads=hT_b)
                Esel = sb2("Esel", [128, 32, 128], BF16)
                PB = sb2("PB", [128, 32, 32], F32)
                C1 = sb2("C1", [128, 32, 32], F32)
                C2 = sb2("C2", [128, 32, 32], F32)
                zt = sb2("zt", [128, 32, 32], F32)
                b_c2 = Buf()
                S.op(POOL, lambda e: e.memset(Esel[:], 1.0), writes=[b_c2])
                S.op(POOL, lambda e: e.affine_select(out=Esel[:], in_=Esel[:], pattern=[[-1, 32], [0, 128]],
                                                     compare_op=ALU.is_equal, fill=0.0, base=0, channel_multiplier=1),
                     reads=[b_c2], writes=[b_c2])
                S.op(POOL, lambda e: e.memset(zt[:], 0.0), writes=[b_c2])
                blkpat = [[1, 16], [0, 2], [-1, 16], [0, 2]]
                S.op(POOL, lambda e: e.affine_select(out=PB[:], in_=zt[:], pattern=blkpat, compare_op=ALU.is_ge,
                                                     fill=-1e30, base=-1, channel_multiplier=0), reads=[b_c2], writes=[b_c2])
                S.op(POOL, lambda e: e.affine_select(out=C2[:], in_=zt[:], pattern=blkpat, compare_op=ALU.is_equal,
                                                     fill=-30000.0, base=0, channel_multiplier=0), reads=[b_c2], writes=[b_c2])
                S.op(POOL, lambda e: e.affine_select(out=C2[:], in_=C2[:], pattern=[[1, 32], [-1, 32]], compare_op=ALU.is_ge,
                                                     fill=-30000.0, base=0, channel_multiplier=0), reads=[b_c2], writes=[b_c2])
                S.op(POOL, lambda e: e.memset(zt[:], 30000.0), reads=[b_c2], writes=[b_c2])
                S.op(POOL, lambda e: e.affine_select(out=C1[:], in_=zt[:], pattern=blkpat, compare_op=ALU.is_ge,
                                                     fill=0.0, base=-1, channel_multiplier=0), reads=[b_c2], writes=[b_c2])

                wqkv_r = Ring(sb2, "wqkv", [128, 3, 8, 128], BF16, 3)
                NB2 = 2
                qT_l = [sb2("qT%d" % i, [128, S_TOK], BF16) for i in range(NB2)]
                kT_l = [sb2("kT%d" % i, [128, S_TOK], BF16) for i in range(NB2)]
                Vt_l = [sb2("Vt%d" % i, [128, NT, 128], BF16) for i in range(NB2)]
                bT_l = [sb2("biasT%d" % i, [128, S_TOK], BF16) for i in range(NB2)]
                qT_bl = [[Buf() for _ in range(8)] for _ in range(NB2)]
                kT_bl = [[Buf() for _ in range(8)] for _ in range(NB2)]
                V_bl = [[Buf() for _ in range(8)] for _ in range(NB2)]
                bias_bl = [[Buf() for _ in range(8)] for _ in range(NB2)]
                for i in range(NB2):
                    S.op(POOL, lambda e, i=i: e.memset(bT_l[i][:], 0.0), writes=bias_bl[i])
                kmf = sb2("kmf", [128, 16], F32)
                km2 = sb2("km2", [128, 32], BF16)
                b_km = Buf()
                gm = sb2("gm", [128, 32, 32], F32)
                b_gm = Buf()
                m8 = sb2("m8", [128, 32, 8], F32)
                b_m8 = Buf()
                pT_r = Ring(sb2, "pT", [128, 512], BF16, 4)
                rinv_r = Ring(sb2, "rinv", [128, 512], F32, 2)
                osb_r = Ring(sb2, "osb", [128, 512], F32, 2)
                ybt_r = Ring(sb2, "ybt", [128, 512], BF16, 2)
                qscale = float(128 ** -0.5)

                wq_of = {}

                def load_w(hd):
                    wq, b_wq = wqkv_r.next()
                    for i in range(3):
                        c0 = 2048 + i * 1024 + hd * 128
                        load_w_cast(wq[:, i, :, :], win_v[:, :, c0:c0 + 128], b_wq, "w")
                    wq_of[hd] = (wq, b_wq)

                def prep_head(hd):
                    sl = hd % NB2
                    qT, kT, Vt, biasT = qT_l[sl], kT_l[sl], Vt_l[sl], bT_l[sl]
                    qT_b, kT_b, V_b, bias_b = qT_bl[sl], kT_bl[sl], V_bl[sl], bias_bl[sl]
                    wq, b_wq = wq_of[hd]
                    for gi in range(8):
                        ps, b_ps = psA.next()
                        S.group(PE, mm8(ps[:], lambda k: wq[:, 0, k, :], lambda k: hT[:, k, gi * 512:(gi + 1) * 512]),
                                reads=[b_wq] + hT_b[4 * gi:4 * gi + 4], writes=[b_ps])
                        S.op(ACT, lambda e: e.activation(out=qT[:, gi * 512:(gi + 1) * 512], in_=ps[:], func=AF.Copy,
                                                         scale=qscale), reads=[b_ps], writes=[qT_b[gi]])
                        ps, b_ps = psA.next()
                        S.group(PE, mm8(ps[:], lambda k: wq[:, 1, k, :], lambda k: hT[:, k, gi * 512:(gi + 1) * 512]),
                                reads=[b_wq] + hT_b[4 * gi:4 * gi + 4], writes=[b_ps])
                        S.op(DVE, lambda e: e.tensor_copy(out=kT[:, gi * 512:(gi + 1) * 512], in_=ps[:]),
                             reads=[b_ps], writes=[kT_b[gi]])
                    for g4 in range(8):
                        ps, b_ps = psA.next()
                        fns = []
                        for i in range(4):
                            t = 4 * g4 + i
                            fns += mm8(ps[:, i * 128:(i + 1) * 128], lambda k, t=t: hT[:, k, t * 128:(t + 1) * 128],
                                       lambda k: wq[:, 2, k, :])
                        S.group(PE, fns, reads=[b_wq] + hT_b[4 * g4:4 * g4 + 4], writes=[b_ps])
                        S.op(ACT, lambda e: e.copy(out=Vt[:, 4 * g4:4 * g4 + 4, :],
                                                   in_=ps[:].rearrange("p (a b) -> p a b", a=4)),
                             reads=[b_ps], writes=[V_b[g4]])
                    S.op(DVE, lambda e: e.tensor_reduce(out=kmf[:], in_=kT[:].rearrange("p (n l) -> p n l", l=256),
                                                        axis=AX.X, op=ALU.add), reads=kT_b, writes=[b_km])
                    S.op(DVE, lambda e: e.tensor_scalar(out=km2[:].rearrange("p (n two) -> p n two", two=2),
                                                        in0=kmf[:].unsqueeze(2).to_broadcast([128, 16, 2]),
                                                        scalar1=1.0 / 256.0, scalar2=None, op0=ALU.mult),
                         reads=[b_km], writes=[b_km])
                    for half in range(2):
                        ps, b_ps = psA.next()
                        fns = []
                        for i in range(16):
                            t = half * 16 + i
                            fns.append(lambda e, t=t, i=i: e.matmul(ps[:, i * 32:(i + 1) * 32], lhsT=qT[:, t * 128:(t + 1) * 128],
                                                                   rhs=km2[:], start=True, stop=True))
                        S.group(PE, fns, reads=[b_km] + qT_b[4 * half:4 * half + 4], writes=[b_ps])
                        S.op(DVE, lambda e: e.tensor_tensor(out=gm[:, half * 16:(half + 1) * 16, :].rearrange("p a b -> p (a b)"),
                                                            in0=ps[:],
                                                            in1=PB[:, half * 16:(half + 1) * 16, :].rearrange("p a b -> p (a b)"),
                                                            op=ALU.add), reads=[b_ps, b_c2], writes=[b_gm])
                    for t in range(NT):
                        S.op(DVE, lambda e, t=t: e.max(out=m8[:, t, :], in_=gm[:, t, :]), reads=[b_gm], writes=[b_m8])
                    S.op(DVE, lambda e: e.tensor_tensor(out=gm[:], in0=gm[:], in1=m8[:, :, 5:6].to_broadcast([128, 32, 32]),
                                                        op=ALU.is_ge), reads=[b_gm, b_m8], writes=[b_gm])
                    S.op(DVE, lambda e: e.tensor_tensor(out=gm[:], in0=gm[:], in1=C1[:], op=ALU.mult),
                         reads=[b_gm, b_c2], writes=[b_gm])
                    S.op(DVE, lambda e: e.tensor_tensor(out=gm[:], in0=gm[:], in1=C2[:], op=ALU.add),
                         reads=[b_gm, b_c2], writes=[b_gm])
                def prep_b(hd):
                    sl = hd % NB2
                    biasT, bias_b = bT_l[sl], bias_bl[sl]
                    for c in range(8):
                        ps, b_ps = psA.next()
                        S.group(PE, [(lambda e, i=i: e.transpose(out=ps[0:32, i * 128:(i + 1) * 128], in_=gm[:, 4 * c + i, :],
                                                                 identity=identf[:])) for i in range(4)],
                                reads=[b_gm, b_const], writes=[b_ps])
                        S.op(ACT, lambda e: e.copy(out=biasT[0:32, c * 512:(c + 1) * 512], in_=ps[0:32, :]),
                             reads=[b_ps], writes=[bias_b[c]])

                def main_head(hd, mid_fn=None):
                    sl = hd % NB2
                    qT, kT, Vt, biasT = qT_l[sl], kT_l[sl], Vt_l[sl], bT_l[sl]
                    qT_b, kT_b, V_b, bias_b = qT_bl[sl], kT_bl[sl], V_bl[sl], bias_bl[sl]
                    its = [(c, j) for c in range(8) for j in range(4 * c + 4)]

                    def stage_a(c, j):
                        ps, b_ps = psA.next()
                        S.group(PE, [
                            lambda e: e.matmul(ps[:], lhsT=kT[:, j * 128:(j + 1) * 128], rhs=qT[:, c * 512:(c + 1) * 512],
                                               start=True, stop=False),
                            lambda e: e.matmul(ps[:], lhsT=Esel[:, j, :], rhs=biasT[:, c * 512:(c + 1) * 512],
                                               start=False, stop=True)],
                            reads=[kT_b[j // 4], qT_b[c], bias_b[c], b_c2], writes=[b_ps])
                        pT, b_pT = pT_r.next()
                        S.op(ACT, lambda e: e.activation(out=pT[:], in_=ps[:], func=AF.Exp), reads=[b_ps], writes=[b_pT])
                        if j >= 4 * c:
                            col = (j - 4 * c) * 128
                            S.op(POOL, lambda e: e.tensor_tensor(out=pT[:, col:col + 128], in0=pT[:, col:col + 128],
                                                                 in1=tri[:], op=ALU.mult),
                                 reads=[b_pT, b_const], writes=[b_pT])
                        return pT, b_pT

                    def stage_b(c, j, pT, b_pT):
                        nj = 4 * c + 4
                        S.group(PE, [
                            lambda e: e.matmul(psO_t[:], lhsT=Vt[:, j, :], rhs=pT[:], start=(j == 0), stop=(j == nj - 1)),
                            lambda e: e.matmul(psR_t[:], lhsT=onesb[:], rhs=pT[:], start=(j == 0), stop=(j == nj - 1))],
                            reads=[V_b[j // 4], b_pT, b_const], writes=[psO_b, psR_b])
                        if j == nj - 1:
                            rinv, b_rinv = rinv_r.next()
                            osb, b_osb = osb_r.next()
                            S.op(ACT, lambda e: e.copy(out=rinv[:], in_=psR_t[:]), reads=[psR_b], writes=[b_rinv])
                            S.op(ACT, lambda e: e.copy(out=osb[:], in_=psO_t[:]), reads=[psO_b], writes=[b_osb])
                            S.op(DVE, lambda e: e.reciprocal(out=rinv[:], in_=rinv[:]), reads=[b_rinv], writes=[b_rinv])
                            ybt, b_ybt = ybt_r.next()
                            S.op(DVE, lambda e: e.tensor_tensor(out=ybt[:], in0=osb[:], in1=rinv[:], op=ALU.mult),
                                 reads=[b_osb, b_rinv], writes=[b_ybt])
                            S.dma(SP, "st", lambda e: e.dma_start(out=YBd[:, hd, c * 512:(c + 1) * 512], in_=ybt[:]),
                                  reads=[b_ybt])

                    SK = 2
                    pend = []
                    for i in range(len(its) + SK):
                        if i == 48 and mid_fn is not None:
                            mid_fn()
                        if i < len(its):
                            pend.append(stage_a(*its[i]))
                        if i >= SK:
                            stage_b(*its[i - SK], *pend[i - SK])

                load_w(0)
                load_w(1)
                prep_head(0)
                prep_b(0)
                for hd in range(8):
                    if hd + 1 < 8:
                        prep_head(hd + 1)
                    if hd + 2 < 8:
                        load_w(hd + 2)
                    main_head(hd, (lambda h=hd + 1: prep_b(h)) if hd + 1 < 8 else None)
                S.barrier()

            if STOP_AFTER >= 3:
              with ExitStack() as p3:
                def sb3(name, shape, dt):
                    return p3.enter_context(nc.sbuf_tensor(name, shape, dt))

                wb = sb3("wb", [128, 8, D], BF16)
                wgb = sb3("wgb", [128, 8, D], BF16)
                b_w3 = Buf()
                load_w_cast(wb[:], w_bb.rearrange("(k p) n -> p k n", p=128), b_w3)
                load_w_cast(wgb[:], win_v[:, :, 6144:7168], b_w3)
                ybg_r = Ring(sb3, "ybg", [128, 8, 512], BF16, 2)
                sg_r = Ring(sb3, "sg3", [128, 512], F32, 2)
                mbt_r = Ring(sb3, "mbt", [128, 8, 512], BF16, 2)
                for c in range(8):
                    ybg, b_ybg = ybg_r.next()
                    S.dma(POOL, "ld3", lambda e: e.dma_start(out=ybg[:], in_=YBd[:, :, c * 512:(c + 1) * 512]), writes=[b_ybg])
                    mbt, b_mbt = mbt_r.next()
                    for dc in range(8):
                        ps1, b_ps1 = psA.next()
                        S.group(PE, mm8(ps1[:], lambda k: wb[:, k, dc * 128:(dc + 1) * 128], lambda k: ybg[:, k, :]),
                                reads=[b_w3, b_ybg], writes=[b_ps1])
                        ps2, b_ps2 = psA.next()
                        S.group(PE, mm8(ps2[:], lambda k: wgb[:, k, dc * 128:(dc + 1) * 128],
                                        lambda k: hT[:, k, c * 512:(c + 1) * 512]),
                                reads=[b_w3] + hT_b[4 * c:4 * c + 4], writes=[b_ps2])
                        sg, b_sg = sg_r.next()
                        S.op(ACT, lambda e: e.activation(out=sg[:], in_=ps2[:], func=AF.Sigmoid), reads=[b_ps2], writes=[b_sg])
                        S.op(DVE, lambda e: e.tensor_tensor(out=mbt[:, dc, :], in0=ps1[:], in1=sg[:], op=ALU.mult),
                             reads=[b_ps1, b_sg], writes=[b_mbt])
                    S.dma(SP, "st", lambda e: e.dma_start(out=MBd[:, :, c * 512:(c + 1) * 512], in_=mbt[:]), reads=[b_mbt])
                S.barrier()

        if STOP_AFTER >= 4:
          with ExitStack() as p4:
            def sb4(name, shape, dt):
                return p4.enter_context(nc.sbuf_tensor(name, shape, dt))

            wu = sb4("wu", [128, 8, D], BF16)
            wv = sb4("wv", [128, 8, D], BF16)
            wa = sb4("wa", [128, 8, D], BF16)
            wga = sb4("wga", [128, 8, D], BF16)
            b_w4 = Buf()
            load_w_cast(wu[:], win_v[:, :, 0:1024], b_w4)
            load_w_cast(wv[:], win_v[:, :, 1024:2048], b_w4)
            load_w_cast(wa[:], w_ba.rearrange("(k p) n -> p k n", p=128), b_w4)
            load_w_cast(wga[:], win_v[:, :, 5120:6144], b_w4)
            g_in = sb4("g_in4", [128, D], F32)
            bb_in = sb4("bb_in4", [128, D], F32)
            g_gm = sb4("g_gm", [128, D], F32)
            bb_gm = sb4("bb_gm", [128, D], F32)
            bsb = sb4("bsb", [128, 4, 128], F32)
            b_par4 = Buf()
            bcast_load(g_in[:], ln_in_g, b_par4)
            bcast_load(bb_in[:], ln_in_b, b_par4)
            bcast_load(g_gm[:], gmlp_g, b_par4)
            bcast_load(bb_gm[:], gmlp_b, b_par4)
            bcast_load(bsb[:].rearrange("p g t -> p (g t)"), b_sp, b_par4)
            wsn = sb4("wsn", [128, 4, 128], F32)
            wsT = sb4("wsT", [128, 4, 128], BF16)
            b_ws = Buf()
            S.dma(SP, "const", lambda e: e.dma_start(out=wsn[:], in_=w_sp.rearrange("g t s -> t g s")), writes=[b_ws])
            ps, b_ps = psA.next()
            S.group(PE, [(lambda e, g=g: e.transpose(out=ps[:, g * 128:(g + 1) * 128], in_=wsn[:, g, :], identity=identf[:]))
                         for g in range(4)], reads=[b_ws, b_const], writes=[b_ps])
            S.op(DVE, lambda e: e.tensor_tensor(out=wsT[:], in0=ps[:].rearrange("p (g t) -> p g t", g=4),
                                                in1=tri[:].unsqueeze(1).to_broadcast([128, 4, 128]), op=ALU.mult),
                 reads=[b_ps, b_const], writes=[b_ws])

            xr = Ring(sb4, "x4", [128, D], F32, 2)
            hbr = Ring(sb4, "hb4", [128, D], BF16, 2)
            sc4 = ln_scratch(sb4, "l4")
            hTg_r = Ring(sb4, "hTg", [128, 8, 512], BF16, 2)
            uT_r = Ring(sb4, "uT", [128, 8, 512], BF16, 1)
            gv_r = Ring(sb4, "gv", [128, D], F32, 2)
            vn_r = Ring(sb4, "vn", [128, 4, D], BF16, 2)
            yat_r = Ring(sb4, "yat", [128, 8, 512], BF16, 1)
            mbg_r = Ring(sb4, "mbg", [128, 8, 512], BF16, 2)
            mgt_r = Ring(sb4, "mgt", [128, 8, 512], BF16, 2)
            sg_r = Ring(sb4, "sg4", [128, 512], F32, 2)
            t4_r = Ring(sb4, "t4", [128, 512], F32, 2)
            def prep_group(c, hTg, b_hTg):
                S.dma(POOL, "ld4h", lambda e: e.dma_start(out=hTg[:], in_=HTd[:, :, c * 512:(c + 1) * 512]), writes=[b_hTg])

            def v_tile(c, i, hTg, b_hTg, vn, b_vn):
                gv, b_gv = gv_r.next()
                for half in range(2):
                    ps, b_ps = psA.next()
                    S.group(PE, mm8(ps[:], lambda k: hTg[:, k, i * 128:(i + 1) * 128],
                                    lambda k: wv[:, k, half * 512:(half + 1) * 512]),
                            reads=[b_w4, b_hTg], writes=[b_ps])
                    S.op(ACT, lambda e: e.activation(out=gv[:, half * 512:(half + 1) * 512], in_=ps[:], func=AF.Gelu),
                         reads=[b_ps], writes=[b_gv])
                layer_norm(sc4, gv[:], b_gv, g_gm, bb_gm, b_par4, vn[:, i, :], b_vn)

            def u_stage(c, hTg, b_hTg):
                uT, b_uT = uT_r.next()
                for dc in range(8):
                    ps, b_ps = psA.next()
                    S.group(PE, mm8(ps[:], lambda k: wu[:, k, dc * 128:(dc + 1) * 128], lambda k: hTg[:, k, :]),
                            reads=[b_w4, b_hTg], writes=[b_ps])
                    S.op(ACT, lambda e: e.activation(out=uT[:, dc, :], in_=ps[:], func=AF.Gelu), reads=[b_ps], writes=[b_uT])
                return uT, b_uT

            def vs_stage(c, vn, b_vn, uT, b_uT):
                yat, b_yat = yat_r.next()
                for dc in range(8):
                    g = dc // 2
                    ps, b_ps = psA.next()
                    S.group(PE, [(lambda e, i=i: e.matmul(ps[:, i * 128:(i + 1) * 128], lhsT=vn[:, i, dc * 128:(dc + 1) * 128],
                                                          rhs=wsT[:, g, :], start=True, stop=True)) for i in range(4)],
                            reads=[b_vn, b_ws], writes=[b_ps])
                    t4, b_t4 = t4_r.next()
                    S.op(DVE, lambda e: e.tensor_tensor(out=t4[:].rearrange("p (a b) -> p a b", a=4),
                                                        in0=ps[:].rearrange("p (a b) -> p a b", a=4),
                                                        in1=bsb[:, g:g + 1, :].to_broadcast([128, 4, 128]), op=ALU.add),
                         reads=[b_ps, b_par4], writes=[b_t4])
                    S.op(POOL, lambda e: e.tensor_tensor(out=yat[:, dc, :], in0=t4[:], in1=uT[:, dc, :], op=ALU.mult),
                         reads=[b_t4, b_uT], writes=[b_yat])
                return yat, b_yat

            def zg_stage(c, dcs, yat, b_yat, hTg, b_hTg, mbg, b_mbg, mgt, b_mgt):
                for dc in dcs:
                    ps1, b_ps1 = psA.next()
                    S.group(PE, mm8(ps1[:], lambda k: wa[:, k, dc * 128:(dc + 1) * 128], lambda k: yat[:, k, :]),
                            reads=[b_w4, b_yat], writes=[b_ps1])
                    ps2, b_ps2 = psA.next()
                    S.group(PE, mm8(ps2[:], lambda k: wga[:, k, dc * 128:(dc + 1) * 128], lambda k: hTg[:, k, :]),
                            reads=[b_w4, b_hTg], writes=[b_ps2])
                    sg, b_sg = sg_r.next()
                    S.op(ACT, lambda e: e.activation(out=sg[:], in_=ps2[:], func=AF.Sigmoid), reads=[b_ps2], writes=[b_sg])
                    t4, b_t4 = t4_r.next()
                    S.op(DVE, lambda e: e.tensor_tensor(out=t4[:], in0=ps1[:], in1=sg[:], op=ALU.mult),
                         reads=[b_ps1, b_sg], writes=[b_t4])
                    S.op(POOL, lambda e: e.tensor_tensor(out=mgt[:, dc, :], in0=t4[:], in1=mbg[:, dc, :], op=ALU.add),
                         reads=[b_t4, b_mbg], writes=[b_mgt])

            cur = hTg_r.next()
            prep_group(0, *cur)
            vcur = vn_r.next()
            for i in range(4):
                v_tile(0, i, *cur, *vcur)
            for c in range(8):
                hTg, b_hTg = cur
                vn, b_vn = vcur
                mbg, b_mbg = mbg_r.next()
                S.dma(SP, "ld4", lambda e: e.dma_start(out=mbg[:], in_=MBd[:, :, c * 512:(c + 1) * 512]), writes=[b_mbg])
                nxt = hTg_r.next() if c + 1 < 8 else None
                vnxt = vn_r.next() if c + 1 < 8 else None
                if nxt is not None:
                    prep_group(c + 1, *nxt)
                uT, b_uT = u_stage(c, hTg, b_hTg)
                yat, b_yat = vs_stage(c, vn, b_vn, uT, b_uT)
                mgt, b_mgt = mgt_r.next()
                for i in range(4):
                    zg_stage(c, range(2 * i, 2 * i + 2), yat, b_yat, hTg, b_hTg, mbg, b_mbg, mgt, b_mgt)
                    if nxt is not None:
                        v_tile(c + 1, i, *nxt, *vnxt)
                S.dma(SP, "st", lambda e: e.dma_start(out=MGd[:, :, c * 512:(c + 1) * 512], in_=mgt[:]), reads=[b_mgt])
                cur, vcur = nxt, vnxt
            S.barrier()

        if STOP_AFTER >= 5:
          with ExitStack() as p5:
            def sb5(name, shape, dt):
                return p5.enter_context(nc.sbuf_tensor(name, shape, dt))

            wo = sb5("wo", [128, 8, D], BF16)
            b_w5 = Buf()
            load_w_cast(wo[:], w_o.rearrange("(k p) n -> p k n", p=128), b_w5)
            wr = sb5("wr", [128, 8, NE], F32)
            S.dma(SP, "const", lambda e: e.dma_start(out=wr[:], in_=w_rt.rearrange("(k p) n -> p k n", p=128)), writes=[b_w5])
            g_in = sb5("g_in5", [128, D], F32)
            bb_in = sb5("bb_in5", [128, D], F32)
            g_mx = sb5("g_mx", [128, D], F32)
            bb_mx = sb5("bb_mx", [128, D], F32)
            brt = sb5("brt", [128, NE], F32)
            b_par5 = Buf()
            bcast_load(g_in[:], ln_in_g, b_par5)
            bcast_load(bb_in[:], ln_in_b, b_par5)
            bcast_load(g_mx[:], ln_mix_g, b_par5)
            bcast_load(bb_mx[:], ln_mix_b, b_par5)
            bcast_load(brt[:], b_rt, b_par5)
            ebase = sb5("ebase", [128, NE], F32)
            ebi = sb5("ebi", [128, NE], I32)
            S.op(POOL, lambda e: e.iota(ebi[:], pattern=[[CAP, NE]], base=0, channel_multiplier=0), writes=[b_par5])
            S.op(DVE, lambda e: e.tensor_copy(out=ebase[:], in_=ebi[:]), reads=[b_par5], writes=[b_par5])
            carry = sb5("carry", [128, NE], F32)
            b_carry = Buf()
            S.op(DVE, lambda e: e.memset(carry[:], 0.0), writes=[b_carry])

            B5 = 2
            D5 = 2
            hres_r = Ring(sb5, "hres", [128, D], F32, 6)
            sc5 = ln_scratch(sb5, "l5", 12)
            mgl_r = Ring(sb5, "mgl", [128, 8, 512], BF16, 2)
            t2_r = Ring(sb5, "t2", [128, D], F32, 6)
            h2_r = Ring(sb5, "h2", [128, D], F32, 6)
            h2b_r = Ring(sb5, "h2b", [128, D], BF16, 8)
            h2T_r = Ring(sb5, "h2T", [128, 8, 128], F32, 3)
            sm = {n: Ring(sb5, "sm_" + n, shp, dt, 14) for n, shp, dt in [
                ("lg", [128, NE], F32), ("m8", [128, 8], F32), ("nm", [128, 1], F32), ("ew", [128, 4], F32),
                ("ss", [128, 1], F32), ("selb", [128, NE], BF16), ("pos", [128, NE], F32), ("oh", [128, NE], F32),
                ("junk", [128, NE], F32), ("idxf", [128, 4], F32), ("pp", [128, 2 * NE], F32)]}
            mgl_cur = [None, None]

            def s0(cx):
                t = cx["t"]
                c, i = divmod(t, 4)
                if i == 0:
                    mgl, b_mgl = mgl_r.next()
                    S.dma(SP, "ld5", lambda e: e.dma_start(out=mgl[:], in_=MGd[:, :, c * 512:(c + 1) * 512]), writes=[b_mgl])
                    mgl_cur[0], mgl_cur[1] = mgl, b_mgl
                cx["mgl"], cx["b_mgl"] = mgl_cur[0], mgl_cur[1]
                hres, b_hres = hres_r.next()
                S.dma(SP, "x", lambda e: e.dma_start(out=hres[:], in_=Hd[t * 128:(t + 1) * 128, :]), writes=[b_hres])
                cx["hres"], cx["b_hres"] = hres, b_hres

            def s3a(cx):
                i = cx["t"] % 4
                mgl, b_mgl = cx["mgl"], cx["b_mgl"]
                cx["ps3"] = []
                for half in range(2):
                    ps, b_ps = psA.next()
                    S.group(PE, mm8(ps[:], lambda k: mgl[:, k, i * 128:(i + 1) * 128],
                                    lambda k: wo[:, k, half * 512:(half + 1) * 512]), reads=[b_w5, b_mgl], writes=[b_ps])
                    cx["ps3"].append((ps, b_ps))

            def s3b(cx):
                hres, b_hres = cx["hres"], cx["b_hres"]
                t2, b_t2 = t2_r.next()
                for half in range(2):
                    ps, b_ps = cx["ps3"][half]
                    S.op(DVE, lambda e: e.scalar_tensor_tensor(out=t2[:, half * 512:(half + 1) * 512],
                                                               in0=hres[:, half * 512:(half + 1) * 512], scalar=ALPHA,
                                                               in1=ps[:], op0=ALU.mult, op1=ALU.add),
                         reads=[b_hres, b_ps], writes=[b_t2])
                cx["t2"], cx["b_t2"] = t2, b_t2
                cx["st2"] = ln_stats_a(sc5, t2[:], b_t2)

            def s4b(cx):
                ln_stats_b(cx["st2"])

            def s5(cx):
                t = cx["t"]
                ln_stats_c(cx["st2"])
                h2, b_h2 = h2_r.next()
                ln_apply(sc5, cx["t2"][:], cx["b_t2"], cx["st2"], g_mx, bb_mx, b_par5, h2[:], b_h2)
                cx["h2"], cx["b_h2"] = h2, b_h2

            def s5b(cx):
                t = cx["t"]
                h2, b_h2 = cx["h2"], cx["b_h2"]
                S.dma(POOL, "st", lambda e: e.dma_start(out=H2d[t * 128:(t + 1) * 128, :], in_=h2[:]), reads=[b_h2])
                h2b, b_h2b = h2b_r.next()
                S.op(ACT, lambda e: e.copy(out=h2b[:], in_=h2[:]), reads=[b_h2], writes=[b_h2b])
                cx["h2b"], cx["b_h2b"] = h2b, b_h2b
                h2T, b_h2T = h2T_r.next()
                for hf in range(2):
                    ps, b_ps = psA.next()
                    S.group(PE, [(lambda e, k=k: e.transpose(out=ps[:, k * 128:(k + 1) * 128],
                                                             in_=h2[:, (4 * hf + k) * 128:(4 * hf + k + 1) * 128],
                                                             identity=identf[:])) for k in range(4)],
                            reads=[b_h2, b_const], writes=[b_ps])
                    S.op(ACT, lambda e: e.copy(out=h2T[:, 4 * hf:4 * hf + 4, :], in_=ps[:].rearrange("p (a b) -> p a b", a=4)),
                         reads=[b_ps], writes=[b_h2T])
                cx["h2T"], cx["b_h2T"] = h2T, b_h2T

            def s6b(cx):
                h2T, b_h2T = cx["h2T"], cx["b_h2T"]
                r8 = cx["t"] % 8
                ps, b_ps = psR_t[:, r8 * NE:(r8 + 1) * NE], psR_reg[r8]
                S.group(PE, mm8(ps, lambda k: h2T[:, k, :], lambda k: wr[:, k, :]), reads=[b_h2T, b_w5], writes=[b_ps])
                cx["psr"] = (ps, b_ps)

            def s6c(cx):
                ps, b_ps = cx["psr"]
                lg, b_lg = sm["lg"].next()
                S.op(DVE, lambda e: e.tensor_tensor(out=lg[:], in0=ps, in1=brt[:], op=ALU.add),
                     reads=[b_ps, b_par5], writes=[b_lg])
                m8t, b_m8t = sm["m8"].next()
                S.op(DVE, lambda e: e.max(out=m8t[:], in_=lg[:]), reads=[b_lg], writes=[b_m8t])
                nm, b_nm = sm["nm"].next()
                S.op(DVE, lambda e: e.tensor_scalar(out=nm[:], in0=m8t[:, 0:1], scalar1=-1.0, scalar2=None, op0=ALU.mult),
                     reads=[b_m8t], writes=[b_nm])
                selb, b_selb = sm["selb"].next()
                S.op(DVE, lambda e: e.tensor_scalar(out=selb[:], in0=lg[:], scalar1=m8t[:, 3:4], scalar2=None, op0=ALU.is_ge),
                     reads=[b_lg, b_m8t], writes=[b_selb])
                cx.update(lg=lg, b_lg=b_lg, m8t=m8t, b_m8t=b_m8t, nm=nm, b_nm=b_nm, selb=selb, b_selb=b_selb)

            def s7a(cx):
                m8t, b_m8t, nm, b_nm, selb, b_selb = cx["m8t"], cx["b_m8t"], cx["nm"], cx["b_nm"], cx["selb"], cx["b_selb"]
                ew, b_ew = sm["ew"].next()
                ss, b_ss = sm["ss"].next()
                S.op(ACT, lambda e: e.activation(out=ew[:], in_=m8t[:, 0:4], func=AF.Exp, bias=nm[:, 0:1], scale=1.0,
                                                 accum_out=ss[:]), reads=[b_m8t, b_nm], writes=[b_ew, b_ss])
                r8 = cx["t"] % 8
                ps, b_ps = psO_t[:, r8 * 2 * NE:(r8 + 1) * 2 * NE], psO_reg[r8]
                S.group(PE, [lambda e: e.matmul(ps[:, 0:NE], lhsT=ustr[:], rhs=selb[:], start=True, stop=True),
                             lambda e: e.matmul(ps[:, NE:2 * NE], lhsT=onesb[:], rhs=selb[:], start=True, stop=True)],
                        reads=[b_selb, b_const], writes=[b_ps])
                cx.update(ew=ew, b_ew=b_ew, ss=ss, b_ss=b_ss, psp=(ps, b_ps))

            def s7b(cx):
                t = cx["t"]
                ew, b_ew, ss, b_ss = cx["ew"], cx["b_ew"], cx["ss"], cx["b_ss"]
                ps, b_ps = cx["psp"]
                pp, b_pp = sm["pp"].next()
                S.op(DVE, lambda e: e.tensor_copy(out=pp[:], in_=ps), reads=[b_ps], writes=[b_pp])
                S.op(DVE, lambda e: e.reciprocal(out=ss[:], in_=ss[:]), reads=[b_ss], writes=[b_ss])
                S.op(DVE, lambda e: e.tensor_scalar(out=gate_all[:, t, :], in0=ew[:], scalar1=ss[:, 0:1], scalar2=None,
                                                    op0=ALU.mult), reads=[b_ew, b_ss], writes=[b_gate])
                cx.update(pp=pp, b_pp=b_pp)

            def s8(cx):
                t = cx["t"]
                lg, b_lg, m8t, b_m8t, pp, b_pp = cx["lg"], cx["b_lg"], cx["m8t"], cx["b_m8t"], cx["pp"], cx["b_pp"]
                h2b, b_h2b = cx["h2b"], cx["b_h2b"]
                pos, b_pos = sm["pos"].next()
                S.op(DVE, lambda e: e.tensor_tensor(out=pos[:], in0=pp[:, 0:NE], in1=carry[:], op=ALU.add),
                     reads=[b_pp, b_carry], writes=[b_pos])
                S.op(DVE, lambda e: e.tensor_tensor(out=carry[:], in0=pp[:, NE:2 * NE], in1=carry[:], op=ALU.add),
                     reads=[b_pp, b_carry], writes=[b_carry])
                S.op(DVE, lambda e: e.tensor_tensor(out=pos[:], in0=pos[:], in1=ebase[:], op=ALU.add),
                     reads=[b_pos, b_par5], writes=[b_pos])
                idxf, b_idxf = sm["idxf"].next()
                for k in range(4):
                    oh, b_oh = sm["oh"].next()
                    S.op(DVE, lambda e, k=k: e.tensor_scalar(out=oh[:], in0=lg[:], scalar1=m8t[:, k:k + 1], scalar2=None,
                                                             op0=ALU.is_equal), reads=[b_lg, b_m8t], writes=[b_oh])
                    junk, b_junk = sm["junk"].next()
                    S.op(DVE, lambda e, k=k: e.scalar_tensor_tensor(out=junk[:], in0=oh[:], scalar=1.0, in1=pos[:],
                                                                    op0=ALU.mult, op1=ALU.mult, accum_out=idxf[:, k:k + 1]),
                         reads=[b_oh, b_pos], writes=[b_junk, b_idxf])
                S.op(DVE, lambda e: e.tensor_copy(out=idx_all[:, t, :], in_=idxf[:]), reads=[b_idxf], writes=[b_idx])
                for k in range(4 if not os.environ.get("MK_NOSC") else 0):
                    S.dma(POOL, "sc", lambda e, k=k: e.indirect_dma_start(
                        out=Xg[:, :], out_offset=bass.IndirectOffsetOnAxis(ap=idx_all[:, t, k:k + 1], axis=0),
                        in_=h2b[:], in_offset=None), reads=[b_h2b, b_idx])

            _rb, _ob = Buf(), Buf()
            psR_reg = [_rb] * 8
            psO_reg = [_ob] * 8
            assert 2 * B5 <= len(psA.items)
            stages5 = [(s0,), (s3a, s3b), (s4b,), (s5,), (s5b,), (s6b,), (s6c,), (s7a,), (s7b,), (s8,)]
            nb5 = NT // B5
            batches5 = [[{"t": t} for t in range(b * B5, (b + 1) * B5)] for b in range(nb5)]
            for step in range(len(stages5) + D5 * (nb5 - 1)):
                for b in range(nb5):
                    k = step - D5 * b
                    if 0 <= k < len(stages5):
                        for fn in stages5[k]:
                            for cx in batches5[b]:
                                fn(cx)
            if DEBUG:
                S.dma(SP, "st", lambda e: e.dma_start(out=IDXd[:, :], in_=idx_all[:].rearrange("p a b -> p (a b)")), reads=[b_idx])
                S.dma(SP, "st", lambda e: e.dma_start(out=GATd[:, :], in_=gate_all[:].rearrange("p a b -> p (a b)")), reads=[b_gate])
            S.barrier()

        if STOP_AFTER >= 6:
          with ExitStack() as p6:
            def sb6(name, shape, dt):
                return p6.enter_context(nc.sbuf_tensor(name, shape, dt))

            bun = sb6("bun", [NE, 2 * D], F32)
            bu_all = sb6("bu_all", [128, 16, NE], F32)
            b_bu = Buf()
            S.dma(SP, "const", lambda e: e.dma_start(out=bun[:], in_=b_up[:, :]), writes=[b_bu])
            for q4 in range(4):
                ps, b_ps = psA.next()
                S.group(PE, [(lambda e, i=i: e.transpose(out=ps[:, i * NE:(i + 1) * NE],
                                                         in_=bun[:, (4 * q4 + i) * 128:(4 * q4 + i + 1) * 128],
                                                         identity=identf[0:NE, 0:NE])) for i in range(4)],
                        reads=[b_bu, b_const], writes=[b_ps])
                S.op(DVE, lambda e: e.tensor_copy(out=bu_all[:, 4 * q4:4 * q4 + 4, :],
                                                  in_=ps[:, 0:4 * NE].rearrange("p (a b) -> p a b", a=4)),
                     reads=[b_ps], writes=[b_bu])
            wu_r = Ring(sb6, "wup", [128, 8, 2 * D], BF16, 2)
            wd_r = Ring(sb6, "wdn", [128, 8, D], BF16, 2)
            bd_r = Ring(sb6, "bdn", [128, D], F32, 2)
            xs_r = Ring(sb6, "xs", [128, D], BF16, 6)
            XT_r = Ring(sb6, "XT", [128, 8, CAP], BF16, 2)
            aT_r = Ring(sb6, "aT", [128, 8, CAP], BF16, 1)
            HW = CAP // 2
            gb_r = Ring(sb6, "gb", [128, HW], F32, 2)
            sg_r = Ring(sb6, "sg6", [128, HW], F32, 2)
            ub_r = Ring(sb6, "ub", [128, HW], F32, 2)
            ys_r = Ring(sb6, "ys", [128, D], F32, 2)

            def load_expert(e_):
                wu_t, b_wu = wu_r.next()
                wd_t, b_wd = wd_r.next()
                bd_t, b_bd = bd_r.next()
                load_w_cast(wu_t[:], w_up[e_].rearrange("(k p) n -> p k n", p=128), b_wu, "we")
                load_w_cast(wd_t[:], w_dn[e_].rearrange("(k p) n -> p k n", p=128), b_wd, "we")
                S.dma(SP, "bd", lambda e: e.dma_start(out=bd_t[:], in_=b_dn[e_].partition_broadcast(128)), writes=[b_bd])
                return (wu_t, b_wu, wd_t, b_wd, bd_t, b_bd)

            def load_x_dma(e_):
                tiles = []
                for s_ in range(NSL):
                    xs, b_xs = xs_r.next()
                    r0 = e_ * CAP + s_ * 128
                    S.dma(SP, "xs", lambda e: e.dma_start(out=xs[:], in_=Xg[r0:r0 + 128, :]), writes=[b_xs])
                    tiles.append((xs, b_xs))
                return tiles

            def load_x_tile(tiles, s_, XT, b_XT):
                xs, b_xs = tiles[s_]
                transpose_bf(xs, b_xs, XT[:, :, s_ * 128:(s_ + 1) * 128], b_XT, DVE if s_ % 2 else ACT)

            def load_x(e_):
                XT, b_XT = XT_r.next()
                tiles = load_x_dma(e_)
                for s_ in range(NSL):
                    load_x_tile(tiles, s_, XT, b_XT)
                return XT, b_XT

            def up_proj(ex, wts, XT, b_XT):
                wu_t, b_wu = wts[0], wts[1]
                aT, b_aT = aT_r.next()
                for cc in range(8):
                    for hf in range(2):
                        sl = slice(hf * HW, (hf + 1) * HW)
                        psg_, b_psg = psA.next()
                        S.group(PE, mm8(psg_[:, 0:HW], lambda k: wu_t[:, k, cc * 128:(cc + 1) * 128], lambda k: XT[:, k, sl]),
                                reads=[b_wu, b_XT], writes=[b_psg])
                        psu_, b_psu = psA.next()
                        S.group(PE, mm8(psu_[:, 0:HW], lambda k: wu_t[:, k, D + cc * 128:D + (cc + 1) * 128],
                                        lambda k: XT[:, k, sl]), reads=[b_wu, b_XT], writes=[b_psu])
                        gb, b_gb = gb_r.next()
                        S.op(ACT, lambda e: e.activation(out=gb[:], in_=psg_[:, 0:HW], func=AF.Identity,
                                                         bias=bu_all[:, cc, ex:ex + 1], scale=1.0),
                             reads=[b_psg, b_bu], writes=[b_gb])
                        ub, b_ub = ub_r.next()
                        S.op(ACT, lambda e: e.activation(out=ub[:], in_=psu_[:, 0:HW], func=AF.Identity,
                                                         bias=bu_all[:, 8 + cc, ex:ex + 1], scale=1.0),
                             reads=[b_psu, b_bu], writes=[b_ub])
                        S.op(DVE, lambda e: e.tensor_scalar(out=gb[:], in0=gb[:], scalar1=7.0, scalar2=None, op0=ALU.min),
                             reads=[b_gb], writes=[b_gb])
                        sg, b_sg = sg_r.next()
                        S.op(ACT, lambda e: e.activation(out=sg[:], in_=gb[:], func=AF.Sigmoid, scale=1.702),
                             reads=[b_gb], writes=[b_sg])
                        S.op(POOL, lambda e: e.tensor_tensor(out=sg[:], in0=gb[:], in1=sg[:], op=ALU.mult),
                             reads=[b_gb, b_sg], writes=[b_sg])
                        S.op(DVE, lambda e: e.tensor_scalar(out=ub[:], in0=ub[:], scalar1=7.0, scalar2=-7.0, op0=ALU.min,
                                                            op1=ALU.max), reads=[b_ub], writes=[b_ub])
                        S.op(DVE, lambda e: e.scalar_tensor_tensor(out=aT[:, cc, sl], in0=ub[:], scalar=1.0, in1=sg[:],
                                                                   op0=ALU.add, op1=ALU.mult),
                             reads=[b_ub, b_sg], writes=[b_aT])
                return aT, b_aT

            def down_proj(ex, wts, aT, b_aT, xnext=None):
                wd_t, b_wd, bd_t, b_bd = wts[2], wts[3], wts[4], wts[5]
                for s_ in range(NSL):
                    if xnext is not None:
                        load_x_tile(xnext[0], s_, xnext[1], xnext[2])
                    ys, b_ys = ys_r.next()
                    for hf in range(2):
                        ps, b_ps = psA.next()
                        S.group(PE, mm8(ps[:], lambda k: aT[:, k, s_ * 128:(s_ + 1) * 128],
                                        lambda k: wd_t[:, k, hf * 512:(hf + 1) * 512]), reads=[b_aT, b_wd], writes=[b_ps])
                        S.op(DVE, lambda e: e.tensor_tensor(out=ys[:, hf * 512:(hf + 1) * 512], in0=ps[:],
                                                            in1=bd_t[:, hf * 512:(hf + 1) * 512], op=ALU.add),
                             reads=[b_ps, b_bd], writes=[b_ys])
                    r0 = ex * CAP + s_ * 128
                    S.dma(SP, "st", lambda e: e.dma_start(out=Yg[r0:r0 + 128, :], in_=ys[:]), reads=[b_ys])

            wts = load_expert(0)
            XTc = load_x(0)
            for ex in range(NE):
                wts_n = load_expert(ex + 1) if ex + 1 < NE else None
                aTc = up_proj(ex, wts, *XTc)
                XTn, xnext = None, None
                if ex + 1 < NE:
                    XTn = XT_r.next()
                    xnext = (load_x_dma(ex + 1), XTn[0], XTn[1])
                down_proj(ex, wts, *aTc, xnext=xnext)
                wts, XTc = wts_n, XTn
            S.barrier()

        if STOP_AFTER >= 7:
          with ExitStack() as p7:
            def sb7(name, shape, dt):
                return p7.enter_context(nc.sbuf_tensor(name, shape, dt))

            g_ff = sb7("g_ff", [128, D], F32)
            bb_ff = sb7("bb_ff", [128, D], F32)
            b_par7 = Buf()
            bcast_load(g_ff[:], ln_ffn_g, b_par7)
            bcast_load(bb_ff[:], ln_ffn_b, b_par7)
            B7 = 4
            yk_r = Ring(sb7, "yk", [128, 4, D], F32, B7)
            h2l_r = Ring(sb7, "h2l", [128, D], F32, B7)
            acc_r = Ring(sb7, "acc", [128, D], F32, B7)
            o_r = Ring(sb7, "o7", [128, D], F32, B7)
            sc7 = ln_scratch(sb7, "l7", B7 + 1)

            def c0(cx):
                t = cx["t"]
                yk, b_yk = yk_r.next()
                for k in range(4):
                    S.dma(POOL, "ga", lambda e, k=k: e.indirect_dma_start(
                        out=yk[:, k, :], out_offset=None, in_=Yg[:, :],
                        in_offset=bass.IndirectOffsetOnAxis(ap=idx_all[:, t, k:k + 1], axis=0)), reads=[b_idx], writes=[b_yk])
                h2l, b_h2l = h2l_r.next()
                S.dma(POOL, "ld7", lambda e: e.dma_start(out=h2l[:], in_=H2d[t * 128:(t + 1) * 128, :]), writes=[b_h2l])
                cx.update(yk=yk, b_yk=b_yk, h2l=h2l, b_h2l=b_h2l)

            def c1(cx):
                t = cx["t"]
                yk, b_yk, h2l, b_h2l = cx["yk"], cx["b_yk"], cx["h2l"], cx["b_h2l"]
                acc, b_acc = acc_r.next()
                S.op(ACT, lambda e: e.mul(out=acc[:], in_=h2l[:], mul=ALPHA), reads=[b_h2l], writes=[b_acc])
                for k in range(4 if not os.environ.get('MK_NOSTT') else 1):
                    S.op(DVE, lambda e, k=k: e.scalar_tensor_tensor(out=acc[:], in0=yk[:, k, :], scalar=gate_all[:, t, k:k + 1],
                                                                    in1=acc[:], op0=ALU.mult, op1=ALU.add),
                         reads=[b_yk, b_gate, b_acc], writes=[b_acc])
                cx.update(acc=acc, b_acc=b_acc)

            def c2(cx):
                cx["st"] = ln_stats(sc7, cx["acc"][:], cx["b_acc"])

            def c3(cx):
                t = cx["t"]
                ot, b_ot = o_r.next()
                ln_apply(sc7, cx["acc"][:], cx["b_acc"], cx["st"], g_ff, bb_ff, b_par7, ot[:], b_ot)
                S.dma(SP, "out", lambda e: e.dma_start(out=out_d[t * 128:(t + 1) * 128, :], in_=ot[:]), reads=[b_ot])

            for b0 in range(0, NT, B7):
                cxs = [{"t": t} for t in range(b0, b0 + B7)]
                for st_fn in [c0, c1, c2, c3]:
                    for cx in cxs:
                        st_fn(cx)
            S.barrier()

        S.barrier()
        waited = S.waited
    return nc, waited


_INPUT_ORDER = ["x", "ln_in_g", "ln_in_b", "w_in", "gmlp_ln_g", "gmlp_ln_b", "w_spatial", "b_spatial", "w_branch_a",
                "w_branch_b", "w_out", "ln_mix_g", "ln_mix_b", "w_router", "b_router", "w_up", "b_up", "w_down",
                "b_down", "ln_ffn_g", "ln_ffn_b"]


def _prep_inputs(inputs):
    a = {k: np.ascontiguousarray(np.asarray(v), dtype=np.float32) for k, v in inputs.items()}
    shared = {
        "ln_in_g": a["ln_in_g"].reshape(D), "ln_in_b": a["ln_in_b"].reshape(D),
        "w_in": a["w_in"].reshape(D, 7 * D),
        "gmlp_ln_g": a["gmlp_ln_g"].reshape(D), "gmlp_ln_b": a["gmlp_ln_b"].reshape(D),
        "w_spatial": a["w_spatial"].reshape(4, 128, 128), "b_spatial": a["b_spatial"].reshape(512),
        "w_branch_a": a["w_branch_a"].reshape(D, D), "w_branch_b": a["w_branch_b"].reshape(D, D),
        "w_out": a["w_out"].reshape(D, D),
        "ln_mix_g": a["ln_mix_g"].reshape(D), "ln_mix_b": a["ln_mix_b"].reshape(D),
        "w_router": a["w_router"].reshape(D, NE), "b_router": a["b_router"].reshape(NE),
        "w_up": a["w_up"].reshape(NE, D, 2 * D), "b_up": a["b_up"].reshape(NE, 2 * D),
        "w_down": a["w_down"].reshape(NE, D, D), "b_down": a["b_down"].reshape(NE, D),
        "ln_ffn_g": a["ln_ffn_g"].reshape(D), "ln_ffn_b": a["ln_ffn_b"].reshape(D),
    }
    return a["x"], shared


def kernel(**inputs):
    x, shared = _prep_inputs(inputs)
    n = x.shape[0]
    nc = build_program()
    in_maps = []
    for b in range(n):
        m = dict(shared)
        m["x"] = np.ascontiguousarray(x[b])
        in_maps.append(m)
    res = run_bass_kernel_spmd(nc, in_maps, core_ids=list(range(n)))
    out = np.stack([np.asarray(r["out"]) for r in res.results], axis=0).astype(np.float32)
    return out
```

```python
import os
import bisect
import numpy as np
from contextlib import ExitStack
import concourse.bass as bass
import concourse.mybir as mybir
from concourse.bass_utils import run_bass_kernel_spmd

F32 = mybir.dt.float32
BF16 = mybir.dt.bfloat16
I32 = mybir.dt.int32
AF = mybir.ActivationFunctionType
ALU = mybir.AluOpType
AX = mybir.AxisListType

S_TOK = 4096
D = 1024
NT = 32
NE = 32
CAP = 768
NSL = CAP // 128
ALPHA = float(2 ** 0.25)
EPS = 1e-5
DEBUG = bool(int(os.environ.get("MK_DEBUG", "0")))
STOP_AFTER = int(os.environ.get("MK_STOP", "99"))


class Buf:
    __slots__ = ("w", "r")

    def __init__(self):
        self.w = None
        self.r = []


class Eng:
    def __init__(self, nc, eng, name):
        self.eng = eng
        self.name = name
        self.sem = nc.alloc_semaphore(name=name)
        self.count = 0
        self.seen = {}


class Sync:
    def __init__(self, nc, waitsets=None):
        self.nc = nc
        self.waitsets = waitsets
        self.waited = {}
        self.pe = Eng(nc, nc.tensor, "s_pe")
        self.act = Eng(nc, nc.scalar, "s_act")
        self.dve = Eng(nc, nc.vector, "s_dve")
        self.pool = Eng(nc, nc.gpsimd, "s_pool")
        self.sp = Eng(nc, nc.sync, "s_sp")
        self.engs = [self.pe, self.act, self.dve, self.pool, self.sp]
        self.dsems = {}
        self._keep = []
        self.wsets = {k: set(v) for k, v in (waitsets or {}).items()}

    def dsem(self, key):
        k = id(key)
        if k not in self.dsems:
            self.dsems[k] = Eng(self.nc, None, "d_%d" % len(self.dsems))
            self._keep.append(key)
        return self.dsems[k]

    def _wait(self, E, reads, writes):
        deps = {}

        def add(tok):
            if tok is None:
                return
            s, v = tok
            if deps.get(s, 0) < v:
                deps[s] = v

        for t in reads:
            add(t.w)
        for t in writes:
            add(t.w)
            for r in t.r:
                add(r)
        for s, v in deps.items():
            if E.seen.get(s, 0) < v:
                E.eng.wait_ge(s.sem, self._val(s, v))
                E.seen[s] = v

    def _val(self, s, v):
        if s.eng is None:
            return v
        if self.waitsets is None:
            self.waited.setdefault(s.name, set()).add(v)
            return v
        return bisect.bisect_right(self.waitsets[s.name], v)

    def _signals(self, E):
        return self.waitsets is None or E.count in self.wsets.get(E.name, ())

    def _done(self, tok, reads, writes):
        for t in reads:
            t.r.append(tok)
            if len(t.r) > 64:
                best = {}
                for s, v in t.r:
                    if best.get(s, 0) < v:
                        best[s] = v
                t.r = list(best.items())
        for t in writes:
            t.w = tok
            t.r = []

    def op(self, E, fn, reads=(), writes=()):
        self._wait(E, reads, writes)
        inst = fn(E.eng)
        E.count += 1
        if self._signals(E):
            inst.then_inc(E.sem, 1)
        self._done((E, E.count), reads, writes)

    def group(self, E, fns, reads=(), writes=()):
        self._wait(E, reads, writes)
        inst = None
        for fn in fns:
            inst = fn(E.eng)
        E.count += 1
        if self._signals(E):
            inst.then_inc(E.sem, 1)
        self._done((E, E.count), reads, writes)

    def dma(self, Q, dname, fn, reads=(), writes=()):
        key = writes[0] if len(writes) else reads[0]
        Dm = self.dsem(key)
        self._wait(Q, reads, writes)
        inst = fn(Q.eng)
        Dm.count += 16
        inst.then_inc(Dm.sem, 16)
        self._done((Dm, Dm.count), reads, writes)

    def barrier(self):
        allq = self.engs + list(self.dsems.values())
        for E in self.engs:
            for X in allq:
                if X is E or X.count == 0:
                    continue
                if E.seen.get(X, 0) < X.count:
                    E.eng.wait_ge(X.sem, self._val(X, X.count))
                    E.seen[X] = X.count


class Ring:
    def __init__(self, alloc, name, shape, dt, n):
        self.items = [(alloc("%s_%d" % (name, i), shape, dt), Buf()) for i in range(n)]
        self.i = 0

    def next(self):
        it = self.items[self.i % len(self.items)]
        self.i += 1
        return it


def build_program():
    _, waited = _build(None)
    nc, _ = _build({k: sorted(v) for k, v in waited.items()})
    return nc


def _build(waitsets):
    nc = bass.Bass("TRN2", target_bir_lowering=False)

    def din(name, shape, dt=F32):
        return nc.dram_tensor(name, list(shape), dt, kind="ExternalInput").ap()

    x_d = din("x", [S_TOK, D])
    ln_in_g = din("ln_in_g", [D])
    ln_in_b = din("ln_in_b", [D])
    w_in = din("w_in", [D, 7 * D])
    gmlp_g = din("gmlp_ln_g", [D])
    gmlp_b = din("gmlp_ln_b", [D])
    w_sp = din("w_spatial", [4, 128, 128])
    b_sp = din("b_spatial", [512])
    w_ba = din("w_branch_a", [D, D])
    w_bb = din("w_branch_b", [D, D])
    w_o = din("w_out", [D, D])
    ln_mix_g = din("ln_mix_g", [D])
    ln_mix_b = din("ln_mix_b", [D])
    w_rt = din("w_router", [D, NE])
    b_rt = din("b_router", [NE])
    w_up = din("w_up", [NE, D, 2 * D])
    b_up = din("b_up", [NE, 2 * D])
    w_dn = din("w_down", [NE, D, D])
    b_dn = din("b_down", [NE, D])
    ln_ffn_g = din("ln_ffn_g", [D])
    ln_ffn_b = din("ln_ffn_b", [D])
    out_d = nc.dram_tensor("out", [S_TOK, D], F32, kind="ExternalOutput").ap()

    dbgkind = "ExternalOutput" if DEBUG else "Internal"
    YBd = nc.dram_tensor("ybd", [128, 8, S_TOK], BF16, kind=dbgkind).ap()
    MBd = nc.dram_tensor("mbd", [128, 8, S_TOK], BF16, kind="Internal").ap()
    MGd = nc.dram_tensor("mgd", [128, 8, S_TOK], BF16, kind=dbgkind).ap()
    H2d = nc.dram_tensor("h2d", [S_TOK, D], F32, kind=dbgkind).ap()
    Xg = nc.dram_tensor("xg", [NE * CAP, D], BF16, kind="Internal").ap()
    Hd = nc.dram_tensor("hd", [S_TOK, D], F32, kind="Internal").ap()
    INV = nc.dram_tensor("inv", [NE * CAP, 16], I32, kind="Internal").ap()
    YT = nc.dram_tensor("yt", [S_TOK * 4, D], F32, kind="Internal").ap()
    HTd = nc.dram_tensor("htd", [128, 8, S_TOK], BF16, kind="Internal").ap()
    Yg = nc.dram_tensor("yg", [NE * CAP, D], F32, kind="Internal").ap()
    if DEBUG:
        IDXd = nc.dram_tensor("idxd", [128, NT * 4], I32, kind="ExternalOutput").ap()
        GATd = nc.dram_tensor("gatd", [128, NT * 4], F32, kind="ExternalOutput").ap()

    win_v = w_in.rearrange("(k p) n -> p k n", p=128)

    with ExitStack() as es:
        def sbg(name, shape, dt):
            return es.enter_context(nc.sbuf_tensor(name, shape, dt))

        def psg(name, shape, dt):
            return es.enter_context(nc.psum_tensor(name, shape, dt))

        S = Sync(nc, waitsets)
        PE, ACT, DVE, POOL, SP = S.pe, S.act, S.dve, S.pool, S.sp

        psT = Ring(psg, "psT", [128, 8, 128], BF16, 2)
        psA = Ring(psg, "psA", [128, 512], F32, 4)
        psO_t = psg("psO", [128, 512], F32)
        psO_b = Buf()
        psR_t = psg("psR", [128, 512], F32)
        psR_b = Buf()

        onesf = sbg("onesf", [128, 128], F32)
        identf = sbg("identf", [128, 128], F32)
        ident = sbg("ident", [128, 128], BF16)
        onesb = sbg("onesb", [128, 128], BF16)
        tri = sbg("tri", [128, 128], BF16)
        ustr = sbg("ustr", [128, 128], BF16)
        idx_all = sbg("idx_all", [128, NT, 4], I32)
        gate_all = sbg("gate_all", [128, NT, 4], F32)
        b_const = Buf()
        b_idx = Buf()
        b_gate = Buf()
        S.op(POOL, lambda e: e.memset(onesf[:], 1.0), writes=[b_const])
        S.op(POOL, lambda e: e.memset(onesb[:], 1.0), writes=[b_const])
        S.op(POOL, lambda e: e.affine_select(out=identf[:], in_=onesf[:], pattern=[[-1, 128]], compare_op=ALU.is_equal,
                                             fill=0.0, base=0, channel_multiplier=1), reads=[b_const], writes=[b_const])
        S.op(POOL, lambda e: e.affine_select(out=ident[:], in_=onesf[:], pattern=[[-1, 128]], compare_op=ALU.is_equal,
                                             fill=0.0, base=0, channel_multiplier=1), reads=[b_const], writes=[b_const])
        S.op(POOL, lambda e: e.affine_select(out=tri[:], in_=onesf[:], pattern=[[1, 128]], compare_op=ALU.is_ge,
                                             fill=0.0, base=0, channel_multiplier=-1), reads=[b_const], writes=[b_const])
        S.op(POOL, lambda e: e.affine_select(out=ustr[:], in_=onesf[:], pattern=[[1, 128]], compare_op=ALU.is_ge,
                                             fill=0.0, base=-1, channel_multiplier=-1), reads=[b_const], writes=[b_const])

        def bcast_load(dst, src1d, buf):
            S.dma(SP, "const", lambda e: e.dma_start(out=dst, in_=src1d.partition_broadcast(128)), writes=[buf])

        def ln_stats_a(sc, src, b_src):
            st, b_st = sc["st"].next()
            mv, b_mv = sc["mv"].next()
            rs, b_rs = sc["rs"].next()
            for i in range(2):
                S.op(DVE, lambda e, i=i: e.bn_stats(out=st[:, i, :], in_=src[:, i * 512:(i + 1) * 512]),
                     reads=[b_src], writes=[b_st])
            S.op(DVE, lambda e: e.bn_aggr(out=mv[:], in_=st[:].rearrange("p a b -> p (a b)")), reads=[b_st], writes=[b_mv])
            S.op(DVE, lambda e: e.tensor_scalar(out=rs[:], in0=mv[:, 1:2], scalar1=EPS, scalar2=None, op0=ALU.add),
                 reads=[b_mv], writes=[b_rs])
            return (mv, b_mv, rs, b_rs)

        def ln_stats_b(stats):
            mv, b_mv, rs, b_rs = stats
            S.op(ACT, lambda e: e.activation(out=rs[:], in_=rs[:], func=AF.Sqrt), reads=[b_rs], writes=[b_rs])

        def ln_stats_c(stats):
            mv, b_mv, rs, b_rs = stats
            S.op(DVE, lambda e: e.reciprocal(out=rs[:], in_=rs[:]), reads=[b_rs], writes=[b_rs])

        def ln_stats(sc, src, b_src):
            stats = ln_stats_a(sc, src, b_src)
            ln_stats_b(stats)
            ln_stats_c(stats)
            return stats

        def ln_apply(sc, src, b_src, stats, g_t, b_t, b_par, dst, b_dst):
            mv, b_mv, rs, b_rs = stats
            tmp, b_tmp = sc["tmp"].next()
            S.op(DVE, lambda e: e.scalar_tensor_tensor(out=tmp[:], in0=src, scalar=mv[:, 0:1], in1=g_t[:],
                                                       op0=ALU.subtract, op1=ALU.mult),
                 reads=[b_src, b_mv, b_par], writes=[b_tmp])
            S.op(DVE, lambda e: e.scalar_tensor_tensor(out=dst, in0=tmp[:], scalar=rs[:, 0:1], in1=b_t[:],
                                                       op0=ALU.mult, op1=ALU.add),
                 reads=[b_tmp, b_rs, b_par], writes=[b_dst])

        def ln_apply_split(sc, src, b_src, stats, g_t, b_t, b_par, dst, b_dst):
            mv, b_mv, rs, b_rs = stats
            tmp, b_tmp = sc["tmp"].next()
            S.op(DVE, lambda e: e.scalar_tensor_tensor(out=tmp[:], in0=src, scalar=mv[:, 0:1], in1=g_t[:],
                                                       op0=ALU.subtract, op1=ALU.mult),
                 reads=[b_src, b_mv, b_par], writes=[b_tmp])
            S.op(ACT, lambda e: e.activation(out=tmp[:], in_=tmp[:], func=AF.Identity, scale=rs[:, 0:1]),
                 reads=[b_tmp, b_rs], writes=[b_tmp])
            S.op(POOL, lambda e: e.tensor_tensor(out=dst, in0=tmp[:], in1=b_t[:], op=ALU.add),
                 reads=[b_tmp, b_par], writes=[b_dst])

        def layer_norm(sc, src, b_src, g_t, b_t, b_par, dst, b_dst):
            stats = ln_stats(sc, src, b_src)
            ln_apply(sc, src, b_src, stats, g_t, b_t, b_par, dst, b_dst)

        def ln_scratch(alloc, pfx, n=2):
            return {"st": Ring(alloc, pfx + "st", [128, 2, 6], F32, n), "mv": Ring(alloc, pfx + "mv", [128, 2], F32, n),
                    "rs": Ring(alloc, pfx + "rs", [128, 1], F32, n), "tmp": Ring(alloc, pfx + "tmp", [128, D], F32, 2)}

        def transpose_bf(src_t, b_src, dst_ap, b_dst, evac):
            pt, b_pt = psT.next()
            S.group(PE, [(lambda e, k=k: e.transpose(out=pt[:, k, :], in_=src_t[:, k * 128:(k + 1) * 128], identity=ident[:]))
                         for k in range(8)], reads=[b_src, b_const], writes=[b_pt])
            if evac is ACT:
                S.op(ACT, lambda e: e.copy(out=dst_ap, in_=pt[:]), reads=[b_pt], writes=[b_dst])
            else:
                S.op(DVE, lambda e: e.tensor_copy(out=dst_ap, in_=pt[:]), reads=[b_pt], writes=[b_dst])

        def mm8(ps_ap, lhs_fn, rhs_fn):
            return [(lambda e, k=k: e.matmul(ps_ap, lhsT=lhs_fn(k), rhs=rhs_fn(k), start=(k == 0), stop=(k == 7)))
                    for k in range(8)]

        def load_w_cast(dst, src, buf, dname="w"):
            S.dma(POOL, dname, lambda e: e.dma_start(out=dst, in_=src), writes=[buf])

        with ExitStack() as sa:
            def sba(name, shape, dt):
                return sa.enter_context(nc.sbuf_tensor(name, shape, dt))

            hT = sba("hT", [128, 8, S_TOK], BF16)
            hT_b = [Buf() for _ in range(NT)]

            with ExitStack() as p1:
                def sb1(name, shape, dt):
                    return p1.enter_context(nc.sbuf_tensor(name, shape, dt))

                g_in = sb1("g_in", [128, D], F32)
                bb_in = sb1("bb_in", [128, D], F32)
                b_par1 = Buf()
                bcast_load(g_in[:], ln_in_g, b_par1)
                bcast_load(bb_in[:], ln_in_b, b_par1)
                xr = Ring(sb1, "x1", [128, D], F32, 2)
                hbr = Ring(sb1, "hb1", [128, D], BF16, 2)
                hfr = Ring(sb1, "hf1", [128, D], F32, 3)
                sc1 = ln_scratch(sb1, "l1", 3)
                for t in range(NT):
                    xt, b_xt = xr.next()
                    S.dma(SP, "x", lambda e: e.dma_start(out=xt[:], in_=x_d[t * 128:(t + 1) * 128, :]), writes=[b_xt])
                    hf, b_hf = hfr.next()
                    layer_norm(sc1, xt[:], b_xt, g_in, bb_in, b_par1, hf[:], b_hf)
                    S.dma(POOL, "st", lambda e: e.dma_start(out=Hd[t * 128:(t + 1) * 128, :], in_=hf[:]), reads=[b_hf])
                    hb, b_hb = hbr.next()
                    S.op(ACT, lambda e: e.copy(out=hb[:], in_=hf[:]), reads=[b_hf], writes=[b_hb])
                    transpose_bf(hb, b_hb, hT[:, :, t * 128:(t + 1) * 128], hT_b[t], DVE if t % 2 else ACT)
                S.barrier()

            if STOP_AFTER >= 2:
              with ExitStack() as p2:
                def sb2(name, shape, dt):
                    return p2.enter_context(nc.sbuf_tensor(name, shape, dt))

                S.dma(SP, "st", lambda e: e.dma_start(out=HTd[:, :, :], in_=hT[:]), reads=hT_b)
                Esel = sb2("Esel", [128, 32, 128], BF16)
                PB = sb2("PB", [128, 32, 32], F32)
                C1 = sb2("C1", [128, 32, 32], F32)
                C2 = sb2("C2", [128, 32, 32], F32)
                zt = sb2("zt", [128, 32, 32], F32)
                b_c2 = Buf()
                S.op(POOL, lambda e: e.memset(Esel[:], 1.0), writes=[b_c2])
                S.op(POOL, lambda e: e.affine_select(out=Esel[:], in_=Esel[:], pattern=[[-1, 32], [0, 128]],
                                                     compare_op=ALU.is_equal, fill=0.0, base=0, channel_multiplier=1),
                     reads=[b_c2], writes=[b_c2])
                S.op(POOL, lambda e: e.memset(zt[:], 0.0), writes=[b_c2])
                blkpat = [[1, 16], [0, 2], [-1, 16], [0, 2]]
                S.op(POOL, lambda e: e.affine_select(out=PB[:], in_=zt[:], pattern=blkpat, compare_op=ALU.is_ge,
                                                     fill=-1e30, base=-1, channel_multiplier=0), reads=[b_c2], writes=[b_c2])
                S.op(POOL, lambda e: e.affine_select(out=C2[:], in_=zt[:], pattern=blkpat, compare_op=ALU.is_equal,
                                                     fill=-30000.0, base=0, channel_multiplier=0), reads=[b_c2], writes=[b_c2])
                S.op(POOL, lambda e: e.affine_select(out=C2[:], in_=C2[:], pattern=[[1, 32], [-1, 32]], compare_op=ALU.is_ge,
                                                     fill=-30000.0, base=0, channel_multiplier=0), reads=[b_c2], writes=[b_c2])
                S.op(POOL, lambda e: e.memset(zt[:], 30000.0), reads=[b_c2], writes=[b_c2])
                S.op(POOL, lambda e: e.affine_select(out=C1[:], in_=zt[:], pattern=blkpat, compare_op=ALU.is_ge,
                                                     fill=0.0, base=-1, channel_multiplier=0), reads=[b_c2], writes=[b_c2])

                wqkv_r = Ring(sb2, "wqkv", [128, 3, 8, 128], BF16, 3)
                NB2 = 2
                qT_l = [sb2("qT%d" % i, [128, S_TOK], BF16) for i in range(NB2)]
                kT_l = [sb2("kT%d" % i, [128, S_TOK], BF16) for i in range(NB2)]
                Vt_l = [sb2("Vt%d" % i, [128, NT, 128], BF16) for i in range(NB2)]
                bT_l = [sb2("biasT%d" % i, [128, S_TOK], BF16) for i in range(NB2)]
                qT_bl = [[Buf() for _ in range(8)] for _ in range(NB2)]
                kT_bl = [[Buf() for _ in range(8)] for _ in range(NB2)]
                V_bl = [[Buf() for _ in range(8)] for _ in range(NB2)]
                bias_bl = [[Buf() for _ in range(8)] for _ in range(NB2)]
                for i in range(NB2):
                    S.op(POOL, lambda e, i=i: e.memset(bT_l[i][:], 0.0), writes=bias_bl[i])
                kmf = sb2("kmf", [128, 16], F32)
                km2 = sb2("km2", [128, 32], BF16)
                b_km = Buf()
                gm = sb2("gm", [128, 32, 32], F32)
                b_gm = Buf()
                m8 = sb2("m8", [128, 32, 8], F32)
                b_m8 = Buf()
                pT_r = Ring(sb2, "pT", [128, 512], BF16, 4)
                rinv_r = Ring(sb2, "rinv", [128, 512], F32, 2)
                osb_r = Ring(sb2, "osb", [128, 512], F32, 2)
                ybt_r = Ring(sb2, "ybt", [128, 512], BF16, 2)
                qscale = float(128 ** -0.5)

                wq_of = {}

                def load_w(hd):
                    wq, b_wq = wqkv_r.next()
                    for i in range(3):
                        c0 = 2048 + i * 1024 + hd * 128
                        load_w_cast(wq[:, i, :, :], win_v[:, :, c0:c0 + 128], b_wq, "w")
                    wq_of[hd] = (wq, b_wq)

                def prep_head(hd):
                    sl = hd % NB2
                    qT, kT, Vt, biasT = qT_l[sl], kT_l[sl], Vt_l[sl], bT_l[sl]
                    qT_b, kT_b, V_b, bias_b = qT_bl[sl], kT_bl[sl], V_bl[sl], bias_bl[sl]
                    wq, b_wq = wq_of[hd]
                    for gi in range(8):
                        ps, b_ps = psA.next()
                        S.group(PE, mm8(ps[:], lambda k: wq[:, 0, k, :], lambda k: hT[:, k, gi * 512:(gi + 1) * 512]),
                                reads=[b_wq] + hT_b[4 * gi:4 * gi + 4], writes=[b_ps])
                        S.op(ACT, lambda e: e.activation(out=qT[:, gi * 512:(gi + 1) * 512], in_=ps[:], func=AF.Copy,
                                                         scale=qscale), reads=[b_ps], writes=[qT_b[gi]])
                        ps, b_ps = psA.next()
                        S.group(PE, mm8(ps[:], lambda k: wq[:, 1, k, :], lambda k: hT[:, k, gi * 512:(gi + 1) * 512]),
                                reads=[b_wq] + hT_b[4 * gi:4 * gi + 4], writes=[b_ps])
                        S.op(DVE, lambda e: e.tensor_copy(out=kT[:, gi * 512:(gi + 1) * 512], in_=ps[:]),
                             reads=[b_ps], writes=[kT_b[gi]])
                    for g4 in range(8):
                        ps, b_ps = psA.next()
                        fns = []
                        for i in range(4):
                            t = 4 * g4 + i
                            fns += mm8(ps[:, i * 128:(i + 1) * 128], lambda k, t=t: hT[:, k, t * 128:(t + 1) * 128],
                                       lambda k: wq[:, 2, k, :])
                        S.group(PE, fns, reads=[b_wq] + hT_b[4 * g4:4 * g4 + 4], writes=[b_ps])
                        S.op(ACT, lambda e: e.copy(out=Vt[:, 4 * g4:4 * g4 + 4, :],
                                                   in_=ps[:].rearrange("p (a b) -> p a b", a=4)),
                             reads=[b_ps], writes=[V_b[g4]])
                    S.op(DVE, lambda e: e.tensor_reduce(out=kmf[:], in_=kT[:].rearrange("p (n l) -> p n l", l=256),
                                                        axis=AX.X, op=ALU.add), reads=kT_b, writes=[b_km])
                    S.op(DVE, lambda e: e.tensor_scalar(out=km2[:].rearrange("p (n two) -> p n two", two=2),
                                                        in0=kmf[:].unsqueeze(2).to_broadcast([128, 16, 2]),
                                                        scalar1=1.0 / 256.0, scalar2=None, op0=ALU.mult),
                         reads=[b_km], writes=[b_km])
                    for half in range(2):
                        ps, b_ps = psA.next()
                        fns = []
                        for i in range(16):
                            t = half * 16 + i
                            fns.append(lambda e, t=t, i=i: e.matmul(ps[:, i * 32:(i + 1) * 32], lhsT=qT[:, t * 128:(t + 1) * 128],
                                                                   rhs=km2[:], start=True, stop=True))
                        S.group(PE, fns, reads=[b_km] + qT_b[4 * half:4 * half + 4], writes=[b_ps])
                        S.op(DVE, lambda e: e.tensor_tensor(out=gm[:, half * 16:(half + 1) * 16, :].rearrange("p a b -> p (a b)"),
                                                            in0=ps[:],
                                                            in1=PB[:, half * 16:(half + 1) * 16, :].rearrange("p a b -> p (a b)"),
                                                            op=ALU.add), reads=[b_ps, b_c2], writes=[b_gm])
                    for t in range(NT):
                        S.op(DVE, lambda e, t=t: e.max(out=m8[:, t, :], in_=gm[:, t, :]), reads=[b_gm], writes=[b_m8])
                    S.op(DVE, lambda e: e.tensor_tensor(out=gm[:], in0=gm[:], in1=m8[:, :, 5:6].to_broadcast([128, 32, 32]),
                                                        op=ALU.is_ge), reads=[b_gm, b_m8], writes=[b_gm])
                    S.op(DVE, lambda e: e.tensor_tensor(out=gm[:], in0=gm[:], in1=C1[:], op=ALU.mult),
                         reads=[b_gm, b_c2], writes=[b_gm])
                    S.op(DVE, lambda e: e.tensor_tensor(out=gm[:], in0=gm[:], in1=C2[:], op=ALU.add),
                         reads=[b_gm, b_c2], writes=[b_gm])
                def prep_b(hd):
                    sl = hd % NB2
                    biasT, bias_b = bT_l[sl], bias_bl[sl]
                    for c in range(8):
                        ps, b_ps = psA.next()
                        S.group(PE, [(lambda e, i=i: e.transpose(out=ps[0:32, i * 128:(i + 1) * 128], in_=gm[:, 4 * c + i, :],
                                                                 identity=identf[:])) for i in range(4)],
                                reads=[b_gm, b_const], writes=[b_ps])
                        S.op(ACT, lambda e: e.copy(out=biasT[0:32, c * 512:(c + 1) * 512], in_=ps[0:32, :]),
                             reads=[b_ps], writes=[bias_b[c]])

                def main_head(hd, mid_fn=None):
                    sl = hd % NB2
                    qT, kT, Vt, biasT = qT_l[sl], kT_l[sl], Vt_l[sl], bT_l[sl]
                    qT_b, kT_b, V_b, bias_b = qT_bl[sl], kT_bl[sl], V_bl[sl], bias_bl[sl]
                    its = [(c, j) for c in range(8) for j in range(4 * c + 4)]

                    def stage_a(c, j):
                        ps, b_ps = psA.next()
                        S.group(PE, [
                            lambda e: e.matmul(ps[:], lhsT=kT[:, j * 128:(j + 1) * 128], rhs=qT[:, c * 512:(c + 1) * 512],
                                               start=True, stop=False),
                            lambda e: e.matmul(ps[:], lhsT=Esel[:, j, :], rhs=biasT[:, c * 512:(c + 1) * 512],
                                               start=False, stop=True)],
                            reads=[kT_b[j // 4], qT_b[c], bias_b[c], b_c2], writes=[b_ps])
                        pT, b_pT = pT_r.next()
                        S.op(ACT, lambda e: e.activation(out=pT[:], in_=ps[:], func=AF.Exp), reads=[b_ps], writes=[b_pT])
                        if j >= 4 * c:
                            col = (j - 4 * c) * 128
                            S.op(POOL, lambda e: e.tensor_tensor(out=pT[:, col:col + 128], in0=pT[:, col:col + 128],
                                                                 in1=tri[:], op=ALU.mult),
                                 reads=[b_pT, b_const], writes=[b_pT])
                        return pT, b_pT

                    def stage_b(c, j, pT, b_pT):
                        nj = 4 * c + 4
                        S.group(PE, [
                            lambda e: e.matmul(psO_t[:], lhsT=Vt[:, j, :], rhs=pT[:], start=(j == 0), stop=(j == nj - 1)),
                            lambda e: e.matmul(psR_t[:], lhsT=onesb[:], rhs=pT[:], start=(j == 0), stop=(j == nj - 1))],
                            reads=[V_b[j // 4], b_pT, b_const], writes=[psO_b, psR_b])
                        if j == nj - 1:
                            rinv, b_rinv = rinv_r.next()
                            osb, b_osb = osb_r.next()
                            S.op(ACT, lambda e: e.copy(out=rinv[:], in_=psR_t[:]), reads=[psR_b], writes=[b_rinv])
                            S.op(ACT, lambda e: e.copy(out=osb[:], in_=psO_t[:]), reads=[psO_b], writes=[b_osb])
                            S.op(DVE, lambda e: e.reciprocal(out=rinv[:], in_=rinv[:]), reads=[b_rinv], writes=[b_rinv])
                            ybt, b_ybt = ybt_r.next()
                            S.op(DVE, lambda e: e.tensor_tensor(out=ybt[:], in0=osb[:], in1=rinv[:], op=ALU.mult),
                                 reads=[b_osb, b_rinv], writes=[b_ybt])
                            S.dma(SP, "st", lambda e: e.dma_start(out=YBd[:, hd, c * 512:(c + 1) * 512], in_=ybt[:]),
                                  reads=[b_ybt])

                    SK = 2
                    pend = []
                    for i in range(len(its) + SK):
                        if i == 48 and mid_fn is not None:
                            mid_fn()
                        if i < len(its):
                            pend.append(stage_a(*its[i]))
                        if i >= SK:
                            stage_b(*its[i - SK], *pend[i - SK])

                load_w(0)
                load_w(1)
                prep_head(0)
                prep_b(0)
                for hd in range(8):
                    if hd + 1 < 8:
                        prep_head(hd + 1)
                    if hd + 2 < 8:
                        load_w(hd + 2)
                    main_head(hd, (lambda h=hd + 1: prep_b(h)) if hd + 1 < 8 else None)
                S.barrier()

            if STOP_AFTER >= 3:
              with ExitStack() as p3:
                def sb3(name, shape, dt):
                    return p3.enter_context(nc.sbuf_tensor(name, shape, dt))

                wb = sb3("wb", [128, 8, D], BF16)
                wgb = sb3("wgb", [128, 8, D], BF16)
                b_w3 = Buf()
                load_w_cast(wb[:], w_bb.rearrange("(k p) n -> p k n", p=128), b_w3)
                load_w_cast(wgb[:], win_v[:, :, 6144:7168], b_w3)
                ybg_r = Ring(sb3, "ybg", [128, 8, 512], BF16, 2)
                sg_r = Ring(sb3, "sg3", [128, 512], F32, 2)
                mbt_r = Ring(sb3, "mbt", [128, 8, 512], BF16, 2)
                for c in range(8):
                    ybg, b_ybg = ybg_r.next()
                    S.dma(POOL, "ld3", lambda e: e.dma_start(out=ybg[:], in_=YBd[:, :, c * 512:(c + 1) * 512]), writes=[b_ybg])
                    mbt, b_mbt = mbt_r.next()
                    for dc in range(8):
                        ps1, b_ps1 = psA.next()
                        S.group(PE, mm8(ps1[:], lambda k: wb[:, k, dc * 128:(dc + 1) * 128], lambda k: ybg[:, k, :]),
                                reads=[b_w3, b_ybg], writes=[b_ps1])
                        ps2, b_ps2 = psA.next()
                        S.group(PE, mm8(ps2[:], lambda k: wgb[:, k, dc * 128:(dc + 1) * 128],
                                        lambda k: hT[:, k, c * 512:(c + 1) * 512]),
                                reads=[b_w3] + hT_b[4 * c:4 * c + 4], writes=[b_ps2])
                        sg, b_sg = sg_r.next()
                        S.op(ACT, lambda e: e.activation(out=sg[:], in_=ps2[:], func=AF.Sigmoid), reads=[b_ps2], writes=[b_sg])
                        S.op(DVE, lambda e: e.tensor_tensor(out=mbt[:, dc, :], in0=ps1[:], in1=sg[:], op=ALU.mult),
                             reads=[b_ps1, b_sg], writes=[b_mbt])
                    S.dma(SP, "st", lambda e: e.dma_start(out=MBd[:, :, c * 512:(c + 1) * 512], in_=mbt[:]), reads=[b_mbt])
                S.barrier()

        if STOP_AFTER >= 4:
          with ExitStack() as p4:
            def sb4(name, shape, dt):
                return p4.enter_context(nc.sbuf_tensor(name, shape, dt))

            wu = sb4("wu", [128, 8, D], BF16)
            wv = sb4("wv", [128, 8, D], BF16)
            wa = sb4("wa", [128, 8, D], BF16)
            wga = sb4("wga", [128, 8, D], BF16)
            b_w4 = Buf()
            load_w_cast(wu[:], win_v[:, :, 0:1024], b_w4)
            load_w_cast(wv[:], win_v[:, :, 1024:2048], b_w4)
            load_w_cast(wa[:], w_ba.rearrange("(k p) n -> p k n", p=128), b_w4)
            load_w_cast(wga[:], win_v[:, :, 5120:6144], b_w4)
            g_in = sb4("g_in4", [128, D], F32)
            bb_in = sb4("bb_in4", [128, D], F32)
            g_gm = sb4("g_gm", [128, D], F32)
            bb_gm = sb4("bb_gm", [128, D], F32)
            bsb = sb4("bsb", [128, 4, 128], F32)
            b_par4 = Buf()
            bcast_load(g_in[:], ln_in_g, b_par4)
            bcast_load(bb_in[:], ln_in_b, b_par4)
            bcast_load(g_gm[:], gmlp_g, b_par4)
            bcast_load(bb_gm[:], gmlp_b, b_par4)
            bcast_load(bsb[:].rearrange("p g t -> p (g t)"), b_sp, b_par4)
            wsn = sb4("wsn", [128, 4, 128], F32)
            wsT = sb4("wsT", [128, 4, 128], BF16)
            b_ws = Buf()
            S.dma(SP, "const", lambda e: e.dma_start(out=wsn[:], in_=w_sp.rearrange("g t s -> t g s")), writes=[b_ws])
            ps, b_ps = psA.next()
            S.group(PE, [(lambda e, g=g: e.transpose(out=ps[:, g * 128:(g + 1) * 128], in_=wsn[:, g, :], identity=identf[:]))
                         for g in range(4)], reads=[b_ws, b_const], writes=[b_ps])
            S.op(DVE, lambda e: e.tensor_tensor(out=wsT[:], in0=ps[:].rearrange("p (g t) -> p g t", g=4),
                                                in1=tri[:].unsqueeze(1).to_broadcast([128, 4, 128]), op=ALU.mult),
                 reads=[b_ps, b_const], writes=[b_ws])

            xr = Ring(sb4, "x4", [128, D], F32, 2)
            hbr = Ring(sb4, "hb4", [128, D], BF16, 2)
            sc4 = ln_scratch(sb4, "l4")
            hTg_r = Ring(sb4, "hTg", [128, 8, 512], BF16, 2)
            uT_r = Ring(sb4, "uT", [128, 8, 512], BF16, 1)
            gv_r = Ring(sb4, "gv", [128, D], F32, 2)
            vn_r = Ring(sb4, "vn", [128, 4, D], BF16, 2)
            yat_r = Ring(sb4, "yat", [128, 8, 512], BF16, 1)
            mbg_r = Ring(sb4, "mbg", [128, 8, 512], BF16, 2)
            mgt_r = Ring(sb4, "mgt", [128, 8, 512], BF16, 2)
            sg_r = Ring(sb4, "sg4", [128, 512], F32, 2)
            t4_r = Ring(sb4, "t4", [128, 512], F32, 2)
            def prep_group(c, hTg, b_hTg):
                S.dma(POOL, "ld4h", lambda e: e.dma_start(out=hTg[:], in_=HTd[:, :, c * 512:(c + 1) * 512]), writes=[b_hTg])

            def v_tile(c, i, hTg, b_hTg, vn, b_vn):
                gv, b_gv = gv_r.next()
                for half in range(2):
                    ps, b_ps = psA.next()
                    S.group(PE, mm8(ps[:], lambda k: hTg[:, k, i * 128:(i + 1) * 128],
                                    lambda k: wv[:, k, half * 512:(half + 1) * 512]),
                            reads=[b_w4, b_hTg], writes=[b_ps])
                    S.op(ACT, lambda e: e.activation(out=gv[:, half * 512:(half + 1) * 512], in_=ps[:], func=AF.Gelu),
                         reads=[b_ps], writes=[b_gv])
                layer_norm(sc4, gv[:], b_gv, g_gm, bb_gm, b_par4, vn[:, i, :], b_vn)

            def u_stage(c, hTg, b_hTg):
                uT, b_uT = uT_r.next()
                for dc in range(8):
                    ps, b_ps = psA.next()
                    S.group(PE, mm8(ps[:], lambda k: wu[:, k, dc * 128:(dc + 1) * 128], lambda k: hTg[:, k, :]),
                            reads=[b_w4, b_hTg], writes=[b_ps])
                    S.op(ACT, lambda e: e.activation(out=uT[:, dc, :], in_=ps[:], func=AF.Gelu), reads=[b_ps], writes=[b_uT])
                return uT, b_uT

            def vs_stage(c, vn, b_vn, uT, b_uT):
                yat, b_yat = yat_r.next()
                for dc in range(8):
                    g = dc // 2
                    ps, b_ps = psA.next()
                    S.group(PE, [(lambda e, i=i: e.matmul(ps[:, i * 128:(i + 1) * 128], lhsT=vn[:, i, dc * 128:(dc + 1) * 128],
                                                          rhs=wsT[:, g, :], start=True, stop=True)) for i in range(4)],
                            reads=[b_vn, b_ws], writes=[b_ps])
                    t4, b_t4 = t4_r.next()
                    S.op(DVE, lambda e: e.tensor_tensor(out=t4[:].rearrange("p (a b) -> p a b", a=4),
                                                        in0=ps[:].rearrange("p (a b) -> p a b", a=4),
                                                        in1=bsb[:, g:g + 1, :].to_broadcast([128, 4, 128]), op=ALU.add),
                         reads=[b_ps, b_par4], writes=[b_t4])
                    S.op(POOL, lambda e: e.tensor_tensor(out=yat[:, dc, :], in0=t4[:], in1=uT[:, dc, :], op=ALU.mult),
                         reads=[b_t4, b_uT], writes=[b_yat])
                return yat, b_yat

            def zg_stage(c, dcs, yat, b_yat, hTg, b_hTg, mbg, b_mbg, mgt, b_mgt):
                for dc in dcs:
                    ps1, b_ps1 = psA.next()
                    S.group(PE, mm8(ps1[:], lambda k: wa[:, k, dc * 128:(dc + 1) * 128], lambda k: yat[:, k, :]),
                            reads=[b_w4, b_yat], writes=[b_ps1])
                    ps2, b_ps2 = psA.next()
                    S.group(PE, mm8(ps2[:], lambda k: wga[:, k, dc * 128:(dc + 1) * 128], lambda k: hTg[:, k, :]),
                            reads=[b_w4, b_hTg], writes=[b_ps2])
                    sg, b_sg = sg_r.next()
                    S.op(ACT, lambda e: e.activation(out=sg[:], in_=ps2[:], func=AF.Sigmoid), reads=[b_ps2], writes=[b_sg])
                    t4, b_t4 = t4_r.next()
                    S.op(DVE, lambda e: e.tensor_tensor(out=t4[:], in0=ps1[:], in1=sg[:], op=ALU.mult),
                         reads=[b_ps1, b_sg], writes=[b_t4])
                    S.op(POOL, lambda e: e.tensor_tensor(out=mgt[:, dc, :], in0=t4[:], in1=mbg[:, dc, :], op=ALU.add),
                         reads=[b_t4, b_mbg], writes=[b_mgt])

            cur = hTg_r.next()
            prep_group(0, *cur)
            vcur = vn_r.next()
            for i in range(4):
                v_tile(0, i, *cur, *vcur)
            for c in range(8):
                hTg, b_hTg = cur
                vn, b_vn = vcur
                mbg, b_mbg = mbg_r.next()
                S.dma(SP, "ld4", lambda e: e.dma_start(out=mbg[:], in_=MBd[:, :, c * 512:(c + 1) * 512]), writes=[b_mbg])
                nxt = hTg_r.next() if c + 1 < 8 else None
                vnxt = vn_r.next() if c + 1 < 8 else None
                if nxt is not None:
                    prep_group(c + 1, *nxt)
                uT, b_uT = u_stage(c, hTg, b_hTg)
                yat, b_yat = vs_stage(c, vn, b_vn, uT, b_uT)
                mgt, b_mgt = mgt_r.next()
                for i in range(4):
                    zg_stage(c, range(2 * i, 2 * i + 2), yat, b_yat, hTg, b_hTg, mbg, b_mbg, mgt, b_mgt)
                    if nxt is not None:
                        v_tile(c + 1, i, *nxt, *vnxt)
                S.dma(SP, "st", lambda e: e.dma_start(out=MGd[:, :, c * 512:(c + 1) * 512], in_=mgt[:]), reads=[b_mgt])
                cur, vcur = nxt, vnxt
            S.barrier()

        if STOP_AFTER >= 5:
          with ExitStack() as p5:
            def sb5(name, shape, dt):
                return p5.enter_context(nc.sbuf_tensor(name, shape, dt))

            wo = sb5("wo", [128, 8, D], BF16)
            b_w5 = Buf()
            load_w_cast(wo[:], w_o.rearrange("(k p) n -> p k n", p=128), b_w5)
            wr = sb5("wr", [128, 8, NE], F32)
            S.dma(SP, "const", lambda e: e.dma_start(out=wr[:], in_=w_rt.rearrange("(k p) n -> p k n", p=128)), writes=[b_w5])
            g_in = sb5("g_in5", [128, D], F32)
            bb_in = sb5("bb_in5", [128, D], F32)
            g_mx = sb5("g_mx", [128, D], F32)
            bb_mx = sb5("bb_mx", [128, D], F32)
            brt = sb5("brt", [128, NE], F32)
            b_par5 = Buf()
            bcast_load(g_in[:], ln_in_g, b_par5)
            bcast_load(bb_in[:], ln_in_b, b_par5)
            bcast_load(g_mx[:], ln_mix_g, b_par5)
            bcast_load(bb_mx[:], ln_mix_b, b_par5)
            bcast_load(brt[:], b_rt, b_par5)
            ebase = sb5("ebase", [128, NE], F32)
            ebi = sb5("ebi", [128, NE], I32)
            S.op(POOL, lambda e: e.iota(ebi[:], pattern=[[CAP, NE]], base=0, channel_multiplier=0), writes=[b_par5])
            S.op(DVE, lambda e: e.tensor_copy(out=ebase[:], in_=ebi[:]), reads=[b_par5], writes=[b_par5])
            invi = sb5("invi", [128, (NE * CAP // 128) * 16], I32)
            b_invi = Buf()
            S.op(POOL, lambda e: e.iota(invi[:], pattern=[[0, (NE * CAP // 128) * 16]], base=1 << 30, channel_multiplier=0),
                 writes=[b_invi])
            S.dma(SP, "invinit", lambda e: e.dma_start(out=INV.rearrange("(p a) c -> p (a c)", p=128), in_=invi[:]),
                  reads=[b_invi])
            vals_r = Ring(sb5, "vals", [128, 4, 16], I32, 3)
            carry = sb5("carry", [128, NE], F32)
            b_carry = Buf()
            S.op(DVE, lambda e: e.memset(carry[:], 0.0), writes=[b_carry])

            B5 = 2
            D5 = 3
            hres_r = Ring(sb5, "hres", [128, D], F32, 4)
            sc5 = ln_scratch(sb5, "l5", 8)
            mgl_r = Ring(sb5, "mgl", [128, 8, 512], BF16, 2)
            t2_r = Ring(sb5, "t2", [128, D], F32, 6)
            h2_r = Ring(sb5, "h2", [128, D], F32, 4)
            h2b_r = Ring(sb5, "h2b", [128, D], BF16, 8)
            h2T_r = Ring(sb5, "h2T", [128, 8, 128], F32, 3)
            sm = {n: Ring(sb5, "sm_" + n, shp, dt, 10) for n, shp, dt in [
                ("lg", [128, NE], F32), ("m8", [128, 8], F32), ("nm", [128, 1], F32), ("ew", [128, 4], F32),
                ("ss", [128, 1], F32), ("selb", [128, NE], BF16), ("pos", [128, NE], F32), ("oh", [128, NE], F32),
                ("junk", [128, NE], F32), ("idxf", [128, 4], F32), ("pp", [128, 2 * NE], F32)]}
            mgl_cur = [None, None]

            def s0(cx):
                t = cx["t"]
                c, i = divmod(t, 4)
                if i == 0:
                    mgl, b_mgl = mgl_r.next()
                    S.dma(SP, "ld5", lambda e: e.dma_start(out=mgl[:], in_=MGd[:, :, c * 512:(c + 1) * 512]), writes=[b_mgl])
                    mgl_cur[0], mgl_cur[1] = mgl, b_mgl
                cx["mgl"], cx["b_mgl"] = mgl_cur[0], mgl_cur[1]
                hres, b_hres = hres_r.next()
                S.dma(SP, "x", lambda e: e.dma_start(out=hres[:], in_=Hd[t * 128:(t + 1) * 128, :]), writes=[b_hres])
                cx["hres"], cx["b_hres"] = hres, b_hres

            def s3a(cx):
                i = cx["t"] % 4
                mgl, b_mgl = cx["mgl"], cx["b_mgl"]
                cx["ps3"] = []
                for half in range(2):
                    ps, b_ps = psA.next()
                    S.group(PE, mm8(ps[:], lambda k: mgl[:, k, i * 128:(i + 1) * 128],
                                    lambda k: wo[:, k, half * 512:(half + 1) * 512]), reads=[b_w5, b_mgl], writes=[b_ps])
                    cx["ps3"].append((ps, b_ps))

            def s3b(cx):
                hres, b_hres = cx["hres"], cx["b_hres"]
                t2, b_t2 = t2_r.next()
                for half in range(2):
                    ps, b_ps = cx["ps3"][half]
                    S.op(DVE, lambda e: e.scalar_tensor_tensor(out=t2[:, half * 512:(half + 1) * 512],
                                                               in0=hres[:, half * 512:(half + 1) * 512], scalar=ALPHA,
                                                               in1=ps[:], op0=ALU.mult, op1=ALU.add),
                         reads=[b_hres, b_ps], writes=[b_t2])
                cx["t2"], cx["b_t2"] = t2, b_t2
                cx["st2"] = ln_stats_a(sc5, t2[:], b_t2)

            def s4b(cx):
                ln_stats_b(cx["st2"])

            def s5(cx):
                t = cx["t"]
                ln_stats_c(cx["st2"])
                h2, b_h2 = h2_r.next()
                ln_apply(sc5, cx["t2"][:], cx["b_t2"], cx["st2"], g_mx, bb_mx, b_par5, h2[:], b_h2)
                cx["h2"], cx["b_h2"] = h2, b_h2

            def s5b(cx):
                t = cx["t"]
                h2, b_h2 = cx["h2"], cx["b_h2"]
                S.dma(POOL, "st", lambda e: e.dma_start(out=H2d[t * 128:(t + 1) * 128, :], in_=h2[:]), reads=[b_h2])
                h2b, b_h2b = h2b_r.next()
                S.op(ACT, lambda e: e.copy(out=h2b[:], in_=h2[:]), reads=[b_h2], writes=[b_h2b])
                cx["h2b"], cx["b_h2b"] = h2b, b_h2b
                h2T, b_h2T = h2T_r.next()
                for hf in range(2):
                    ps, b_ps = psA.next()
                    S.group(PE, [(lambda e, k=k: e.transpose(out=ps[:, k * 128:(k + 1) * 128],
                                                             in_=h2[:, (4 * hf + k) * 128:(4 * hf + k + 1) * 128],
                                                             identity=identf[:])) for k in range(4)],
                            reads=[b_h2, b_const], writes=[b_ps])
                    S.op(ACT, lambda e: e.copy(out=h2T[:, 4 * hf:4 * hf + 4, :], in_=ps[:].rearrange("p (a b) -> p a b", a=4)),
                         reads=[b_ps], writes=[b_h2T])
                cx["h2T"], cx["b_h2T"] = h2T, b_h2T

            def s6b(cx):
                h2T, b_h2T = cx["h2T"], cx["b_h2T"]
                r8 = cx["t"] % 8
                ps, b_ps = psR_t[:, r8 * NE:(r8 + 1) * NE], psR_reg[r8]
                S.group(PE, mm8(ps, lambda k: h2T[:, k, :], lambda k: wr[:, k, :]), reads=[b_h2T, b_w5], writes=[b_ps])
                cx["psr"] = (ps, b_ps)

            def s6c(cx):
                ps, b_ps = cx["psr"]
                lg, b_lg = sm["lg"].next()
                S.op(DVE, lambda e: e.tensor_tensor(out=lg[:], in0=ps, in1=brt[:], op=ALU.add),
                     reads=[b_ps, b_par5], writes=[b_lg])
                m8t, b_m8t = sm["m8"].next()
                S.op(DVE, lambda e: e.max(out=m8t[:], in_=lg[:]), reads=[b_lg], writes=[b_m8t])
                nm, b_nm = sm["nm"].next()
                S.op(DVE, lambda e: e.tensor_scalar(out=nm[:], in0=m8t[:, 0:1], scalar1=-1.0, scalar2=None, op0=ALU.mult),
                     reads=[b_m8t], writes=[b_nm])
                selb, b_selb = sm["selb"].next()
                S.op(DVE, lambda e: e.tensor_scalar(out=selb[:], in0=lg[:], scalar1=m8t[:, 3:4], scalar2=None, op0=ALU.is_ge),
                     reads=[b_lg, b_m8t], writes=[b_selb])
                cx.update(lg=lg, b_lg=b_lg, m8t=m8t, b_m8t=b_m8t, nm=nm, b_nm=b_nm, selb=selb, b_selb=b_selb)

            def s7a(cx):
                m8t, b_m8t, nm, b_nm, selb, b_selb = cx["m8t"], cx["b_m8t"], cx["nm"], cx["b_nm"], cx["selb"], cx["b_selb"]
                ew, b_ew = sm["ew"].next()
                ss, b_ss = sm["ss"].next()
                S.op(ACT, lambda e: e.activation(out=ew[:], in_=m8t[:, 0:4], func=AF.Exp, bias=nm[:, 0:1], scale=1.0,
                                                 accum_out=ss[:]), reads=[b_m8t, b_nm], writes=[b_ew, b_ss])
                r8 = cx["t"] % 8
                ps, b_ps = psO_t[:, r8 * 2 * NE:(r8 + 1) * 2 * NE], psO_reg[r8]
                S.group(PE, [lambda e: e.matmul(ps[:, 0:NE], lhsT=ustr[:], rhs=selb[:], start=True, stop=True),
                             lambda e: e.matmul(ps[:, NE:2 * NE], lhsT=onesb[:], rhs=selb[:], start=True, stop=True)],
                        reads=[b_selb, b_const], writes=[b_ps])
                cx.update(ew=ew, b_ew=b_ew, ss=ss, b_ss=b_ss, psp=(ps, b_ps))

            def s7b(cx):
                t = cx["t"]
                ew, b_ew, ss, b_ss = cx["ew"], cx["b_ew"], cx["ss"], cx["b_ss"]
                ps, b_ps = cx["psp"]
                pp, b_pp = sm["pp"].next()
                S.op(DVE, lambda e: e.tensor_copy(out=pp[:], in_=ps), reads=[b_ps], writes=[b_pp])
                S.op(DVE, lambda e: e.reciprocal(out=ss[:], in_=ss[:]), reads=[b_ss], writes=[b_ss])
                S.op(DVE, lambda e: e.tensor_scalar(out=gate_all[:, t, :], in0=ew[:], scalar1=ss[:, 0:1], scalar2=None,
                                                    op0=ALU.mult), reads=[b_ew, b_ss], writes=[b_gate])
                cx.update(pp=pp, b_pp=b_pp)

            def s8(cx):
                t = cx["t"]
                lg, b_lg, m8t, b_m8t, pp, b_pp = cx["lg"], cx["b_lg"], cx["m8t"], cx["b_m8t"], cx["pp"], cx["b_pp"]
                h2b, b_h2b = cx["h2b"], cx["b_h2b"]
                pos, b_pos = sm["pos"].next()
                S.op(DVE, lambda e: e.tensor_tensor(out=pos[:], in0=pp[:, 0:NE], in1=carry[:], op=ALU.add),
                     reads=[b_pp, b_carry], writes=[b_pos])
                S.op(DVE, lambda e: e.tensor_tensor(out=carry[:], in0=pp[:, NE:2 * NE], in1=carry[:], op=ALU.add),
                     reads=[b_pp, b_carry], writes=[b_carry])
                S.op(DVE, lambda e: e.tensor_tensor(out=pos[:], in0=pos[:], in1=ebase[:], op=ALU.add),
                     reads=[b_pos, b_par5], writes=[b_pos])
                idxf, b_idxf = sm["idxf"].next()
                for k in range(4):
                    oh, b_oh = sm["oh"].next()
                    S.op(DVE, lambda e, k=k: e.tensor_scalar(out=oh[:], in0=lg[:], scalar1=m8t[:, k:k + 1], scalar2=None,
                                                             op0=ALU.is_equal), reads=[b_lg, b_m8t], writes=[b_oh])
                    junk, b_junk = sm["junk"].next()
                    S.op(DVE, lambda e, k=k: e.scalar_tensor_tensor(out=junk[:], in0=oh[:], scalar=1.0, in1=pos[:],
                                                                    op0=ALU.mult, op1=ALU.mult, accum_out=idxf[:, k:k + 1]),
                         reads=[b_oh, b_pos], writes=[b_junk, b_idxf])
                S.op(DVE, lambda e: e.tensor_copy(out=idx_all[:, t, :], in_=idxf[:]), reads=[b_idxf], writes=[b_idx])
                for k in range(4 if not os.environ.get("MK_NOSC") else 0):
                    S.dma(POOL, "sc", lambda e, k=k: e.indirect_dma_start(
                        out=Xg[:, :], out_offset=bass.IndirectOffsetOnAxis(ap=idx_all[:, t, k:k + 1], axis=0),
                        in_=h2b[:], in_offset=None), reads=[b_h2b, b_idx])
                vals, b_vals = vals_r.next()
                S.op(POOL, lambda e: e.iota(vals[:], pattern=[[1, 4], [0, 16]], base=t * 512, channel_multiplier=4),
                     writes=[b_vals])
                for k in range(4):
                    S.dma(POOL, "sci", lambda e, k=k: e.indirect_dma_start(
                        out=INV[:, :], out_offset=bass.IndirectOffsetOnAxis(ap=idx_all[:, t, k:k + 1], axis=0),
                        in_=vals[:, k, :], in_offset=None), reads=[b_vals, b_idx, b_invi])

            _rb, _ob = Buf(), Buf()
            psR_reg = [_rb] * 8
            psO_reg = [_ob] * 8
            assert 2 * B5 <= len(psA.items)
            stages5 = [(s0,), (s3a, s3b), (s4b,), (s5,), (s5b,), (s6b,), (s6c,), (s7a,), (s7b,), (s8,)]
            nb5 = NT // B5
            batches5 = [[{"t": t} for t in range(b * B5, (b + 1) * B5)] for b in range(nb5)]
            for step in range(len(stages5) + D5 * (nb5 - 1)):
                for b in range(nb5):
                    k = step - D5 * b
                    if 0 <= k < len(stages5):
                        for fn in stages5[k]:
                            for cx in batches5[b]:
                                fn(cx)
            if DEBUG:
                S.dma(SP, "st", lambda e: e.dma_start(out=IDXd[:, :], in_=idx_all[:].rearrange("p a b -> p (a b)")), reads=[b_idx])
                S.dma(SP, "st", lambda e: e.dma_start(out=GATd[:, :], in_=gate_all[:].rearrange("p a b -> p (a b)")), reads=[b_gate])
            S.barrier()

        if STOP_AFTER >= 6:
          with ExitStack() as p6:
            def sb6(name, shape, dt):
                return p6.enter_context(nc.sbuf_tensor(name, shape, dt))

            bun = sb6("bun", [NE, 2 * D], F32)
            bu_all = sb6("bu_all", [128, 16, NE], F32)
            b_bu = Buf()
            S.dma(SP, "const", lambda e: e.dma_start(out=bun[:], in_=b_up[:, :]), writes=[b_bu])
            for q4 in range(4):
                ps, b_ps = psA.next()
                S.group(PE, [(lambda e, i=i: e.transpose(out=ps[:, i * NE:(i + 1) * NE],
                                                         in_=bun[:, (4 * q4 + i) * 128:(4 * q4 + i + 1) * 128],
                                                         identity=identf[0:NE, 0:NE])) for i in range(4)],
                        reads=[b_bu, b_const], writes=[b_ps])
                S.op(DVE, lambda e: e.tensor_copy(out=bu_all[:, 4 * q4:4 * q4 + 4, :],
                                                  in_=ps[:, 0:4 * NE].rearrange("p (a b) -> p a b", a=4)),
                     reads=[b_ps], writes=[b_bu])
            wu_r = Ring(sb6, "wup", [128, 8, 2 * D], BF16, 2)
            wd_r = Ring(sb6, "wdn", [128, 8, D], BF16, 2)
            bd_r = Ring(sb6, "bdn", [128, D], F32, 2)
            xs_r = Ring(sb6, "xs", [128, D], BF16, 6)
            XT_r = Ring(sb6, "XT", [128, 8, CAP], BF16, 2)
            aT_r = Ring(sb6, "aT", [128, 8, CAP], BF16, 1)
            HW = CAP // 2
            gb_r = Ring(sb6, "gb", [128, HW], F32, 2)
            sg_r = Ring(sb6, "sg6", [128, HW], F32, 2)
            ub_r = Ring(sb6, "ub", [128, HW], F32, 2)
            ys_r = Ring(sb6, "ys", [128, D], F32, 3)
            inv_r = Ring(sb6, "invt", [128, 1], I32, 6)
            bc_reg = nc.gpsimd.to_reg(S_TOK * 4 - 1)

            def load_expert(e_):
                wu_t, b_wu = wu_r.next()
                wd_t, b_wd = wd_r.next()
                bd_t, b_bd = bd_r.next()
                load_w_cast(wu_t[:], w_up[e_].rearrange("(k p) n -> p k n", p=128), b_wu, "we")
                load_w_cast(wd_t[:], w_dn[e_].rearrange("(k p) n -> p k n", p=128), b_wd, "we")
                S.dma(SP, "bd", lambda e: e.dma_start(out=bd_t[:], in_=b_dn[e_].partition_broadcast(128)), writes=[b_bd])
                return (wu_t, b_wu, wd_t, b_wd, bd_t, b_bd)

            def load_x_dma(e_):
                tiles = []
                for s_ in range(NSL):
                    xs, b_xs = xs_r.next()
                    r0 = e_ * CAP + s_ * 128
                    S.dma(SP, "xs", lambda e: e.dma_start(out=xs[:], in_=Xg[r0:r0 + 128, :]), writes=[b_xs])
                    tiles.append((xs, b_xs))
                return tiles

            def load_x_tile(tiles, s_, XT, b_XT):
                xs, b_xs = tiles[s_]
                transpose_bf(xs, b_xs, XT[:, :, s_ * 128:(s_ + 1) * 128], b_XT, DVE if s_ % 2 else ACT)

            def load_x(e_):
                XT, b_XT = XT_r.next()
                tiles = load_x_dma(e_)
                for s_ in range(NSL):
                    load_x_tile(tiles, s_, XT, b_XT)
                return XT, b_XT

            def up_proj(ex, wts, XT, b_XT):
                wu_t, b_wu = wts[0], wts[1]
                aT, b_aT = aT_r.next()
                for cc in range(8):
                    for hf in range(2):
                        sl = slice(hf * HW, (hf + 1) * HW)
                        psg_, b_psg = psA.next()
                        S.group(PE, mm8(psg_[:, 0:HW], lambda k: wu_t[:, k, cc * 128:(cc + 1) * 128], lambda k: XT[:, k, sl]),
                                reads=[b_wu, b_XT], writes=[b_psg])
                        psu_, b_psu = psA.next()
                        S.group(PE, mm8(psu_[:, 0:HW], lambda k: wu_t[:, k, D + cc * 128:D + (cc + 1) * 128],
                                        lambda k: XT[:, k, sl]), reads=[b_wu, b_XT], writes=[b_psu])
                        gb, b_gb = gb_r.next()
                        S.op(ACT, lambda e: e.activation(out=gb[:], in_=psg_[:, 0:HW], func=AF.Identity,
                                                         bias=bu_all[:, cc, ex:ex + 1], scale=1.0),
                             reads=[b_psg, b_bu], writes=[b_gb])
                        ub, b_ub = ub_r.next()
                        S.op(ACT, lambda e: e.activation(out=ub[:], in_=psu_[:, 0:HW], func=AF.Identity,
                                                         bias=bu_all[:, 8 + cc, ex:ex + 1], scale=1.0),
                             reads=[b_psu, b_bu], writes=[b_ub])
                        S.op(DVE, lambda e: e.tensor_scalar(out=gb[:], in0=gb[:], scalar1=7.0, scalar2=None, op0=ALU.min),
                             reads=[b_gb], writes=[b_gb])
                        sg, b_sg = sg_r.next()
                        S.op(ACT, lambda e: e.activation(out=sg[:], in_=gb[:], func=AF.Sigmoid, scale=1.702),
                             reads=[b_gb], writes=[b_sg])
                        S.op(POOL, lambda e: e.tensor_tensor(out=sg[:], in0=gb[:], in1=sg[:], op=ALU.mult),
                             reads=[b_gb, b_sg], writes=[b_sg])
                        S.op(DVE, lambda e: e.tensor_scalar(out=ub[:], in0=ub[:], scalar1=7.0, scalar2=-7.0, op0=ALU.min,
                                                            op1=ALU.max), reads=[b_ub], writes=[b_ub])
                        S.op(DVE, lambda e: e.scalar_tensor_tensor(out=aT[:, cc, sl], in0=ub[:], scalar=1.0, in1=sg[:],
                                                                   op0=ALU.add, op1=ALU.mult),
                             reads=[b_ub, b_sg], writes=[b_aT])
                return aT, b_aT

            def down_proj(ex, wts, aT, b_aT, xnext=None):
                wd_t, b_wd, bd_t, b_bd = wts[2], wts[3], wts[4], wts[5]
                invs = []
                for s_ in range(NSL):
                    it, b_it = inv_r.next()
                    r0 = ex * CAP + s_ * 128
                    S.dma(SP, "inv", lambda e: e.dma_start(out=it[:], in_=INV[r0:r0 + 128, 0:1], allow_slow_non_contiguous=True), writes=[b_it])
                    invs.append((it, b_it))
                for s_ in range(NSL):
                    if xnext is not None:
                        load_x_tile(xnext[0], s_, xnext[1], xnext[2])
                    ys, b_ys = ys_r.next()
                    for hf in range(2):
                        ps, b_ps = psA.next()
                        S.group(PE, mm8(ps[:], lambda k: aT[:, k, s_ * 128:(s_ + 1) * 128],
                                        lambda k: wd_t[:, k, hf * 512:(hf + 1) * 512]), reads=[b_aT, b_wd], writes=[b_ps])
                        S.op(DVE, lambda e: e.tensor_tensor(out=ys[:, hf * 512:(hf + 1) * 512], in0=ps[:],
                                                            in1=bd_t[:, hf * 512:(hf + 1) * 512], op=ALU.add),
                             reads=[b_ps, b_bd], writes=[b_ys])
                    it, b_it = invs[s_]
                    S.dma(POOL, "st", lambda e: e.indirect_dma_start(
                        out=YT[:, :], out_offset=bass.IndirectOffsetOnAxis(ap=it[:, :], axis=0),
                        in_=ys[:], in_offset=None, bounds_check=bc_reg, oob_is_err=False), reads=[b_ys, b_it])

            wts = load_expert(0)
            XTc = load_x(0)
            for ex in range(NE):
                wts_n = load_expert(ex + 1) if ex + 1 < NE else None
                aTc = up_proj(ex, wts, *XTc)
                XTn, xnext = None, None
                if ex + 1 < NE:
                    XTn = XT_r.next()
                    xnext = (load_x_dma(ex + 1), XTn[0], XTn[1])
                down_proj(ex, wts, *aTc, xnext=xnext)
                wts, XTc = wts_n, XTn
            S.barrier()

        if STOP_AFTER >= 7:
          with ExitStack() as p7:
            def sb7(name, shape, dt):
                return p7.enter_context(nc.sbuf_tensor(name, shape, dt))

            g_ff = sb7("g_ff", [128, D], F32)
            bb_ff = sb7("bb_ff", [128, D], F32)
            b_par7 = Buf()
            bcast_load(g_ff[:], ln_ffn_g, b_par7)
            bcast_load(bb_ff[:], ln_ffn_b, b_par7)
            B7 = 4
            yk_r = Ring(sb7, "yk", [128, 4, D], F32, B7)
            h2l_r = Ring(sb7, "h2l", [128, D], F32, B7)
            acc_r = Ring(sb7, "acc", [128, D], F32, B7)
            o_r = Ring(sb7, "o7", [128, D], F32, B7)
            sc7 = ln_scratch(sb7, "l7", B7 + 1)

            def c0(cx):
                t = cx["t"]
                yk, b_yk = yk_r.next()
                S.dma(SP if t % 2 else POOL, "ga", lambda e: e.dma_start(
                    out=yk[:], in_=YT[t * 512:(t + 1) * 512, :].rearrange("(p k) d -> p k d", k=4)), writes=[b_yk])
                h2l, b_h2l = h2l_r.next()
                S.dma(POOL, "ld7", lambda e: e.dma_start(out=h2l[:], in_=H2d[t * 128:(t + 1) * 128, :]), writes=[b_h2l])
                cx.update(yk=yk, b_yk=b_yk, h2l=h2l, b_h2l=b_h2l)

            def c1(cx):
                t = cx["t"]
                yk, b_yk, h2l, b_h2l = cx["yk"], cx["b_yk"], cx["h2l"], cx["b_h2l"]
                acc, b_acc = acc_r.next()
                S.op(ACT, lambda e: e.mul(out=acc[:], in_=h2l[:], mul=ALPHA), reads=[b_h2l], writes=[b_acc])
                for k in range(4 if not os.environ.get('MK_NOSTT') else 1):
                    S.op(DVE, lambda e, k=k: e.scalar_tensor_tensor(out=acc[:], in0=yk[:, k, :], scalar=gate_all[:, t, k:k + 1],
                                                                    in1=acc[:], op0=ALU.mult, op1=ALU.add),
                         reads=[b_yk, b_gate, b_acc], writes=[b_acc])
                cx.update(acc=acc, b_acc=b_acc)

            def c2(cx):
                cx["st"] = ln_stats(sc7, cx["acc"][:], cx["b_acc"])

            def c3(cx):
                t = cx["t"]
                ot, b_ot = o_r.next()
                ln_apply(sc7, cx["acc"][:], cx["b_acc"], cx["st"], g_ff, bb_ff, b_par7, ot[:], b_ot)
                S.dma(SP, "out", lambda e: e.dma_start(out=out_d[t * 128:(t + 1) * 128, :], in_=ot[:]), reads=[b_ot])

            for b0 in range(0, NT, B7):
                cxs = [{"t": t} for t in range(b0, b0 + B7)]
                for st_fn in [c0, c1, c2, c3]:
                    for cx in cxs:
                        st_fn(cx)
            S.barrier()

        S.barrier()
        waited = S.waited
    return nc, waited


_INPUT_ORDER = ["x", "ln_in_g", "ln_in_b", "w_in", "gmlp_ln_g", "gmlp_ln_b", "w_spatial", "b_spatial", "w_branch_a",
                "w_branch_b", "w_out", "ln_mix_g", "ln_mix_b", "w_router", "b_router", "w_up", "b_up", "w_down",
                "b_down", "ln_ffn_g", "ln_ffn_b"]


def _prep_inputs(inputs):
    a = {k: np.ascontiguousarray(np.asarray(v), dtype=np.float32) for k, v in inputs.items()}
    shared = {
        "ln_in_g": a["ln_in_g"].reshape(D), "ln_in_b": a["ln_in_b"].reshape(D),
        "w_in": a["w_in"].reshape(D, 7 * D),
        "gmlp_ln_g": a["gmlp_ln_g"].reshape(D), "gmlp_ln_b": a["gmlp_ln_b"].reshape(D),
        "w_spatial": a["w_spatial"].reshape(4, 128, 128), "b_spatial": a["b_spatial"].reshape(512),
        "w_branch_a": a["w_branch_a"].reshape(D, D), "w_branch_b": a["w_branch_b"].reshape(D, D),
        "w_out": a["w_out"].reshape(D, D),
        "ln_mix_g": a["ln_mix_g"].reshape(D), "ln_mix_b": a["ln_mix_b"].reshape(D),
        "w_router": a["w_router"].reshape(D, NE), "b_router": a["b_router"].reshape(NE),
        "w_up": a["w_up"].reshape(NE, D, 2 * D), "b_up": a["b_up"].reshape(NE, 2 * D),
        "w_down": a["w_down"].reshape(NE, D, D), "b_down": a["b_down"].reshape(NE, D),
        "ln_ffn_g": a["ln_ffn_g"].reshape(D), "ln_ffn_b": a["ln_ffn_b"].reshape(D),
    }
    return a["x"], shared


def kernel(**inputs):
    x, shared = _prep_inputs(inputs)
    n = x.shape[0]
    nc = build_program()
    in_maps = []
    for b in range(n):
        m = dict(shared)
        m["x"] = np.ascontiguousarray(x[b])
        in_maps.append(m)
    res = run_bass_kernel_spmd(nc, in_maps, core_ids=list(range(n)))
    out = np.stack([np.asarray(r["out"]) for r in res.results], axis=0).astype(np.float32)
    return out
```

```python
import os
import bisect
import numpy as np
from contextlib import ExitStack
import concourse.bass as bass
import concourse.mybir as mybir
from concourse.bass_utils import run_bass_kernel_spmd

F32 = mybir.dt.float32
BF16 = mybir.dt.bfloat16
I32 = mybir.dt.int32
AF = mybir.ActivationFunctionType
ALU = mybir.AluOpType
AX = mybir.AxisListType

S_TOK = 4096
D = 1024
NT = 32
NE = 32
CAP = 768
NSL = CAP // 128
ALPHA = float(2 ** 0.25)
EPS = 1e-5
DEBUG = bool(int(os.environ.get("MK_DEBUG", "0")))
STOP_AFTER = int(os.environ.get("MK_STOP", "99"))


class Buf:
    __slots__ = ("w", "r")

    def __init__(self):
        self.w = None
        self.r = []


class Eng:
    def __init__(self, nc, eng, name):
        self.eng = eng
        self.name = name
        self.sem = nc.alloc_semaphore(name=name)
        self.count = 0
        self.seen = {}


class Sync:
    def __init__(self, nc, waitsets=None):
        self.nc = nc
        self.waitsets = waitsets
        self.waited = {}
        self.pe = Eng(nc, nc.tensor, "s_pe")
        self.act = Eng(nc, nc.scalar, "s_act")
        self.dve = Eng(nc, nc.vector, "s_dve")
        self.pool = Eng(nc, nc.gpsimd, "s_pool")
        self.sp = Eng(nc, nc.sync, "s_sp")
        self.engs = [self.pe, self.act, self.dve, self.pool, self.sp]
        self.dsems = {}
        self._keep = []
        self.wsets = {k: set(v) for k, v in (waitsets or {}).items()}

    def dsem(self, key):
        k = id(key)
        if k not in self.dsems:
            self.dsems[k] = Eng(self.nc, None, "d_%d" % len(self.dsems))
            self._keep.append(key)
        return self.dsems[k]

    def _wait(self, E, reads, writes):
        deps = {}

        def add(tok):
            if tok is None:
                return
            s, v = tok
            if deps.get(s, 0) < v:
                deps[s] = v

        for t in reads:
            add(t.w)
        for t in writes:
            add(t.w)
            for r in t.r:
                add(r)
        for s, v in deps.items():
            if E.seen.get(s, 0) < v:
                E.eng.wait_ge(s.sem, self._val(s, v))
                E.seen[s] = v

    def _val(self, s, v):
        if s.eng is None:
            return v
        if self.waitsets is None:
            self.waited.setdefault(s.name, set()).add(v)
            return v
        return bisect.bisect_right(self.waitsets[s.name], v)

    def _signals(self, E):
        return self.waitsets is None or E.count in self.wsets.get(E.name, ())

    def _done(self, tok, reads, writes):
        for t in reads:
            t.r.append(tok)
            if len(t.r) > 64:
                best = {}
                for s, v in t.r:
                    if best.get(s, 0) < v:
                        best[s] = v
                t.r = list(best.items())
        for t in writes:
            t.w = tok
            t.r = []

    def op(self, E, fn, reads=(), writes=()):
        self._wait(E, reads, writes)
        inst = fn(E.eng)
        E.count += 1
        if self._signals(E):
            inst.then_inc(E.sem, 1)
        self._done((E, E.count), reads, writes)

    def group(self, E, fns, reads=(), writes=()):
        self._wait(E, reads, writes)
        inst = None
        for fn in fns:
            inst = fn(E.eng)
        E.count += 1
        if self._signals(E):
            inst.then_inc(E.sem, 1)
        self._done((E, E.count), reads, writes)

    def dma(self, Q, dname, fn, reads=(), writes=()):
        key = writes[0] if len(writes) else reads[0]
        Dm = self.dsem(key)
        self._wait(Q, reads, writes)
        inst = fn(Q.eng)
        Dm.count += 16
        inst.then_inc(Dm.sem, 16)
        self._done((Dm, Dm.count), reads, writes)

    def barrier(self):
        allq = self.engs + list(self.dsems.values())
        for E in self.engs:
            for X in allq:
                if X is E or X.count == 0:
                    continue
                if E.seen.get(X, 0) < X.count:
                    E.eng.wait_ge(X.sem, self._val(X, X.count))
                    E.seen[X] = X.count


class Ring:
    def __init__(self, alloc, name, shape, dt, n):
        self.items = [(alloc("%s_%d" % (name, i), shape, dt), Buf()) for i in range(n)]
        self.i = 0

    def next(self):
        it = self.items[self.i % len(self.items)]
        self.i += 1
        return it


def build_program():
    _, waited = _build(None)
    nc, _ = _build({k: sorted(v) for k, v in waited.items()})
    return nc


def _build(waitsets):
    nc = bass.Bass("TRN2", target_bir_lowering=False)

    def din(name, shape, dt=F32):
        return nc.dram_tensor(name, list(shape), dt, kind="ExternalInput").ap()

    x_d = din("x", [S_TOK, D])
    ln_in_g = din("ln_in_g", [D])
    ln_in_b = din("ln_in_b", [D])
    w_in = din("w_in", [D, 7 * D])
    gmlp_g = din("gmlp_ln_g", [D])
    gmlp_b = din("gmlp_ln_b", [D])
    w_sp = din("w_spatial", [4, 128, 128])
    b_sp = din("b_spatial", [512])
    w_ba = din("w_branch_a", [D, D])
    w_bb = din("w_branch_b", [D, D])
    w_o = din("w_out", [D, D])
    ln_mix_g = din("ln_mix_g", [D])
    ln_mix_b = din("ln_mix_b", [D])
    w_rt = din("w_router", [D, NE])
    b_rt = din("b_router", [NE])
    w_up = din("w_up", [NE, D, 2 * D])
    b_up = din("b_up", [NE, 2 * D])
    w_dn = din("w_down", [NE, D, D])
    b_dn = din("b_down", [NE, D])
    ln_ffn_g = din("ln_ffn_g", [D])
    ln_ffn_b = din("ln_ffn_b", [D])
    out_d = nc.dram_tensor("out", [S_TOK, D], F32, kind="ExternalOutput").ap()

    dbgkind = "ExternalOutput" if DEBUG else "Internal"
    YBd = nc.dram_tensor("ybd", [128, 8, S_TOK], BF16, kind=dbgkind).ap()
    MBd = nc.dram_tensor("mbd", [128, 8, S_TOK], BF16, kind="Internal").ap()
    MGd = nc.dram_tensor("mgd", [128, 8, S_TOK], BF16, kind=dbgkind).ap()
    H2d = nc.dram_tensor("h2d", [S_TOK, D], F32, kind=dbgkind).ap()
    Xg = nc.dram_tensor("xg", [NE * CAP, D], BF16, kind="Internal").ap()
    Hd = nc.dram_tensor("hd", [S_TOK, D], F32, kind="Internal").ap()
    INV = nc.dram_tensor("inv", [NE * CAP, 16], I32, kind="Internal").ap()
    YT = nc.dram_tensor("yt", [S_TOK * 4, D], F32, kind="Internal").ap()
    HTd = nc.dram_tensor("htd", [128, 8, S_TOK], BF16, kind="Internal").ap()
    Yg = nc.dram_tensor("yg", [NE * CAP, D], F32, kind="Internal").ap()
    if DEBUG:
        IDXd = nc.dram_tensor("idxd", [128, NT * 4], I32, kind="ExternalOutput").ap()
        GATd = nc.dram_tensor("gatd", [128, NT * 4], F32, kind="ExternalOutput").ap()

    win_v = w_in.rearrange("(k p) n -> p k n", p=128)

    with ExitStack() as es:
        def sbg(name, shape, dt):
            return es.enter_context(nc.sbuf_tensor(name, shape, dt))

        def psg(name, shape, dt):
            return es.enter_context(nc.psum_tensor(name, shape, dt))

        S = Sync(nc, waitsets)
        PE, ACT, DVE, POOL, SP = S.pe, S.act, S.dve, S.pool, S.sp

        psT = Ring(psg, "psT", [128, 8, 128], BF16, 2)
        psA = Ring(psg, "psA", [128, 512], F32, 4)
        psO_t = psg("psO", [128, 512], F32)
        psO_b = Buf()
        psR_t = psg("psR", [128, 512], F32)
        psR_b = Buf()

        onesf = sbg("onesf", [128, 128], F32)
        identf = sbg("identf", [128, 128], F32)
        ident = sbg("ident", [128, 128], BF16)
        onesb = sbg("onesb", [128, 128], BF16)
        tri = sbg("tri", [128, 128], BF16)
        ustr = sbg("ustr", [128, 128], BF16)
        idx_all = sbg("idx_all", [128, NT, 4], I32)
        gate_all = sbg("gate_all", [128, NT, 4], F32)
        b_const = Buf()
        b_idx = Buf()
        b_gate = Buf()
        S.op(POOL, lambda e: e.memset(onesf[:], 1.0), writes=[b_const])
        S.op(POOL, lambda e: e.memset(onesb[:], 1.0), writes=[b_const])
        S.op(POOL, lambda e: e.affine_select(out=identf[:], in_=onesf[:], pattern=[[-1, 128]], compare_op=ALU.is_equal,
                                             fill=0.0, base=0, channel_multiplier=1), reads=[b_const], writes=[b_const])
        S.op(POOL, lambda e: e.affine_select(out=ident[:], in_=onesf[:], pattern=[[-1, 128]], compare_op=ALU.is_equal,
                                             fill=0.0, base=0, channel_multiplier=1), reads=[b_const], writes=[b_const])
        S.op(POOL, lambda e: e.affine_select(out=tri[:], in_=onesf[:], pattern=[[1, 128]], compare_op=ALU.is_ge,
                                             fill=0.0, base=0, channel_multiplier=-1), reads=[b_const], writes=[b_const])
        S.op(POOL, lambda e: e.affine_select(out=ustr[:], in_=onesf[:], pattern=[[1, 128]], compare_op=ALU.is_ge,
                                             fill=0.0, base=-1, channel_multiplier=-1), reads=[b_const], writes=[b_const])

        def bcast_load(dst, src1d, buf):
            S.dma(SP, "const", lambda e: e.dma_start(out=dst, in_=src1d.partition_broadcast(128)), writes=[buf])

        def ln_stats_a(sc, src, b_src):
            st, b_st = sc["st"].next()
            mv, b_mv = sc["mv"].next()
            rs, b_rs = sc["rs"].next()
            for i in range(2):
                S.op(DVE, lambda e, i=i: e.bn_stats(out=st[:, i, :], in_=src[:, i * 512:(i + 1) * 512]),
                     reads=[b_src], writes=[b_st])
            S.op(DVE, lambda e: e.bn_aggr(out=mv[:], in_=st[:].rearrange("p a b -> p (a b)")), reads=[b_st], writes=[b_mv])
            S.op(DVE, lambda e: e.tensor_scalar(out=rs[:], in0=mv[:, 1:2], scalar1=EPS, scalar2=None, op0=ALU.add),
                 reads=[b_mv], writes=[b_rs])
            return (mv, b_mv, rs, b_rs)

        def ln_stats_b(stats):
            mv, b_mv, rs, b_rs = stats
            S.op(ACT, lambda e: e.activation(out=rs[:], in_=rs[:], func=AF.Sqrt), reads=[b_rs], writes=[b_rs])

        def ln_stats_c(stats):
            mv, b_mv, rs, b_rs = stats
            S.op(DVE, lambda e: e.reciprocal(out=rs[:], in_=rs[:]), reads=[b_rs], writes=[b_rs])

        def ln_stats(sc, src, b_src):
            stats = ln_stats_a(sc, src, b_src)
            ln_stats_b(stats)
            ln_stats_c(stats)
            return stats

        def ln_apply(sc, src, b_src, stats, g_t, b_t, b_par, dst, b_dst):
            mv, b_mv, rs, b_rs = stats
            tmp, b_tmp = sc["tmp"].next()
            S.op(DVE, lambda e: e.scalar_tensor_tensor(out=tmp[:], in0=src, scalar=mv[:, 0:1], in1=g_t[:],
                                                       op0=ALU.subtract, op1=ALU.mult),
                 reads=[b_src, b_mv, b_par], writes=[b_tmp])
            S.op(DVE, lambda e: e.scalar_tensor_tensor(out=dst, in0=tmp[:], scalar=rs[:, 0:1], in1=b_t[:],
                                                       op0=ALU.mult, op1=ALU.add),
                 reads=[b_tmp, b_rs, b_par], writes=[b_dst])

        def ln_apply_split(sc, src, b_src, stats, g_t, b_t, b_par, dst, b_dst):
            mv, b_mv, rs, b_rs = stats
            tmp, b_tmp = sc["tmp"].next()
            S.op(DVE, lambda e: e.scalar_tensor_tensor(out=tmp[:], in0=src, scalar=mv[:, 0:1], in1=g_t[:],
                                                       op0=ALU.subtract, op1=ALU.mult),
                 reads=[b_src, b_mv, b_par], writes=[b_tmp])
            S.op(ACT, lambda e: e.activation(out=tmp[:], in_=tmp[:], func=AF.Identity, scale=rs[:, 0:1]),
                 reads=[b_tmp, b_rs], writes=[b_tmp])
            S.op(POOL, lambda e: e.tensor_tensor(out=dst, in0=tmp[:], in1=b_t[:], op=ALU.add),
                 reads=[b_tmp, b_par], writes=[b_dst])

        def layer_norm(sc, src, b_src, g_t, b_t, b_par, dst, b_dst):
            stats = ln_stats(sc, src, b_src)
            ln_apply(sc, src, b_src, stats, g_t, b_t, b_par, dst, b_dst)

        def ln_scratch(alloc, pfx, n=2):
            return {"st": Ring(alloc, pfx + "st", [128, 2, 6], F32, n), "mv": Ring(alloc, pfx + "mv", [128, 2], F32, n),
                    "rs": Ring(alloc, pfx + "rs", [128, 1], F32, n), "tmp": Ring(alloc, pfx + "tmp", [128, D], F32, 2)}

        def transpose_bf(src_t, b_src, dst_ap, b_dst, evac):
            pt, b_pt = psT.next()
            S.group(PE, [(lambda e, k=k: e.transpose(out=pt[:, k, :], in_=src_t[:, k * 128:(k + 1) * 128], identity=ident[:]))
                         for k in range(8)], reads=[b_src, b_const], writes=[b_pt])
            if evac is ACT:
                S.op(ACT, lambda e: e.copy(out=dst_ap, in_=pt[:]), reads=[b_pt], writes=[b_dst])
            else:
                S.op(DVE, lambda e: e.tensor_copy(out=dst_ap, in_=pt[:]), reads=[b_pt], writes=[b_dst])

        def mm8(ps_ap, lhs_fn, rhs_fn):
            return [(lambda e, k=k: e.matmul(ps_ap, lhsT=lhs_fn(k), rhs=rhs_fn(k), start=(k == 0), stop=(k == 7)))
                    for k in range(8)]

        def load_w_cast(dst, src, buf, dname="w"):
            S.dma(POOL, dname, lambda e: e.dma_start(out=dst, in_=src), writes=[buf])

        with ExitStack() as sa:
            def sba(name, shape, dt):
                return sa.enter_context(nc.sbuf_tensor(name, shape, dt))

            hT = sba("hT", [128, 8, S_TOK], BF16)
            hT_b = [Buf() for _ in range(NT)]

            with ExitStack() as p1:
                def sb1(name, shape, dt):
                    return p1.enter_context(nc.sbuf_tensor(name, shape, dt))

                g_in = sb1("g_in", [128, D], F32)
                bb_in = sb1("bb_in", [128, D], F32)
                b_par1 = Buf()
                bcast_load(g_in[:], ln_in_g, b_par1)
                bcast_load(bb_in[:], ln_in_b, b_par1)
                xr = Ring(sb1, "x1", [128, D], F32, 2)
                hbr = Ring(sb1, "hb1", [128, D], BF16, 2)
                hfr = Ring(sb1, "hf1", [128, D], F32, 3)
                sc1 = ln_scratch(sb1, "l1", 3)
                for t in range(NT):
                    xt, b_xt = xr.next()
                    S.dma(SP, "x", lambda e: e.dma_start(out=xt[:], in_=x_d[t * 128:(t + 1) * 128, :]), writes=[b_xt])
                    hf, b_hf = hfr.next()
                    layer_norm(sc1, xt[:], b_xt, g_in, bb_in, b_par1, hf[:], b_hf)
                    S.dma(POOL, "st", lambda e: e.dma_start(out=Hd[t * 128:(t + 1) * 128, :], in_=hf[:]), reads=[b_hf])
                    hb, b_hb = hbr.next()
                    S.op(ACT, lambda e: e.copy(out=hb[:], in_=hf[:]), reads=[b_hf], writes=[b_hb])
                    transpose_bf(hb, b_hb, hT[:, :, t * 128:(t + 1) * 128], hT_b[t], DVE if t % 2 else ACT)
                S.barrier()

            if STOP_AFTER >= 2:
              with ExitStack() as p2:
                def sb2(name, shape, dt):
                    return p2.enter_context(nc.sbuf_tensor(name, shape, dt))

                S.dma(SP, "st", lambda e: e.dma_start(out=HTd[:, :, :], in_=hT[:]), reads=hT_b)
                Esel = sb2("Esel", [128, 32, 128], BF16)
                PB = sb2("PB", [128, 32, 32], F32)
                C1 = sb2("C1", [128, 32, 32], F32)
                C2 = sb2("C2", [128, 32, 32], F32)
                zt = sb2("zt", [128, 32, 32], F32)
                b_c2 = Buf()
                S.op(POOL, lambda e: e.memset(Esel[:], 1.0), writes=[b_c2])
                S.op(POOL, lambda e: e.affine_select(out=Esel[:], in_=Esel[:], pattern=[[-1, 32], [0, 128]],
                                                     compare_op=ALU.is_equal, fill=0.0, base=0, channel_multiplier=1),
                     reads=[b_c2], writes=[b_c2])
                S.op(POOL, lambda e: e.memset(zt[:], 0.0), writes=[b_c2])
                blkpat = [[1, 16], [0, 2], [-1, 16], [0, 2]]
                S.op(POOL, lambda e: e.affine_select(out=PB[:], in_=zt[:], pattern=blkpat, compare_op=ALU.is_ge,
                                                     fill=-1e30, base=-1, channel_multiplier=0), reads=[b_c2], writes=[b_c2])
                S.op(POOL, lambda e: e.affine_select(out=C2[:], in_=zt[:], pattern=blkpat, compare_op=ALU.is_equal,
                                                     fill=-30000.0, base=0, channel_multiplier=0), reads=[b_c2], writes=[b_c2])
                S.op(POOL, lambda e: e.affine_select(out=C2[:], in_=C2[:], pattern=[[1, 32], [-1, 32]], compare_op=ALU.is_ge,
                                                     fill=-30000.0, base=0, channel_multiplier=0), reads=[b_c2], writes=[b_c2])
                S.op(POOL, lambda e: e.memset(zt[:], 30000.0), reads=[b_c2], writes=[b_c2])
                S.op(POOL, lambda e: e.affine_select(out=C1[:], in_=zt[:], pattern=blkpat, compare_op=ALU.is_ge,
                                                     fill=0.0, base=-1, channel_multiplier=0), reads=[b_c2], writes=[b_c2])

                wqkv_r = Ring(sb2, "wqkv", [128, 3, 8, 128], BF16, 3)
                NB2 = 2
                qT_l = [sb2("qT%d" % i, [128, S_TOK], BF16) for i in range(NB2)]
                kT_l = [sb2("kT%d" % i, [128, S_TOK], BF16) for i in range(NB2)]
                Vt_l = [sb2("Vt%d" % i, [128, NT, 128], BF16) for i in range(NB2)]
                bT_l = [sb2("biasT%d" % i, [128, S_TOK], BF16) for i in range(NB2)]
                qT_bl = [[Buf() for _ in range(8)] for _ in range(NB2)]
                kT_bl = [[Buf() for _ in range(8)] for _ in range(NB2)]
                V_bl = [[Buf() for _ in range(8)] for _ in range(NB2)]
                bias_bl = [[Buf() for _ in range(8)] for _ in range(NB2)]
                for i in range(NB2):
                    S.op(POOL, lambda e, i=i: e.memset(bT_l[i][:], 0.0), writes=bias_bl[i])
                kmf = sb2("kmf", [128, 16], F32)
                km2 = sb2("km2", [128, 32], BF16)
                b_km = Buf()
                gm = sb2("gm", [128, 32, 32], F32)
                b_gm = Buf()
                m8 = sb2("m8", [128, 32, 8], F32)
                b_m8 = Buf()
                pT_r = Ring(sb2, "pT", [128, 512], BF16, 4)
                rinv_r = Ring(sb2, "rinv", [128, 512], F32, 2)
                osb_r = Ring(sb2, "osb", [128, 512], F32, 2)
                ybt_r = Ring(sb2, "ybt", [128, 512], BF16, 2)
                qscale = float(128 ** -0.5)

                wq_of = {}

                def load_w(hd):
                    wq, b_wq = wqkv_r.next()
                    for i in range(3):
                        c0 = 2048 + i * 1024 + hd * 128
                        load_w_cast(wq[:, i, :, :], win_v[:, :, c0:c0 + 128], b_wq, "w")
                    wq_of[hd] = (wq, b_wq)

                def prep_head(hd):
                    sl = hd % NB2
                    qT, kT, Vt, biasT = qT_l[sl], kT_l[sl], Vt_l[sl], bT_l[sl]
                    qT_b, kT_b, V_b, bias_b = qT_bl[sl], kT_bl[sl], V_bl[sl], bias_bl[sl]
                    wq, b_wq = wq_of[hd]
                    for gi in range(8):
                        ps, b_ps = psA.next()
                        S.group(PE, mm8(ps[:], lambda k: wq[:, 0, k, :], lambda k: hT[:, k, gi * 512:(gi + 1) * 512]),
                                reads=[b_wq] + hT_b[4 * gi:4 * gi + 4], writes=[b_ps])
                        S.op(ACT, lambda e: e.activation(out=qT[:, gi * 512:(gi + 1) * 512], in_=ps[:], func=AF.Copy,
                                                         scale=qscale), reads=[b_ps], writes=[qT_b[gi]])
                        ps, b_ps = psA.next()
                        S.group(PE, mm8(ps[:], lambda k: wq[:, 1, k, :], lambda k: hT[:, k, gi * 512:(gi + 1) * 512]),
                                reads=[b_wq] + hT_b[4 * gi:4 * gi + 4], writes=[b_ps])
                        S.op(DVE, lambda e: e.tensor_copy(out=kT[:, gi * 512:(gi + 1) * 512], in_=ps[:]),
                             reads=[b_ps], writes=[kT_b[gi]])
                    for g4 in range(8):
                        ps, b_ps = psA.next()
                        fns = []
                        for i in range(4):
                            t = 4 * g4 + i
                            fns += mm8(ps[:, i * 128:(i + 1) * 128], lambda k, t=t: hT[:, k, t * 128:(t + 1) * 128],
                                       lambda k: wq[:, 2, k, :])
                        S.group(PE, fns, reads=[b_wq] + hT_b[4 * g4:4 * g4 + 4], writes=[b_ps])
                        S.op(ACT, lambda e: e.copy(out=Vt[:, 4 * g4:4 * g4 + 4, :],
                                                   in_=ps[:].rearrange("p (a b) -> p a b", a=4)),
                             reads=[b_ps], writes=[V_b[g4]])
                    S.op(DVE, lambda e: e.tensor_reduce(out=kmf[:], in_=kT[:].rearrange("p (n l) -> p n l", l=256),
                                                        axis=AX.X, op=ALU.add), reads=kT_b, writes=[b_km])
                    S.op(DVE, lambda e: e.tensor_scalar(out=km2[:].rearrange("p (n two) -> p n two", two=2),
                                                        in0=kmf[:].unsqueeze(2).to_broadcast([128, 16, 2]),
                                                        scalar1=1.0 / 256.0, scalar2=None, op0=ALU.mult),
                         reads=[b_km], writes=[b_km])
                    for half in range(2):
                        ps, b_ps = psA.next()
                        fns = []
                        for i in range(16):
                            t = half * 16 + i
                            fns.append(lambda e, t=t, i=i: e.matmul(ps[:, i * 32:(i + 1) * 32], lhsT=qT[:, t * 128:(t + 1) * 128],
                                                                   rhs=km2[:], start=True, stop=True))
                        S.group(PE, fns, reads=[b_km] + qT_b[4 * half:4 * half + 4], writes=[b_ps])
                        S.op(DVE, lambda e: e.tensor_tensor(out=gm[:, half * 16:(half + 1) * 16, :].rearrange("p a b -> p (a b)"),
                                                            in0=ps[:],
                                                            in1=PB[:, half * 16:(half + 1) * 16, :].rearrange("p a b -> p (a b)"),
                                                            op=ALU.add), reads=[b_ps, b_c2], writes=[b_gm])
                    for t in range(NT):
                        S.op(DVE, lambda e, t=t: e.max(out=m8[:, t, :], in_=gm[:, t, :]), reads=[b_gm], writes=[b_m8])
                    S.op(DVE, lambda e: e.tensor_tensor(out=gm[:], in0=gm[:], in1=m8[:, :, 5:6].to_broadcast([128, 32, 32]),
                                                        op=ALU.is_ge), reads=[b_gm, b_m8], writes=[b_gm])
                    S.op(DVE, lambda e: e.tensor_tensor(out=gm[:], in0=gm[:], in1=C1[:], op=ALU.mult),
                         reads=[b_gm, b_c2], writes=[b_gm])
                    S.op(DVE, lambda e: e.tensor_tensor(out=gm[:], in0=gm[:], in1=C2[:], op=ALU.add),
                         reads=[b_gm, b_c2], writes=[b_gm])
                def prep_b(hd):
                    sl = hd % NB2
                    biasT, bias_b = bT_l[sl], bias_bl[sl]
                    for c in range(8):
                        ps, b_ps = psA.next()
                        S.group(PE, [(lambda e, i=i: e.transpose(out=ps[0:32, i * 128:(i + 1) * 128], in_=gm[:, 4 * c + i, :],
                                                                 identity=identf[:])) for i in range(4)],
                                reads=[b_gm, b_const], writes=[b_ps])
                        S.op(ACT, lambda e: e.copy(out=biasT[0:32, c * 512:(c + 1) * 512], in_=ps[0:32, :]),
                             reads=[b_ps], writes=[bias_b[c]])

                def main_head(hd, mid_fn=None):
                    sl = hd % NB2
                    qT, kT, Vt, biasT = qT_l[sl], kT_l[sl], Vt_l[sl], bT_l[sl]
                    qT_b, kT_b, V_b, bias_b = qT_bl[sl], kT_bl[sl], V_bl[sl], bias_bl[sl]
                    its = [(c, j) for c in range(8) for j in range(4 * c + 4)]

                    def stage_a(c, j):
                        ps, b_ps = psA.next()
                        S.group(PE, [
                            lambda e: e.matmul(ps[:], lhsT=kT[:, j * 128:(j + 1) * 128], rhs=qT[:, c * 512:(c + 1) * 512],
                                               start=True, stop=False),
                            lambda e: e.matmul(ps[:], lhsT=Esel[:, j, :], rhs=biasT[:, c * 512:(c + 1) * 512],
                                               start=False, stop=True)],
                            reads=[kT_b[j // 4], qT_b[c], bias_b[c], b_c2], writes=[b_ps])
                        pT, b_pT = pT_r.next()
                        S.op(ACT, lambda e: e.activation(out=pT[:], in_=ps[:], func=AF.Exp), reads=[b_ps], writes=[b_pT])
                        if j >= 4 * c:
                            col = (j - 4 * c) * 128
                            S.op(POOL, lambda e: e.tensor_tensor(out=pT[:, col:col + 128], in0=pT[:, col:col + 128],
                                                                 in1=tri[:], op=ALU.mult),
                                 reads=[b_pT, b_const], writes=[b_pT])
                        return pT, b_pT

                    def stage_b(c, j, pT, b_pT):
                        nj = 4 * c + 4
                        S.group(PE, [
                            lambda e: e.matmul(psO_t[:], lhsT=Vt[:, j, :], rhs=pT[:], start=(j == 0), stop=(j == nj - 1)),
                            lambda e: e.matmul(psR_t[:], lhsT=onesb[:], rhs=pT[:], start=(j == 0), stop=(j == nj - 1))],
                            reads=[V_b[j // 4], b_pT, b_const], writes=[psO_b, psR_b])
                        if j == nj - 1:
                            rinv, b_rinv = rinv_r.next()
                            osb, b_osb = osb_r.next()
                            S.op(ACT, lambda e: e.copy(out=rinv[:], in_=psR_t[:]), reads=[psR_b], writes=[b_rinv])
                            S.op(ACT, lambda e: e.copy(out=osb[:], in_=psO_t[:]), reads=[psO_b], writes=[b_osb])
                            S.op(DVE, lambda e: e.reciprocal(out=rinv[:], in_=rinv[:]), reads=[b_rinv], writes=[b_rinv])
                            ybt, b_ybt = ybt_r.next()
                            S.op(DVE, lambda e: e.tensor_tensor(out=ybt[:], in0=osb[:], in1=rinv[:], op=ALU.mult),
                                 reads=[b_osb, b_rinv], writes=[b_ybt])
                            S.dma(SP, "st", lambda e: e.dma_start(out=YBd[:, hd, c * 512:(c + 1) * 512], in_=ybt[:]),
                                  reads=[b_ybt])

                    SK = 2
                    pend = []
                    for i in range(len(its) + SK):
                        if i == 48 and mid_fn is not None:
                            mid_fn()
                        if i < len(its):
                            pend.append(stage_a(*its[i]))
                        if i >= SK:
                            stage_b(*its[i - SK], *pend[i - SK])

                load_w(0)
                load_w(1)
                prep_head(0)
                prep_b(0)
                for hd in range(8):
                    if hd + 1 < 8:
                        prep_head(hd + 1)
                    if hd + 2 < 8:
                        load_w(hd + 2)
                    main_head(hd, (lambda h=hd + 1: prep_b(h)) if hd + 1 < 8 else None)
                S.barrier()

            if STOP_AFTER >= 3:
              with ExitStack() as p3:
                def sb3(name, shape, dt):
                    return p3.enter_context(nc.sbuf_tensor(name, shape, dt))

                wb = sb3("wb", [128, 8, D], BF16)
                wgb = sb3("wgb", [128, 8, D], BF16)
                b_w3 = Buf()
                load_w_cast(wb[:], w_bb.rearrange("(k p) n -> p k n", p=128), b_w3)
                load_w_cast(wgb[:], win_v[:, :, 6144:7168], b_w3)
                ybg_r = Ring(sb3, "ybg", [128, 8, 512], BF16, 2)
                sg_r = Ring(sb3, "sg3", [128, 512], F32, 2)
                mbt_r = Ring(sb3, "mbt", [128, 8, 512], BF16, 2)
                for c in range(8):
                    ybg, b_ybg = ybg_r.next()
                    S.dma(POOL, "ld3", lambda e: e.dma_start(out=ybg[:], in_=YBd[:, :, c * 512:(c + 1) * 512]), writes=[b_ybg])
                    mbt, b_mbt = mbt_r.next()
                    for dc in range(8):
                        ps1, b_ps1 = psA.next()
                        S.group(PE, mm8(ps1[:], lambda k: wb[:, k, dc * 128:(dc + 1) * 128], lambda k: ybg[:, k, :]),
                                reads=[b_w3, b_ybg], writes=[b_ps1])
                        ps2, b_ps2 = psA.next()
                        S.group(PE, mm8(ps2[:], lambda k: wgb[:, k, dc * 128:(dc + 1) * 128],
                                        lambda k: hT[:, k, c * 512:(c + 1) * 512]),
                                reads=[b_w3] + hT_b[4 * c:4 * c + 4], writes=[b_ps2])
                        sg, b_sg = sg_r.next()
                        S.op(ACT, lambda e: e.activation(out=sg[:], in_=ps2[:], func=AF.Sigmoid), reads=[b_ps2], writes=[b_sg])
                        S.op(DVE, lambda e: e.tensor_tensor(out=mbt[:, dc, :], in0=ps1[:], in1=sg[:], op=ALU.mult),
                             reads=[b_ps1, b_sg], writes=[b_mbt])
                    S.dma(SP, "st", lambda e: e.dma_start(out=MBd[:, :, c * 512:(c + 1) * 512], in_=mbt[:]), reads=[b_mbt])
                S.barrier()

        if STOP_AFTER >= 4:
          with ExitStack() as p4:
            def sb4(name, shape, dt):
                return p4.enter_context(nc.sbuf_tensor(name, shape, dt))

            wu = sb4("wu", [128, 8, D], BF16)
            wv = sb4("wv", [128, 8, D], BF16)
            wa = sb4("wa", [128, 8, D], BF16)
            wga = sb4("wga", [128, 8, D], BF16)
            b_w4 = Buf()
            load_w_cast(wu[:], win_v[:, :, 0:1024], b_w4)
            load_w_cast(wv[:], win_v[:, :, 1024:2048], b_w4)
            load_w_cast(wa[:], w_ba.rearrange("(k p) n -> p k n", p=128), b_w4)
            load_w_cast(wga[:], win_v[:, :, 5120:6144], b_w4)
            g_in = sb4("g_in4", [128, D], F32)
            bb_in = sb4("bb_in4", [128, D], F32)
            g_gm = sb4("g_gm", [128, D], F32)
            bb_gm = sb4("bb_gm", [128, D], F32)
            bsb = sb4("bsb", [128, 4, 128], F32)
            b_par4 = Buf()
            bcast_load(g_in[:], ln_in_g, b_par4)
            bcast_load(bb_in[:], ln_in_b, b_par4)
            bcast_load(g_gm[:], gmlp_g, b_par4)
            bcast_load(bb_gm[:], gmlp_b, b_par4)
            bcast_load(bsb[:].rearrange("p g t -> p (g t)"), b_sp, b_par4)
            wsn = sb4("wsn", [128, 4, 128], F32)
            wsT = sb4("wsT", [128, 4, 128], BF16)
            b_ws = Buf()
            S.dma(SP, "const", lambda e: e.dma_start(out=wsn[:], in_=w_sp.rearrange("g t s -> t g s")), writes=[b_ws])
            ps, b_ps = psA.next()
            S.group(PE, [(lambda e, g=g: e.transpose(out=ps[:, g * 128:(g + 1) * 128], in_=wsn[:, g, :], identity=identf[:]))
                         for g in range(4)], reads=[b_ws, b_const], writes=[b_ps])
            S.op(DVE, lambda e: e.tensor_tensor(out=wsT[:], in0=ps[:].rearrange("p (g t) -> p g t", g=4),
                                                in1=tri[:].unsqueeze(1).to_broadcast([128, 4, 128]), op=ALU.mult),
                 reads=[b_ps, b_const], writes=[b_ws])

            xr = Ring(sb4, "x4", [128, D], F32, 2)
            hbr = Ring(sb4, "hb4", [128, D], BF16, 2)
            sc4 = ln_scratch(sb4, "l4")
            hTg_r = Ring(sb4, "hTg", [128, 8, 512], BF16, 2)
            uT_r = Ring(sb4, "uT", [128, 8, 512], BF16, 1)
            gv_r = Ring(sb4, "gv", [128, D], F32, 2)
            vn_r = Ring(sb4, "vn", [128, 4, D], BF16, 2)
            yat_r = Ring(sb4, "yat", [128, 8, 512], BF16, 1)
            mbg_r = Ring(sb4, "mbg", [128, 8, 512], BF16, 2)
            mgt_r = Ring(sb4, "mgt", [128, 8, 512], BF16, 2)
            sg_r = Ring(sb4, "sg4", [128, 512], F32, 2)
            t4_r = Ring(sb4, "t4", [128, 512], F32, 2)
            def prep_group(c, hTg, b_hTg):
                S.dma(POOL, "ld4h", lambda e: e.dma_start(out=hTg[:], in_=HTd[:, :, c * 512:(c + 1) * 512]), writes=[b_hTg])

            def v_tile(c, i, hTg, b_hTg, vn, b_vn):
                gv, b_gv = gv_r.next()
                for half in range(2):
                    ps, b_ps = psA.next()
                    S.group(PE, mm8(ps[:], lambda k: hTg[:, k, i * 128:(i + 1) * 128],
                                    lambda k: wv[:, k, half * 512:(half + 1) * 512]),
                            reads=[b_w4, b_hTg], writes=[b_ps])
                    S.op(ACT, lambda e: e.activation(out=gv[:, half * 512:(half + 1) * 512], in_=ps[:], func=AF.Gelu),
                         reads=[b_ps], writes=[b_gv])
                layer_norm(sc4, gv[:], b_gv, g_gm, bb_gm, b_par4, vn[:, i, :], b_vn)

            def u_stage(c, hTg, b_hTg):
                uT, b_uT = uT_r.next()
                for dc in range(8):
                    ps, b_ps = psA.next()
                    S.group(PE, mm8(ps[:], lambda k: wu[:, k, dc * 128:(dc + 1) * 128], lambda k: hTg[:, k, :]),
                            reads=[b_w4, b_hTg], writes=[b_ps])
                    S.op(ACT, lambda e: e.activation(out=uT[:, dc, :], in_=ps[:], func=AF.Gelu), reads=[b_ps], writes=[b_uT])
                return uT, b_uT

            def vs_stage(c, vn, b_vn, uT, b_uT):
                yat, b_yat = yat_r.next()
                for dc in range(8):
                    g = dc // 2
                    ps, b_ps = psA.next()
                    S.group(PE, [(lambda e, i=i: e.matmul(ps[:, i * 128:(i + 1) * 128], lhsT=vn[:, i, dc * 128:(dc + 1) * 128],
                                                          rhs=wsT[:, g, :], start=True, stop=True)) for i in range(4)],
                            reads=[b_vn, b_ws], writes=[b_ps])
                    t4, b_t4 = t4_r.next()
                    S.op(DVE, lambda e: e.tensor_tensor(out=t4[:].rearrange("p (a b) -> p a b", a=4),
                                                        in0=ps[:].rearrange("p (a b) -> p a b", a=4),
                                                        in1=bsb[:, g:g + 1, :].to_broadcast([128, 4, 128]), op=ALU.add),
                         reads=[b_ps, b_par4], writes=[b_t4])
                    S.op(POOL, lambda e: e.tensor_tensor(out=yat[:, dc, :], in0=t4[:], in1=uT[:, dc, :], op=ALU.mult),
                         reads=[b_t4, b_uT], writes=[b_yat])
                return yat, b_yat

            def zg_stage(c, dcs, yat, b_yat, hTg, b_hTg, mbg, b_mbg, mgt, b_mgt):
                for dc in dcs:
                    ps1, b_ps1 = psA.next()
                    S.group(PE, mm8(ps1[:], lambda k: wa[:, k, dc * 128:(dc + 1) * 128], lambda k: yat[:, k, :]),
                            reads=[b_w4, b_yat], writes=[b_ps1])
                    ps2, b_ps2 = psA.next()
                    S.group(PE, mm8(ps2[:], lambda k: wga[:, k, dc * 128:(dc + 1) * 128], lambda k: hTg[:, k, :]),
                            reads=[b_w4, b_hTg], writes=[b_ps2])
                    sg, b_sg = sg_r.next()
                    S.op(ACT, lambda e: e.activation(out=sg[:], in_=ps2[:], func=AF.Sigmoid), reads=[b_ps2], writes=[b_sg])
                    t4, b_t4 = t4_r.next()
                    S.op(DVE, lambda e: e.tensor_tensor(out=t4[:], in0=ps1[:], in1=sg[:], op=ALU.mult),
                         reads=[b_ps1, b_sg], writes=[b_t4])
                    S.op(POOL, lambda e: e.tensor_tensor(out=mgt[:, dc, :], in0=t4[:], in1=mbg[:, dc, :], op=ALU.add),
                         reads=[b_t4, b_mbg], writes=[b_mgt])

            cur = hTg_r.next()
            prep_group(0, *cur)
            vcur = vn_r.next()
            for i in range(4):
                v_tile(0, i, *cur, *vcur)
            for c in range(8):
                hTg, b_hTg = cur
                vn, b_vn = vcur
                mbg, b_mbg = mbg_r.next()
                S.dma(SP, "ld4", lambda e: e.dma_start(out=mbg[:], in_=MBd[:, :, c * 512:(c + 1) * 512]), writes=[b_mbg])
                nxt = hTg_r.next() if c + 1 < 8 else None
                vnxt = vn_r.next() if c + 1 < 8 else None
                if nxt is not None:
                    prep_group(c + 1, *nxt)
                uT, b_uT = u_stage(c, hTg, b_hTg)
                yat, b_yat = vs_stage(c, vn, b_vn, uT, b_uT)
                mgt, b_mgt = mgt_r.next()
                for i in range(4):
                    zg_stage(c, range(2 * i, 2 * i + 2), yat, b_yat, hTg, b_hTg, mbg, b_mbg, mgt, b_mgt)
                    if nxt is not None:
                        v_tile(c + 1, i, *nxt, *vnxt)
                S.dma(SP, "st", lambda e: e.dma_start(out=MGd[:, :, c * 512:(c + 1) * 512], in_=mgt[:]), reads=[b_mgt])
                cur, vcur = nxt, vnxt
            S.barrier()

        if STOP_AFTER >= 5:
          with ExitStack() as p5:
            def sb5(name, shape, dt):
                return p5.enter_context(nc.sbuf_tensor(name, shape, dt))

            wo = sb5("wo", [128, 8, D], BF16)
            b_w5 = Buf()
            load_w_cast(wo[:], w_o.rearrange("(k p) n -> p k n", p=128), b_w5)
            wr = sb5("wr", [128, 8, NE], F32)
            S.dma(SP, "const", lambda e: e.dma_start(out=wr[:], in_=w_rt.rearrange("(k p) n -> p k n", p=128)), writes=[b_w5])
            g_in = sb5("g_in5", [128, D], F32)
            bb_in = sb5("bb_in5", [128, D], F32)
            g_mx = sb5("g_mx", [128, D], F32)
            bb_mx = sb5("bb_mx", [128, D], F32)
            brt = sb5("brt", [128, NE], F32)
            b_par5 = Buf()
            bcast_load(g_in[:], ln_in_g, b_par5)
            bcast_load(bb_in[:], ln_in_b, b_par5)
            bcast_load(g_mx[:], ln_mix_g, b_par5)
            bcast_load(bb_mx[:], ln_mix_b, b_par5)
            bcast_load(brt[:], b_rt, b_par5)
            ebase = sb5("ebase", [128, NE], F32)
            ebi = sb5("ebi", [128, NE], I32)
            S.op(POOL, lambda e: e.iota(ebi[:], pattern=[[CAP, NE]], base=0, channel_multiplier=0), writes=[b_par5])
            S.op(DVE, lambda e: e.tensor_copy(out=ebase[:], in_=ebi[:]), reads=[b_par5], writes=[b_par5])
            invi = sb5("invi", [128, (NE * CAP // 128) * 16], I32)
            b_invi = Buf()
            S.op(POOL, lambda e: e.iota(invi[:], pattern=[[0, (NE * CAP // 128) * 16]], base=1 << 30, channel_multiplier=0),
                 writes=[b_invi])
            S.dma(SP, "invinit", lambda e: e.dma_start(out=INV.rearrange("(p a) c -> p (a c)", p=128), in_=invi[:]),
                  reads=[b_invi])
            vals_r = Ring(sb5, "vals", [128, 4, 16], I32, 3)
            carry = sb5("carry", [128, NE], F32)
            b_carry = Buf()
            S.op(DVE, lambda e: e.memset(carry[:], 0.0), writes=[b_carry])

            B5 = 2
            D5 = 2
            hres_r = Ring(sb5, "hres", [128, D], F32, 4)
            sc5 = ln_scratch(sb5, "l5", 8)
            mgl_r = Ring(sb5, "mgl", [128, 8, 512], BF16, 2)
            t2_r = Ring(sb5, "t2", [128, D], F32, 6)
            h2_r = Ring(sb5, "h2", [128, D], F32, 4)
            h2b_r = Ring(sb5, "h2b", [128, D], BF16, 8)
            h2T_r = Ring(sb5, "h2T", [128, 8, 128], F32, 3)
            sm = {n: Ring(sb5, "sm_" + n, shp, dt, 10) for n, shp, dt in [
                ("lg", [128, NE], F32), ("m8", [128, 8], F32), ("nm", [128, 1], F32), ("ew", [128, 4], F32),
                ("ss", [128, 1], F32), ("selb", [128, NE], BF16), ("pos", [128, NE], F32), ("oh", [128, NE], F32),
                ("junk", [128, NE], F32), ("idxf", [128, 4], F32), ("pp", [128, 2 * NE], F32)]}
            mgl_cur = [None, None]

            def s0(cx):
                t = cx["t"]
                c, i = divmod(t, 4)
                if i == 0:
                    mgl, b_mgl = mgl_r.next()
                    S.dma(SP, "ld5", lambda e: e.dma_start(out=mgl[:], in_=MGd[:, :, c * 512:(c + 1) * 512]), writes=[b_mgl])
                    mgl_cur[0], mgl_cur[1] = mgl, b_mgl
                cx["mgl"], cx["b_mgl"] = mgl_cur[0], mgl_cur[1]
                hres, b_hres = hres_r.next()
                S.dma(SP, "x", lambda e: e.dma_start(out=hres[:], in_=Hd[t * 128:(t + 1) * 128, :]), writes=[b_hres])
                cx["hres"], cx["b_hres"] = hres, b_hres

            def s3a(cx):
                i = cx["t"] % 4
                mgl, b_mgl = cx["mgl"], cx["b_mgl"]
                cx["ps3"] = []
                for half in range(2):
                    ps, b_ps = psA.next()
                    S.group(PE, mm8(ps[:], lambda k: mgl[:, k, i * 128:(i + 1) * 128],
                                    lambda k: wo[:, k, half * 512:(half + 1) * 512]), reads=[b_w5, b_mgl], writes=[b_ps])
                    cx["ps3"].append((ps, b_ps))

            def s3b(cx):
                hres, b_hres = cx["hres"], cx["b_hres"]
                t2, b_t2 = t2_r.next()
                for half in range(2):
                    ps, b_ps = cx["ps3"][half]
                    S.op(DVE, lambda e: e.scalar_tensor_tensor(out=t2[:, half * 512:(half + 1) * 512],
                                                               in0=hres[:, half * 512:(half + 1) * 512], scalar=ALPHA,
                                                               in1=ps[:], op0=ALU.mult, op1=ALU.add),
                         reads=[b_hres, b_ps], writes=[b_t2])
                cx["t2"], cx["b_t2"] = t2, b_t2
                cx["st2"] = ln_stats_a(sc5, t2[:], b_t2)

            def s4b(cx):
                ln_stats_b(cx["st2"])

            def s5(cx):
                t = cx["t"]
                ln_stats_c(cx["st2"])
                h2, b_h2 = h2_r.next()
                ln_apply(sc5, cx["t2"][:], cx["b_t2"], cx["st2"], g_mx, bb_mx, b_par5, h2[:], b_h2)
                cx["h2"], cx["b_h2"] = h2, b_h2

            def s5b(cx):
                t = cx["t"]
                h2, b_h2 = cx["h2"], cx["b_h2"]
                S.dma(ACT, "st", lambda e: e.dma_start(out=H2d[t * 128:(t + 1) * 128, :], in_=h2[:]), reads=[b_h2])
                h2b, b_h2b = h2b_r.next()
                S.op(ACT, lambda e: e.copy(out=h2b[:], in_=h2[:]), reads=[b_h2], writes=[b_h2b])
                cx["h2b"], cx["b_h2b"] = h2b, b_h2b
                h2T, b_h2T = h2T_r.next()
                for hf in range(2):
                    ps, b_ps = psA.next()
                    S.group(PE, [(lambda e, k=k: e.transpose(out=ps[:, k * 128:(k + 1) * 128],
                                                             in_=h2[:, (4 * hf + k) * 128:(4 * hf + k + 1) * 128],
                                                             identity=identf[:])) for k in range(4)],
                            reads=[b_h2, b_const], writes=[b_ps])
                    S.op(ACT, lambda e: e.copy(out=h2T[:, 4 * hf:4 * hf + 4, :], in_=ps[:].rearrange("p (a b) -> p a b", a=4)),
                         reads=[b_ps], writes=[b_h2T])
                cx["h2T"], cx["b_h2T"] = h2T, b_h2T

            def s6b(cx):
                h2T, b_h2T = cx["h2T"], cx["b_h2T"]
                r8 = cx["t"] % 8
                ps, b_ps = psR_t[:, r8 * NE:(r8 + 1) * NE], psR_reg[r8]
                S.group(PE, mm8(ps, lambda k: h2T[:, k, :], lambda k: wr[:, k, :]), reads=[b_h2T, b_w5], writes=[b_ps])
                cx["psr"] = (ps, b_ps)

            def s6c(cx):
                ps, b_ps = cx["psr"]
                lg, b_lg = sm["lg"].next()
                S.op(DVE, lambda e: e.tensor_tensor(out=lg[:], in0=ps, in1=brt[:], op=ALU.add),
                     reads=[b_ps, b_par5], writes=[b_lg])
                m8t, b_m8t = sm["m8"].next()
                S.op(DVE, lambda e: e.max(out=m8t[:], in_=lg[:]), reads=[b_lg], writes=[b_m8t])
                nm, b_nm = sm["nm"].next()
                S.op(DVE, lambda e: e.tensor_scalar(out=nm[:], in0=m8t[:, 0:1], scalar1=-1.0, scalar2=None, op0=ALU.mult),
                     reads=[b_m8t], writes=[b_nm])
                selb, b_selb = sm["selb"].next()
                S.op(DVE, lambda e: e.tensor_scalar(out=selb[:], in0=lg[:], scalar1=m8t[:, 3:4], scalar2=None, op0=ALU.is_ge),
                     reads=[b_lg, b_m8t], writes=[b_selb])
                cx.update(lg=lg, b_lg=b_lg, m8t=m8t, b_m8t=b_m8t, nm=nm, b_nm=b_nm, selb=selb, b_selb=b_selb)

            def s7a(cx):
                m8t, b_m8t, nm, b_nm, selb, b_selb = cx["m8t"], cx["b_m8t"], cx["nm"], cx["b_nm"], cx["selb"], cx["b_selb"]
                ew, b_ew = sm["ew"].next()
                ss, b_ss = sm["ss"].next()
                S.op(ACT, lambda e: e.activation(out=ew[:], in_=m8t[:, 0:4], func=AF.Exp, bias=nm[:, 0:1], scale=1.0,
                                                 accum_out=ss[:]), reads=[b_m8t, b_nm], writes=[b_ew, b_ss])
                r8 = cx["t"] % 8
                ps, b_ps = psO_t[:, r8 * 2 * NE:(r8 + 1) * 2 * NE], psO_reg[r8]
                S.group(PE, [lambda e: e.matmul(ps[:, 0:NE], lhsT=ustr[:], rhs=selb[:], start=True, stop=True),
                             lambda e: e.matmul(ps[:, NE:2 * NE], lhsT=onesb[:], rhs=selb[:], start=True, stop=True)],
                        reads=[b_selb, b_const], writes=[b_ps])
                cx.update(ew=ew, b_ew=b_ew, ss=ss, b_ss=b_ss, psp=(ps, b_ps))

            def s7b(cx):
                t = cx["t"]
                ew, b_ew, ss, b_ss = cx["ew"], cx["b_ew"], cx["ss"], cx["b_ss"]
                ps, b_ps = cx["psp"]
                pp, b_pp = sm["pp"].next()
                S.op(DVE, lambda e: e.tensor_copy(out=pp[:], in_=ps), reads=[b_ps], writes=[b_pp])
                S.op(DVE, lambda e: e.reciprocal(out=ss[:], in_=ss[:]), reads=[b_ss], writes=[b_ss])
                S.op(DVE, lambda e: e.tensor_scalar(out=gate_all[:, t, :], in0=ew[:], scalar1=ss[:, 0:1], scalar2=None,
                                                    op0=ALU.mult), reads=[b_ew, b_ss], writes=[b_gate])
                cx.update(pp=pp, b_pp=b_pp)

            def s8(cx):
                t = cx["t"]
                lg, b_lg, m8t, b_m8t, pp, b_pp = cx["lg"], cx["b_lg"], cx["m8t"], cx["b_m8t"], cx["pp"], cx["b_pp"]
                h2b, b_h2b = cx["h2b"], cx["b_h2b"]
                pos, b_pos = sm["pos"].next()
                S.op(DVE, lambda e: e.tensor_tensor(out=pos[:], in0=pp[:, 0:NE], in1=carry[:], op=ALU.add),
                     reads=[b_pp, b_carry], writes=[b_pos])
                S.op(DVE, lambda e: e.tensor_tensor(out=carry[:], in0=pp[:, NE:2 * NE], in1=carry[:], op=ALU.add),
                     reads=[b_pp, b_carry], writes=[b_carry])
                S.op(DVE, lambda e: e.tensor_tensor(out=pos[:], in0=pos[:], in1=ebase[:], op=ALU.add),
                     reads=[b_pos, b_par5], writes=[b_pos])
                idxf, b_idxf = sm["idxf"].next()
                for k in range(4):
                    oh, b_oh = sm["oh"].next()
                    S.op(DVE, lambda e, k=k: e.tensor_scalar(out=oh[:], in0=lg[:], scalar1=m8t[:, k:k + 1], scalar2=None,
                                                             op0=ALU.is_equal), reads=[b_lg, b_m8t], writes=[b_oh])
                    junk, b_junk = sm["junk"].next()
                    S.op(DVE, lambda e, k=k: e.scalar_tensor_tensor(out=junk[:], in0=oh[:], scalar=1.0, in1=pos[:],
                                                                    op0=ALU.mult, op1=ALU.mult, accum_out=idxf[:, k:k + 1]),
                         reads=[b_oh, b_pos], writes=[b_junk, b_idxf])
                S.op(DVE, lambda e: e.tensor_copy(out=idx_all[:, t, :], in_=idxf[:]), reads=[b_idxf], writes=[b_idx])
                for k in range(4 if not os.environ.get("MK_NOSC") else 0):
                    S.dma(POOL, "sc", lambda e, k=k: e.indirect_dma_start(
                        out=Xg[:, :], out_offset=bass.IndirectOffsetOnAxis(ap=idx_all[:, t, k:k + 1], axis=0),
                        in_=h2b[:], in_offset=None), reads=[b_h2b, b_idx])
                vals, b_vals = vals_r.next()
                S.op(POOL, lambda e: e.iota(vals[:], pattern=[[1, 4], [0, 16]], base=t * 512, channel_multiplier=4),
                     writes=[b_vals])
                for k in range(4):
                    S.dma(POOL, "sci", lambda e, k=k: e.indirect_dma_start(
                        out=INV[:, :], out_offset=bass.IndirectOffsetOnAxis(ap=idx_all[:, t, k:k + 1], axis=0),
                        in_=vals[:, k, :], in_offset=None), reads=[b_vals, b_idx, b_invi])

            _rb, _ob = Buf(), Buf()
            psR_reg = [_rb] * 8
            psO_reg = [_ob] * 8
            assert 2 * B5 <= len(psA.items)
            stages5 = [(s0,), (s3a, s3b), (s4b,), (s5,), (s5b,), (s6b,), (s6c,), (s7a,), (s7b,), (s8,)]
            nb5 = NT // B5
            batches5 = [[{"t": t} for t in range(b * B5, (b + 1) * B5)] for b in range(nb5)]
            for step in range(len(stages5) + D5 * (nb5 - 1)):
                for b in range(nb5):
                    k = step - D5 * b
                    if 0 <= k < len(stages5):
                        for fn in stages5[k]:
                            for cx in batches5[b]:
                                fn(cx)
            if DEBUG:
                S.dma(SP, "st", lambda e: e.dma_start(out=IDXd[:, :], in_=idx_all[:].rearrange("p a b -> p (a b)")), reads=[b_idx])
                S.dma(SP, "st", lambda e: e.dma_start(out=GATd[:, :], in_=gate_all[:].rearrange("p a b -> p (a b)")), reads=[b_gate])
            S.barrier()

        if STOP_AFTER >= 6:
          with ExitStack() as p6:
            def sb6(name, shape, dt):
                return p6.enter_context(nc.sbuf_tensor(name, shape, dt))

            bun = sb6("bun", [NE, 2 * D], F32)
            bu_all = sb6("bu_all", [128, 16, NE], F32)
            b_bu = Buf()
            S.dma(SP, "const", lambda e: e.dma_start(out=bun[:], in_=b_up[:, :]), writes=[b_bu])
            for q4 in range(4):
                ps, b_ps = psA.next()
                S.group(PE, [(lambda e, i=i: e.transpose(out=ps[:, i * NE:(i + 1) * NE],
                                                         in_=bun[:, (4 * q4 + i) * 128:(4 * q4 + i + 1) * 128],
                                                         identity=identf[0:NE, 0:NE])) for i in range(4)],
                        reads=[b_bu, b_const], writes=[b_ps])
                S.op(DVE, lambda e: e.tensor_copy(out=bu_all[:, 4 * q4:4 * q4 + 4, :],
                                                  in_=ps[:, 0:4 * NE].rearrange("p (a b) -> p a b", a=4)),
                     reads=[b_ps], writes=[b_bu])
            wu_r = Ring(sb6, "wup", [128, 8, 2 * D], BF16, 2)
            wd_r = Ring(sb6, "wdn", [128, 8, D], BF16, 2)
            bd_r = Ring(sb6, "bdn", [128, D], F32, 2)
            xs_r = Ring(sb6, "xs", [128, D], BF16, 6)
            XT_r = Ring(sb6, "XT", [128, 8, CAP], BF16, 2)
            aT_r = Ring(sb6, "aT", [128, 8, CAP], BF16, 1)
            HW = CAP // 2
            gb_r = Ring(sb6, "gb", [128, HW], F32, 2)
            sg_r = Ring(sb6, "sg6", [128, HW], F32, 2)
            ub_r = Ring(sb6, "ub", [128, HW], F32, 2)
            ys_r = Ring(sb6, "ys", [128, D], F32, 3)
            inv_r = Ring(sb6, "invt", [128, 1], I32, 6)
            bc_reg = nc.gpsimd.to_reg(S_TOK * 4 - 1)

            def load_expert(e_):
                wu_t, b_wu = wu_r.next()
                wd_t, b_wd = wd_r.next()
                bd_t, b_bd = bd_r.next()
                load_w_cast(wu_t[:], w_up[e_].rearrange("(k p) n -> p k n", p=128), b_wu, "we")
                load_w_cast(wd_t[:], w_dn[e_].rearrange("(k p) n -> p k n", p=128), b_wd, "we")
                S.dma(SP, "bd", lambda e: e.dma_start(out=bd_t[:], in_=b_dn[e_].partition_broadcast(128)), writes=[b_bd])
                return (wu_t, b_wu, wd_t, b_wd, bd_t, b_bd)

            def load_x_dma(e_):
                tiles = []
                for s_ in range(NSL):
                    xs, b_xs = xs_r.next()
                    r0 = e_ * CAP + s_ * 128
                    S.dma(SP, "xs", lambda e: e.dma_start(out=xs[:], in_=Xg[r0:r0 + 128, :]), writes=[b_xs])
                    tiles.append((xs, b_xs))
                return tiles

            def load_x_tile(tiles, s_, XT, b_XT):
                xs, b_xs = tiles[s_]
                transpose_bf(xs, b_xs, XT[:, :, s_ * 128:(s_ + 1) * 128], b_XT, DVE if s_ % 2 else ACT)

            def load_x(e_):
                XT, b_XT = XT_r.next()
                tiles = load_x_dma(e_)
                for s_ in range(NSL):
                    load_x_tile(tiles, s_, XT, b_XT)
                return XT, b_XT

            def up_proj(ex, wts, XT, b_XT):
                wu_t, b_wu = wts[0], wts[1]
                aT, b_aT = aT_r.next()
                for cc in range(8):
                    for hf in range(2):
                        sl = slice(hf * HW, (hf + 1) * HW)
                        psg_, b_psg = psA.next()
                        S.group(PE, mm8(psg_[:, 0:HW], lambda k: wu_t[:, k, cc * 128:(cc + 1) * 128], lambda k: XT[:, k, sl]),
                                reads=[b_wu, b_XT], writes=[b_psg])
                        psu_, b_psu = psA.next()
                        S.group(PE, mm8(psu_[:, 0:HW], lambda k: wu_t[:, k, D + cc * 128:D + (cc + 1) * 128],
                                        lambda k: XT[:, k, sl]), reads=[b_wu, b_XT], writes=[b_psu])
                        gb, b_gb = gb_r.next()
                        S.op(ACT, lambda e: e.activation(out=gb[:], in_=psg_[:, 0:HW], func=AF.Identity,
                                                         bias=bu_all[:, cc, ex:ex + 1], scale=1.0),
                             reads=[b_psg, b_bu], writes=[b_gb])
                        ub, b_ub = ub_r.next()
                        S.op(ACT, lambda e: e.activation(out=ub[:], in_=psu_[:, 0:HW], func=AF.Identity,
                                                         bias=bu_all[:, 8 + cc, ex:ex + 1], scale=1.0),
                             reads=[b_psu, b_bu], writes=[b_ub])
                        S.op(DVE, lambda e: e.tensor_scalar(out=gb[:], in0=gb[:], scalar1=7.0, scalar2=None, op0=ALU.min),
                             reads=[b_gb], writes=[b_gb])
                        sg, b_sg = sg_r.next()
                        S.op(ACT, lambda e: e.activation(out=sg[:], in_=gb[:], func=AF.Sigmoid, scale=1.702),
                             reads=[b_gb], writes=[b_sg])
                        S.op(POOL, lambda e: e.tensor_tensor(out=sg[:], in0=gb[:], in1=sg[:], op=ALU.mult),
                             reads=[b_gb, b_sg], writes=[b_sg])
                        S.op(DVE, lambda e: e.tensor_scalar(out=ub[:], in0=ub[:], scalar1=7.0, scalar2=-7.0, op0=ALU.min,
                                                            op1=ALU.max), reads=[b_ub], writes=[b_ub])
                        S.op(DVE, lambda e: e.scalar_tensor_tensor(out=aT[:, cc, sl], in0=ub[:], scalar=1.0, in1=sg[:],
                                                                   op0=ALU.add, op1=ALU.mult),
                             reads=[b_ub, b_sg], writes=[b_aT])
                return aT, b_aT

            def down_proj(ex, wts, aT, b_aT, xnext=None):
                wd_t, b_wd, bd_t, b_bd = wts[2], wts[3], wts[4], wts[5]
                invs = []
                for s_ in range(NSL):
                    it, b_it = inv_r.next()
                    r0 = ex * CAP + s_ * 128
                    S.dma(SP, "inv", lambda e: e.dma_start(out=it[:], in_=INV[r0:r0 + 128, 0:1], allow_slow_non_contiguous=True), writes=[b_it])
                    invs.append((it, b_it))
                for s_ in range(NSL):
                    if xnext is not None:
                        load_x_tile(xnext[0], s_, xnext[1], xnext[2])
                    ys, b_ys = ys_r.next()
                    for hf in range(2):
                        ps, b_ps = psA.next()
                        S.group(PE, mm8(ps[:], lambda k: aT[:, k, s_ * 128:(s_ + 1) * 128],
                                        lambda k: wd_t[:, k, hf * 512:(hf + 1) * 512]), reads=[b_aT, b_wd], writes=[b_ps])
                        S.op(DVE, lambda e: e.tensor_tensor(out=ys[:, hf * 512:(hf + 1) * 512], in0=ps[:],
                                                            in1=bd_t[:, hf * 512:(hf + 1) * 512], op=ALU.add),
                             reads=[b_ps, b_bd], writes=[b_ys])
                    it, b_it = invs[s_]
                    S.dma(POOL, "st", lambda e: e.indirect_dma_start(
                        out=YT[:, :], out_offset=bass.IndirectOffsetOnAxis(ap=it[:, :], axis=0),
                        in_=ys[:], in_offset=None, bounds_check=bc_reg, oob_is_err=False), reads=[b_ys, b_it])

            wts = load_expert(0)
            XTc = load_x(0)
            for ex in range(NE):
                wts_n = load_expert(ex + 1) if ex + 1 < NE else None
                aTc = up_proj(ex, wts, *XTc)
                XTn, xnext = None, None
                if ex + 1 < NE:
                    XTn = XT_r.next()
                    xnext = (load_x_dma(ex + 1), XTn[0], XTn[1])
                down_proj(ex, wts, *aTc, xnext=xnext)
                wts, XTc = wts_n, XTn
            S.barrier()

        if STOP_AFTER >= 7:
          with ExitStack() as p7:
            def sb7(name, shape, dt):
                return p7.enter_context(nc.sbuf_tensor(name, shape, dt))

            g_ff = sb7("g_ff", [128, D], F32)
            bb_ff = sb7("bb_ff", [128, D], F32)
            b_par7 = Buf()
            bcast_load(g_ff[:], ln_ffn_g, b_par7)
            bcast_load(bb_ff[:], ln_ffn_b, b_par7)
            B7 = 4
            yk_r = Ring(sb7, "yk", [128, 4, D], F32, B7)
            h2l_r = Ring(sb7, "h2l", [128, D], F32, B7)
            acc_r = Ring(sb7, "acc", [128, D], F32, B7)
            o_r = Ring(sb7, "o7", [128, D], F32, B7)
            sc7 = ln_scratch(sb7, "l7", B7 + 1)

            def c0(cx):
                t = cx["t"]
                yk, b_yk = yk_r.next()
                S.dma(SP if t % 2 else POOL, "ga", lambda e: e.dma_start(
                    out=yk[:], in_=YT[t * 512:(t + 1) * 512, :].rearrange("(p k) d -> p k d", k=4)), writes=[b_yk])
                h2l, b_h2l = h2l_r.next()
                S.dma(POOL, "ld7", lambda e: e.dma_start(out=h2l[:], in_=H2d[t * 128:(t + 1) * 128, :]), writes=[b_h2l])
                cx.update(yk=yk, b_yk=b_yk, h2l=h2l, b_h2l=b_h2l)

            def c1(cx):
                t = cx["t"]
                yk, b_yk, h2l, b_h2l = cx["yk"], cx["b_yk"], cx["h2l"], cx["b_h2l"]
                acc, b_acc = acc_r.next()
                S.op(ACT, lambda e: e.mul(out=acc[:], in_=h2l[:], mul=ALPHA), reads=[b_h2l], writes=[b_acc])
                for k in range(4 if not os.environ.get('MK_NOSTT') else 1):
                    S.op(DVE, lambda e, k=k: e.scalar_tensor_tensor(out=acc[:], in0=yk[:, k, :], scalar=gate_all[:, t, k:k + 1],
                                                                    in1=acc[:], op0=ALU.mult, op1=ALU.add),
                         reads=[b_yk, b_gate, b_acc], writes=[b_acc])
                cx.update(acc=acc, b_acc=b_acc)

            def c2(cx):
                cx["st"] = ln_stats(sc7, cx["acc"][:], cx["b_acc"])

            def c3(cx):
                t = cx["t"]
                ot, b_ot = o_r.next()
                ln_apply(sc7, cx["acc"][:], cx["b_acc"], cx["st"], g_ff, bb_ff, b_par7, ot[:], b_ot)
                S.dma(SP, "out", lambda e: e.dma_start(out=out_d[t * 128:(t + 1) * 128, :], in_=ot[:]), reads=[b_ot])

            for b0 in range(0, NT, B7):
                cxs = [{"t": t} for t in range(b0, b0 + B7)]
                for st_fn in [c0, c1, c2, c3]:
                    for cx in cxs:
                        st_fn(cx)
            S.barrier()

        S.barrier()
        waited = S.waited
    return nc, waited


_INPUT_ORDER = ["x", "ln_in_g", "ln_in_b", "w_in", "gmlp_ln_g", "gmlp_ln_b", "w_spatial", "b_spatial", "w_branch_a",
                "w_branch_b", "w_out", "ln_mix_g", "ln_mix_b", "w_router", "b_router", "w_up", "b_up", "w_down",
                "b_down", "ln_ffn_g", "ln_ffn_b"]


def _prep_inputs(inputs):
    a = {k: np.ascontiguousarray(np.asarray(v), dtype=np.float32) for k, v in inputs.items()}
    shared = {
        "ln_in_g": a["ln_in_g"].reshape(D), "ln_in_b": a["ln_in_b"].reshape(D),
        "w_in": a["w_in"].reshape(D, 7 * D),
        "gmlp_ln_g": a["gmlp_ln_g"].reshape(D), "gmlp_ln_b": a["gmlp_ln_b"].reshape(D),
        "w_spatial": a["w_spatial"].reshape(4, 128, 128), "b_spatial": a["b_spatial"].reshape(512),
        "w_branch_a": a["w_branch_a"].reshape(D, D), "w_branch_b": a["w_branch_b"].reshape(D, D),
        "w_out": a["w_out"].reshape(D, D),
        "ln_mix_g": a["ln_mix_g"].reshape(D), "ln_mix_b": a["ln_mix_b"].reshape(D),
        "w_router": a["w_router"].reshape(D, NE), "b_router": a["b_router"].reshape(NE),
        "w_up": a["w_up"].reshape(NE, D, 2 * D), "b_up": a["b_up"].reshape(NE, 2 * D),
        "w_down": a["w_down"].reshape(NE, D, D), "b_down": a["b_down"].reshape(NE, D),
        "ln_ffn_g": a["ln_ffn_g"].reshape(D), "ln_ffn_b": a["ln_ffn_b"].reshape(D),
    }
    return a["x"], shared


def kernel(**inputs):
    x, shared = _prep_inputs(inputs)
    n = x.shape[0]
    nc = build_program()
    in_maps = []
    for b in range(n):
        m = dict(shared)
        m["x"] = np.ascontiguousarray(x[b])
        in_maps.append(m)
    res = run_bass_kernel_spmd(nc, in_maps, core_ids=list(range(n)))
    out = np.stack([np.asarray(r["out"]) for r in res.results], axis=0).astype(np.float32)
    return out
```
